# Optimizing a Trainium2 kernel written in Bass

```python
import math
import jax
import jax.numpy as jnp
from jax import lax
import numpy as np

D_MODEL = 2048
BATCH = 1
SEQ = 8192
DEPTH = 2

GRID_W = 64
CTX_LEN = 256
NORM_EPS = 1e-6
POS_BASE = 10000.0
CONV_W = 4
LRU_WIDTH = 1024
LRU_BLOCKS = 16
LRU_BLOCK = LRU_WIDTH // LRU_BLOCKS
LRU_C = 8.0
GLA_HEADS = 8
GLA_DK = 64
GLA_DV = 128
GLA_QK = GLA_HEADS * GLA_DK
GLA_V = GLA_HEADS * GLA_DV
GLA_RANK = 16
GLA_TAU = 16.0
GLA_CHUNK = 64
EVEN_PROJ = 2 * LRU_WIDTH + 2 * GLA_QK + 2 * GLA_V + 2 * GLA_RANK
EVEN_SPLITS = (LRU_WIDTH, 2 * LRU_WIDTH, 2 * LRU_WIDTH + GLA_QK, 2 * LRU_WIDTH + 2 * GLA_QK, 2 * LRU_WIDTH + 2 * GLA_QK + GLA_V, 2 * LRU_WIDTH + 2 * GLA_QK + 2 * GLA_V)
EVEN_OUT = LRU_WIDTH + GLA_V
SSD_INNER = 2 * D_MODEL
SSD_HEADDIM = 64
SSD_HEADS = SSD_INNER // SSD_HEADDIM
SSD_GROUPS = 8
SSD_STATE = 128
SSD_CHUNK = 64
SSD_CONV_DIM = SSD_INNER + 2 * SSD_GROUPS * SSD_STATE
ODD_PROJ = SSD_INNER + SSD_CONV_DIM + 2 * SSD_HEADS
ODD_SPLITS = (SSD_INNER, SSD_INNER + SSD_CONV_DIM)
N_EXPERTS = 32
N_EXPERT_GROUPS = 4
EXPERTS_PER_GROUP = N_EXPERTS // N_EXPERT_GROUPS
TOP_K = 2
D_EXPERT = 1024
MOE_BLOCK = 128

kernel_name = 'hybrid_lru_gla_ssd_moe_flow_block'


def rmsnorm(x, g):
    xf = x.astype(jnp.float32)
    y = xf * lax.rsqrt(jnp.mean(xf * xf, axis=-1, keepdims=True) + NORM_EPS)
    return (y * g.astype(jnp.float32)).astype(x.dtype)


def ada_norm(x, g, shift, scale):
    return rmsnorm(x, g) * (1 + scale) + shift


def head_rmsnorm(o, g):
    y = o * lax.rsqrt(jnp.mean(o * o, axis=-1, keepdims=True) + NORM_EPS)
    return y.reshape(o.shape[0], o.shape[1], -1) * g.astype(jnp.float32)


def pos_embed_2d(rows, cols, dim):
    quarter = dim // 4
    omega = 1.0 / (POS_BASE ** (jnp.arange(quarter, dtype=jnp.float32) / quarter))
    ang_r = jnp.arange(rows, dtype=jnp.float32)[:, None] * omega
    ang_c = jnp.arange(cols, dtype=jnp.float32)[:, None] * omega
    emb_r = jnp.concatenate([jnp.sin(ang_r), jnp.cos(ang_r)], axis=-1)
    emb_c = jnp.concatenate([jnp.sin(ang_c), jnp.cos(ang_c)], axis=-1)
    pos = jnp.concatenate([jnp.broadcast_to(emb_r[:, None], (rows, cols, dim // 2)), jnp.broadcast_to(emb_c[None], (rows, cols, dim // 2))], axis=-1)
    return pos.reshape(rows * cols, dim)


def dwconv(x, w, b):
    left = CONV_W // 2
    y = lax.conv_general_dilated(x, w[:, None, :], window_strides=(1,), padding=[(left, CONV_W - 1 - left)], dimension_numbers=('NWC', 'WIO', 'NWC'), feature_group_count=x.shape[-1])
    return y + b


def ctx_then_latent(scan_fn, ctx_in, lat_in, state0, reverse):
    if reverse:
        ctx_in = tuple(jnp.flip(t, axis=1) for t in ctx_in)
        lat_in = tuple(jnp.flip(t, axis=1) for t in lat_in)
    y_c, state_c = scan_fn(*ctx_in, state0)
    y_l, _ = scan_fn(*lat_in, state_c)
    if reverse:
        y_c, y_l = jnp.flip(y_c, axis=1), jnp.flip(y_l, axis=1)
    return y_c, y_l


def _lin_comb(left, right):
    a1, b1 = left
    a2, b2 = right
    return a1 * a2, a2 * b1 + b2


def lru_scan(a, b, h0):
    a_cum, b_cum = lax.associative_scan(_lin_comb, (a, b), axis=1)
    h = a_cum * h0[:, None] + b_cum
    return h, h[:, -1]


def gla_scan(q, k, v, log_g, h0):
    bsz, t, nh, dk = q.shape
    dv = v.shape[-1]
    nc = t // GLA_CHUNK
    qc = q.reshape(bsz, nc, GLA_CHUNK, nh, dk)
    kc = k.reshape(bsz, nc, GLA_CHUNK, nh, dk)
    vc = v.reshape(bsz, nc, GLA_CHUNK, nh, dv)
    cum = jnp.cumsum(log_g.astype(jnp.float32).reshape(bsz, nc, GLA_CHUNK, nh, dk), axis=2)
    last = cum[:, :, -1:]
    q_dec = qc * jnp.exp(cum)
    k_inv = kc * jnp.exp(-cum)
    k_end = kc * jnp.exp(last - cum)
    mask = jnp.tril(jnp.ones((GLA_CHUNK, GLA_CHUNK), dtype=bool))
    scores = jnp.where(mask, jnp.einsum('bnchd,bnshd->bnhcs', q_dec, k_inv), 0.0)
    o_intra = jnp.einsum('bnhcs,bnshe->bnche', scores, vc)
    upd = jnp.einsum('bnchd,bnche->bnhde', k_end, vc).astype(jnp.float32)
    decay = jnp.exp(last[:, :, 0])

    def step(s, inp):
        dec, u = inp
        return dec[..., None] * s + u, s

    s_fin, s_start = lax.scan(step, h0, (jnp.moveaxis(decay, 1, 0), jnp.moveaxis(upd, 1, 0)))
    o_inter = jnp.einsum('bnchd,bnhde->bnche', q_dec, jnp.moveaxis(s_start, 0, 1))
    return (o_intra + o_inter).reshape(bsz, t, nh, dv), s_fin


def ssd_scan(xdt, log_a, b_in, c_in, h0):
    bsz, t, nh, hp = xdt.shape
    ng, ns = b_in.shape[2], b_in.shape[3]
    hg = nh // ng
    nc = t // SSD_CHUNK
    xc = xdt.reshape(bsz, nc, SSD_CHUNK, ng, hg, hp)
    bc = b_in.reshape(bsz, nc, SSD_CHUNK, ng, ns)
    cc = c_in.reshape(bsz, nc, SSD_CHUNK, ng, ns)
    cum = jnp.cumsum(log_a.astype(jnp.float32).reshape(bsz, nc, SSD_CHUNK, ng, hg), axis=2)
    cum = jnp.moveaxis(cum, 2, -1)
    mask = jnp.tril(jnp.ones((SSD_CHUNK, SSD_CHUNK), dtype=bool))
    seg = jnp.exp(jnp.where(mask, cum[..., :, None] - cum[..., None, :], -jnp.inf))
    cb = jnp.einsum('bncgz,bnsgz->bngcs', cc, bc)
    y_intra = jnp.einsum('bngcs,bnghcs,bnsghp->bncghp', cb, seg, xc)
    to_end = jnp.exp(cum[..., -1:] - cum)
    chunk_states = jnp.einsum('bncgz,bnghc,bncghp->bnghpz', bc, to_end, xc).astype(jnp.float32)
    chunk_decay = jnp.exp(cum[..., -1])

    def step(s, inp):
        dec, st = inp
        return dec[..., None, None] * s + st, s

    s_fin, s_start = lax.scan(step, h0.reshape(bsz, ng, hg, hp, ns), (jnp.moveaxis(chunk_decay, 1, 0), jnp.moveaxis(chunk_states, 1, 0)))
    y_inter = jnp.einsum('bncgz,bnghpz,bnghc->bncghp', cc, jnp.moveaxis(s_start, 0, 1), jnp.exp(cum))
    y = (y_intra + y_inter).reshape(bsz, t, nh, hp)
    return y, s_fin.reshape(bsz, nh, hp, ns)


def even_mixer(h_c, h_l, w_in, conv_w, conv_b, wa, ba, wx, bx, lam, wg_up, bg, norm_g, w_out):
    def inputs(h):
        bsz, t, _ = h.shape
        proj = h @ w_in
        xa, ga, q, k, v, og, ad = jnp.split(proj, EVEN_SPLITS, axis=-1)
        xa = dwconv(xa, conv_w, conv_b)
        xb = xa.reshape(bsz, t, LRU_BLOCKS, LRU_BLOCK)
        r = jax.nn.sigmoid((jnp.einsum('btnk,dnkj->btdnj', xb, wa).reshape(bsz, t, 2, LRU_WIDTH) + ba).astype(jnp.float32))
        ig = jax.nn.sigmoid((jnp.einsum('btnk,dnkj->btdnj', xb, wx).reshape(bsz, t, 2, LRU_WIDTH) + bx).astype(jnp.float32))
        log_a = -LRU_C * r * jax.nn.softplus(-lam.astype(jnp.float32))
        a = jnp.exp(log_a)
        b = jnp.sqrt(-jnp.expm1(2.0 * log_a)) * ig * xa[:, :, None, :].astype(jnp.float32)
        q = q.reshape(bsz, t, GLA_HEADS, GLA_DK) * (GLA_DK ** -0.5)
        k = k.reshape(bsz, t, GLA_HEADS, GLA_DK)
        v = v.reshape(bsz, t, GLA_HEADS, GLA_DV)
        log_g = jax.nn.log_sigmoid((jnp.einsum('btdr,dre->btde', ad.reshape(bsz, t, 2, GLA_RANK), wg_up) + bg).astype(jnp.float32)) / GLA_TAU
        log_g = log_g.reshape(bsz, t, 2, GLA_HEADS, GLA_DK)
        return ga, og, a, b, q, k, v, log_g

    ga_c, og_c, a_c, b_c, q_c, k_c, v_c, g_c = inputs(h_c)
    ga_l, og_l, a_l, b_l, q_l, k_l, v_l, g_l = inputs(h_l)
    bsz = h_l.shape[0]
    s0_lru = jnp.zeros((bsz, LRU_WIDTH), jnp.float32)
    s0_gla = jnp.zeros((bsz, GLA_HEADS, GLA_DK, GLA_DV), jnp.float32)
    lru_c_f, lru_l_f = ctx_then_latent(lru_scan, (a_c[:, :, 0], b_c[:, :, 0]), (a_l[:, :, 0], b_l[:, :, 0]), s0_lru, False)
    lru_c_b, lru_l_b = ctx_then_latent(lru_scan, (a_c[:, :, 1], b_c[:, :, 1]), (a_l[:, :, 1], b_l[:, :, 1]), s0_lru, True)
    gla_c_f, gla_l_f = ctx_then_latent(gla_scan, (q_c, k_c, v_c, g_c[:, :, 0]), (q_l, k_l, v_l, g_l[:, :, 0]), s0_gla, False)
    gla_c_b, gla_l_b = ctx_then_latent(gla_scan, (q_c, k_c, v_c, g_c[:, :, 1]), (q_l, k_l, v_l, g_l[:, :, 1]), s0_gla, True)

    def merge(lru_f, lru_b, gla_f, gla_b, ga, og):
        y_a = (lru_f + lru_b) * jax.nn.gelu(ga.astype(jnp.float32))
        y_b = head_rmsnorm(gla_f + gla_b, norm_g) * jax.nn.silu(og.astype(jnp.float32))
        return jnp.concatenate([y_a, y_b], axis=-1).astype(w_out.dtype) @ w_out

    return (merge(lru_c_f, lru_c_b, gla_c_f, gla_c_b, ga_c, og_c), merge(lru_l_f, lru_l_b, gla_l_f, gla_l_b, ga_l, og_l))


def odd_mixer(h_c, h_l, w_in, conv_w, conv_b, a_log, dt_bias, d_skip, norm_g, w_out):
    a_neg = -jnp.exp(a_log.astype(jnp.float32))

    def inputs(h):
        bsz, t, _ = h.shape
        z, xbc, dt = jnp.split(h @ w_in, ODD_SPLITS, axis=-1)
        xbc = jax.nn.silu(dwconv(xbc, conv_w, conv_b))
        xs, bm, cm = jnp.split(xbc, (SSD_INNER, SSD_INNER + SSD_GROUPS * SSD_STATE), axis=-1)
        xs = xs.reshape(bsz, t, SSD_HEADS, SSD_HEADDIM)
        bm = bm.reshape(bsz, t, SSD_GROUPS, SSD_STATE)
        cm = cm.reshape(bsz, t, SSD_GROUPS, SSD_STATE)
        dt = jax.nn.softplus(dt.astype(jnp.float32).reshape(bsz, t, 2, SSD_HEADS) + dt_bias.astype(jnp.float32))
        return z, xs, bm, cm, dt

    def direction(xs, bm, cm, dt, d):
        return (xs.astype(jnp.float32) * dt[:, :, d, :, None], dt[:, :, d] * a_neg[d], bm, cm)

    z_c, x_c, b_c, c_c, dt_c = inputs(h_c)
    z_l, x_l, b_l, c_l, dt_l = inputs(h_l)
    s0 = jnp.zeros((h_l.shape[0], SSD_HEADS, SSD_HEADDIM, SSD_STATE), jnp.float32)
    yc_f, yl_f = ctx_then_latent(ssd_scan, direction(x_c, b_c, c_c, dt_c, 0), direction(x_l, b_l, c_l, dt_l, 0), s0, False)
    yc_b, yl_b = ctx_then_latent(ssd_scan, direction(x_c, b_c, c_c, dt_c, 1), direction(x_l, b_l, c_l, dt_l, 1), s0, True)

    def finish(y_f, y_b, xs, z):
        y = y_f + y_b + d_skip.astype(jnp.float32)[:, None] * xs.astype(jnp.float32)
        y = y.reshape(xs.shape[0], xs.shape[1], SSD_INNER)
        return rmsnorm(y * jax.nn.silu(z.astype(jnp.float32)), norm_g).astype(w_out.dtype) @ w_out

    return (finish(yc_f, yc_b, x_c, z_c), finish(yl_f, yl_b, x_l, z_l))


def moe_ffn(h, router_w, router_b, w_gate, w_up, w_down):
    n_tok, d = h.shape
    n_exp = w_gate.shape[0]
    scores = jax.nn.sigmoid((h @ router_w).astype(jnp.float32))
    sel = (scores + router_b.astype(jnp.float32)).reshape(n_tok, N_EXPERT_GROUPS, EXPERTS_PER_GROUP)
    group_score = lax.top_k(sel, TOP_K)[0].sum(-1)
    group = jnp.argmax(group_score, axis=-1)
    in_group = jnp.take_along_axis(sel, group[:, None, None], axis=1)[:, 0]
    _, local = lax.top_k(in_group, TOP_K)
    expert = group[:, None] * EXPERTS_PER_GROUP + local
    weight = jnp.take_along_axis(scores, expert, axis=1)
    weight = weight / weight.sum(-1, keepdims=True)
    flat_e = expert.reshape(-1)
    n_assign = flat_e.shape[0]
    order = jnp.argsort(flat_e)
    e_sorted = flat_e[order]
    tok_sorted = (order // TOP_K).astype(jnp.int32)
    w_sorted = weight.reshape(-1)[order]
    sizes = jnp.bincount(flat_e, length=n_exp)
    padded = (sizes + MOE_BLOCK - 1) // MOE_BLOCK * MOE_BLOCK
    start = jnp.cumsum(sizes) - sizes
    ends = jnp.cumsum(padded)
    pstart = ends - padded
    dest = pstart[e_sorted] + jnp.arange(n_assign) - start[e_sorted]
    n_blocks = -(-n_assign // MOE_BLOCK) + n_exp
    slot_tok = jnp.full((n_blocks * MOE_BLOCK,), n_tok, jnp.int32).at[dest].set(tok_sorted)
    slot_w = jnp.zeros((n_blocks * MOE_BLOCK,), jnp.float32).at[dest].set(w_sorted)
    block_exp = jnp.minimum(jnp.searchsorted(ends, jnp.arange(n_blocks) * MOE_BLOCK, side='right'), n_exp - 1)
    h_pad = jnp.concatenate([h, jnp.zeros((1, d), h.dtype)], axis=0)
    xb = h_pad[slot_tok].reshape(n_blocks, MOE_BLOCK, d)

    def expert_block(args):
        xe, e = args
        return (jax.nn.silu(xe @ w_gate[e]) * (xe @ w_up[e])) @ w_down[e]

    yb = lax.map(expert_block, (xb, block_exp)).reshape(-1, d)
    out = jnp.zeros((n_tok + 1, d), jnp.float32).at[slot_tok].add(yb.astype(jnp.float32) * slot_w[:, None])
    return out[:n_tok].astype(h.dtype)


def setup_inputs(seed: int = 0) -> dict:
    key = jax.random.key(seed)
    ks = iter(jax.random.split(key, 64))
    f32 = jnp.float32
    dm = D_MODEL
    n_even = (DEPTH + 1) // 2
    n_odd = DEPTH // 2

    def nrm(shape, scale):
        return jax.random.normal(next(ks), shape, f32) * scale

    def unif(shape, lo, hi):
        return jax.random.uniform(next(ks), shape, f32, lo, hi)

    lru_s = unif((n_even, 2, LRU_WIDTH), 0.9, 0.999) ** (1.0 / LRU_C)
    dt0 = jnp.exp(unif((n_odd, 2, SSD_HEADS), math.log(1e-3), math.log(1e-1)))
    return {
        'x': nrm((BATCH, SEQ, dm), 1.0),
        'c': nrm((BATCH, dm), 1.0),
        'ctx': nrm((BATCH, CTX_LEN, dm), 1.0),
        'c_ctx': nrm((dm,), 1.0),
        'mod_w': nrm((DEPTH, dm, 6 * dm), 0.5 * dm ** -0.5),
        'mod_b': nrm((DEPTH, 6 * dm), 0.02),
        'norm1_g': 1.0 + nrm((DEPTH, dm), 0.02),
        'norm2_g': 1.0 + nrm((DEPTH, dm), 0.02),
        'ev_w_in': nrm((n_even, dm, EVEN_PROJ), dm ** -0.5),
        'ev_conv_w': nrm((n_even, CONV_W, LRU_WIDTH), CONV_W ** -0.5),
        'ev_conv_b': nrm((n_even, LRU_WIDTH), 0.02),
        'lru_wa': nrm((n_even, 2, LRU_BLOCKS, LRU_BLOCK, LRU_BLOCK), LRU_BLOCK ** -0.5),
        'lru_ba': nrm((n_even, 2, LRU_WIDTH), 0.02),
        'lru_wx': nrm((n_even, 2, LRU_BLOCKS, LRU_BLOCK, LRU_BLOCK), LRU_BLOCK ** -0.5),
        'lru_bx': nrm((n_even, 2, LRU_WIDTH), 0.02),
        'lru_lambda': jnp.log(lru_s) - jnp.log1p(-lru_s),
        'gla_wg_up': nrm((n_even, 2, GLA_RANK, GLA_QK), GLA_RANK ** -0.5),
        'gla_bg': unif((n_even, 2, GLA_QK), 1.0, 4.0),
        'gla_norm_g': 1.0 + nrm((n_even, GLA_V), 0.02),
        'ev_w_out': nrm((n_even, EVEN_OUT, dm), EVEN_OUT ** -0.5),
        'od_w_in': nrm((n_odd, dm, ODD_PROJ), dm ** -0.5),
        'od_conv_w': nrm((n_odd, CONV_W, SSD_CONV_DIM), CONV_W ** -0.5),
        'od_conv_b': nrm((n_odd, SSD_CONV_DIM), 0.02),
        'ssd_a_log': jnp.log(unif((n_odd, 2, SSD_HEADS), 1.0, 16.0)),
        'ssd_dt_bias': dt0 + jnp.log(-jnp.expm1(-dt0)),
        'ssd_d': 1.0 + nrm((n_odd, SSD_HEADS), 0.1),
        'ssd_norm_g': 1.0 + nrm((n_odd, SSD_INNER), 0.02),
        'od_w_out': nrm((n_odd, SSD_INNER, dm), SSD_INNER ** -0.5),
        'router_w': nrm((dm, N_EXPERTS), dm ** -0.5),
        'router_b': nrm((N_EXPERTS,), 0.01),
        'exp_w_gate': nrm((DEPTH, N_EXPERTS, dm, D_EXPERT), dm ** -0.5),
        'exp_w_up': nrm((DEPTH, N_EXPERTS, dm, D_EXPERT), dm ** -0.5),
        'exp_w_down': nrm((DEPTH, N_EXPERTS, D_EXPERT, dm), D_EXPERT ** -0.5),
        'final_norm_g': 1.0 + nrm((dm,), 0.02),
    }


def reference(x, c, ctx, c_ctx, mod_w, mod_b, norm1_g, norm2_g, ev_w_in, ev_conv_w, ev_conv_b, lru_wa, lru_ba, lru_wx, lru_bx, lru_lambda, gla_wg_up, gla_bg, gla_norm_g, ev_w_out, od_w_in, od_conv_w, od_conv_b, ssd_a_log, ssd_dt_bias, ssd_d, ssd_norm_g, od_w_out, router_w, router_b, exp_w_gate, exp_w_up, exp_w_down, final_norm_g):
    bsz, seq, dm = x.shape
    rows = seq // GRID_W
    x_l = x + pos_embed_2d(rows, GRID_W, dm).astype(x.dtype)[None]
    x_c = ctx
    for i in range(DEPTH):
        last = i == DEPTH - 1
        j = i // 2
        mod_l = jax.nn.silu(c) @ mod_w[i] + mod_b[i]
        mod_c = jax.nn.silu(c_ctx)[None] @ mod_w[i] + mod_b[i]
        sh1_l, sc1_l, g1_l, sh2_l, sc2_l, g2_l = jnp.split(mod_l[:, None, :], 6, axis=-1)
        sh1_c, sc1_c, g1_c, sh2_c, sc2_c, g2_c = jnp.split(mod_c[:, None, :], 6, axis=-1)
        h_l = ada_norm(x_l, norm1_g[i], sh1_l, sc1_l)
        h_c = ada_norm(x_c, norm1_g[i], sh1_c, sc1_c)
        if i % 2 == 0:
            y_c, y_l = even_mixer(h_c, h_l, ev_w_in[j], ev_conv_w[j], ev_conv_b[j], lru_wa[j], lru_ba[j], lru_wx[j], lru_bx[j], lru_lambda[j], gla_wg_up[j], gla_bg[j], gla_norm_g[j], ev_w_out[j])
        else:
            y_c, y_l = odd_mixer(h_c, h_l, od_w_in[j], od_conv_w[j], od_conv_b[j], ssd_a_log[j], ssd_dt_bias[j], ssd_d[j], ssd_norm_g[j], od_w_out[j])
        x_l = x_l + (g1_l * y_l).astype(x_l.dtype)
        h2_l = ada_norm(x_l, norm2_g[i], sh2_l, sc2_l)
        if last:
            f_l = moe_ffn(h2_l.reshape(-1, dm), router_w, router_b, exp_w_gate[i], exp_w_up[i], exp_w_down[i])
            x_l = x_l + (g2_l * f_l.reshape(x_l.shape)).astype(x_l.dtype)
        else:
            x_c = x_c + (g1_c * y_c).astype(x_c.dtype)
            h2_c = ada_norm(x_c, norm2_g[i], sh2_c, sc2_c)
            n_ctx = x_c.shape[0] * x_c.shape[1]
            f = moe_ffn(jnp.concatenate([h2_c.reshape(-1, dm), h2_l.reshape(-1, dm)], axis=0), router_w, router_b, exp_w_gate[i], exp_w_up[i], exp_w_down[i])
            x_c = x_c + (g2_c * f[:n_ctx].reshape(x_c.shape)).astype(x_c.dtype)
            x_l = x_l + (g2_l * f[n_ctx:].reshape(x_l.shape)).astype(x_l.dtype)
    return rmsnorm(x_l, final_norm_g)
```

```python
import contextlib
import math
import numpy as np
import ml_dtypes
import concourse.bass as bass
import concourse.mybir as mybir
from concourse.bass_utils import run_bass_kernel_spmd

F32 = mybir.dt.float32
BF16 = mybir.dt.bfloat16
I32 = mybir.dt.int32
AF = mybir.ActivationFunctionType
ALU = mybir.AluOpType
AX = mybir.AxisListType
NPBF = ml_dtypes.bfloat16

NCORES = 8
D = 2048
SEQ = 8192
CTX = 256
T_ALL = SEQ + CTX
NCH = T_ALL // 128
EPS = 1e-6

SAME_ENGINE_SYNC = True


class Prog:
    def __init__(self):
        self.nc = bass.Bass("TRN2", target_bir_lowering=False)
        nc = self.nc
        self.es = contextlib.ExitStack()
        self.eng = {"pe": nc.tensor, "act": nc.scalar, "dve": nc.vector, "pool": nc.gpsimd, "sp": nc.sync}
        self.esem = {}
        self.ecnt = {}
        self.seen = {}
        for e in self.eng:
            self.esem[e] = self.es.enter_context(nc.semaphore("sem_" + e))
            self.ecnt[e] = 0
            self.seen[e] = {}
        self.lastw = {}
        self.readers = {}
        self.dsem = {}
        self.out_tokens = []
        self.nuniq = 0

    def _uid(self):
        self.nuid = getattr(self, 'nuid', 0) + 1
        return self.nuid

    def din(self, name, shape, dt=F32):
        return self.nc.dram_tensor(name, list(shape), dt, kind="ExternalInput").ap()

    def dout(self, name, shape, dt=F32):
        return self.nc.dram_tensor(name, list(shape), dt, kind="ExternalOutput").ap()

    def dscr(self, name, shape, dt=F32):
        return self.nc.dram_tensor(name, list(shape), dt, kind="Internal").ap()

    def sb(self, name, shape, dt=F32):
        return self.es.enter_context(self.nc.sbuf_tensor("s%d_" % self._uid() + name, list(shape), dt))

    def ps(self, name, shape, dt=F32):
        return self.es.enter_context(self.nc.psum_tensor("p%d_" % self._uid() + name, list(shape), dt))

    def _waits(self, e, r, w):
        toks = []
        for k in r:
            t = self.lastw.get(k)
            if t is not None:
                toks.append(t)
        for k in w:
            t = self.lastw.get(k)
            if t is not None:
                toks.append(t)
            toks.extend(self.readers.get(k, ()))
        eng = self.eng[e]
        best = {}
        for (sem, val, sid, owner) in toks:
            if owner == e and (e == "pe" or not SAME_ENGINE_SYNC):
                continue
            if self.seen[e].get(sid, 0) >= val:
                continue
            if sid not in best or best[sid][1] < val:
                best[sid] = (sem, val)
        for sid, (sem, val) in best.items():
            eng.wait_ge(sem, val)
            self.seen[e][sid] = val

    def _update(self, r, w, tok):
        for k in r:
            self.readers.setdefault(k, []).append(tok)
        for k in w:
            self.lastw[k] = tok
            self.readers[k] = []

    def op(self, e, fn, r=(), w=()):
        self._waits(e, r, w)
        inst = fn(self.eng[e])
        self.ecnt[e] += 1
        inst.then_inc(self.esem[e], 1)
        tok = (self.esem[e], self.ecnt[e], "E" + e, e)
        self._update(r, w, tok)
        return tok

    def dma(self, q, out, in_, r=(), w=(), sk=None, **kw):
        if sk is None:
            sk = (w[0] if (w and not str(w[0]).startswith("out:") and not str(w[0]).startswith("d:")) else r[0])
        self._waits(q, r, w)
        if sk not in self.dsem:
            self.nuniq += 1
            self.dsem[sk] = [self.es.enter_context(self.nc.semaphore("dq%d" % self.nuniq)), 0]
        ent = self.dsem[sk]
        ent[1] += 16
        self.eng[q].dma_start(out=out, in_=in_, **kw).then_inc(ent[0], 16)
        tok = (ent[0], ent[1], "D" + str(sk), None)
        self._update(r, w, tok)
        for k in w:
            if str(k).startswith("out:"):
                self.out_tokens.append(tok)
        return tok

    def finish(self):
        eng = self.eng["sp"]
        best = {}
        for (sem, val, sid, owner) in self.out_tokens:
            if sid not in best or best[sid][1] < val:
                best[sid] = (sem, val)
        for sid, (sem, val) in best.items():
            eng.wait_ge(sem, val)
        self.es.close()
        return self.nc


def run_prog(P, in_maps):
    nc = P.finish()
    res = run_bass_kernel_spmd(nc, in_maps, core_ids=list(range(NCORES)))
    return res.results


def feat_pc(v):
    return np.ascontiguousarray(np.asarray(v, np.float32).reshape(-1, 128).T)


def build_k0():
    P = Prog()
    NC_ = 1536
    cvT = P.din("cvT", [128, 16, 2])
    w = P.din("w", [2, D, NC_])
    b = P.din("b", [2, NC_])
    o = P.dout("o", [2, 2, NC_])
    s_raw = P.sb("s_raw", [128, 16, 2])
    s = P.sb("s", [128, 16, 2])
    W = P.sb("W", [128, 16, NC_])
    bt = P.sb("bt", [2, 2, NC_])
    ot = P.sb("ot", [2, 2, NC_])
    pss = [P.ps("ps%d" % i, [2, 512]) for i in range(3)]
    P.dma("sp", s_raw[:], cvT[:, :, :], w=["s_raw"])
    P.op("act", lambda e: e.activation(out=s[:], in_=s_raw[:], func=AF.Silu), r=["s_raw"], w=["s"])
    for i in range(2):
        P.dma("sp", bt[:, i, :], b[i].partition_broadcast(2), w=["bt%d" % i])
    for i in range(2):
        wv = w[i].rearrange("(c p) n -> p c n", p=128)
        for g in range(4):
            P.dma("sp" if g % 2 == 0 else "pool", W[:, 4 * g:4 * g + 4, :], wv[:, 4 * g:4 * g + 4, :], w=["W%d" % g])
        for nb in range(3):
            for c in range(16):
                P.op("pe", lambda e, c=c, nb=nb: e.matmul(pss[nb][:], lhsT=s[:, c, :], rhs=W[:, c, nb * 512:(nb + 1) * 512],
                                                          start=(c == 0), stop=(c == 15)),
                     r=["s", "W%d" % (c // 4)], w=["ps%d" % nb])
            P.op("dve", lambda e, nb=nb, i=i: e.tensor_tensor(out=ot[:, i, nb * 512:(nb + 1) * 512], in0=pss[nb][:],
                                                              in1=bt[:, i, nb * 512:(nb + 1) * 512], op=ALU.add),
                 r=["ps%d" % nb, "bt%d" % i], w=["ot%d_%d" % (i, nb)])
    for i in range(2):
        P.dma("sp", o[i], ot[:, i, :], r=["ot%d_%d" % (i, nb) for nb in range(3)], w=["out:o%d" % i], sk="ot%d" % i)
    return P


def run_k0(c, c_ctx, mod_w, mod_b):
    P = build_k0()
    cv = np.stack([np.asarray(c, np.float32).reshape(-1), np.asarray(c_ctx, np.float32).reshape(-1)], -1)
    cvT = np.ascontiguousarray(cv.reshape(16, 128, 2).transpose(1, 0, 2))
    maps = []
    for j in range(NCORES):
        sl = slice(j * 1536, (j + 1) * 1536)
        maps.append({"cvT": cvT, "w": np.ascontiguousarray(mod_w[:, :, sl]), "b": np.ascontiguousarray(mod_b[:, sl])})
    res = run_prog(P, maps)
    return np.concatenate([r["o"] for r in res], axis=-1)


def _barrier(self):
    for e, eng in self.eng.items():
        for e2 in self.eng:
            if e2 == e or self.ecnt[e2] == 0:
                continue
            if self.seen[e].get("E" + e2, 0) < self.ecnt[e2]:
                eng.wait_ge(self.esem[e2], self.ecnt[e2])
                self.seen[e]["E" + e2] = self.ecnt[e2]
        for sk, (sem, cnt) in self.dsem.items():
            if cnt and self.seen[e].get("D" + str(sk), 0) < cnt:
                eng.wait_ge(sem, cnt)
                self.seen[e]["D" + str(sk)] = cnt


@contextlib.contextmanager
def _phase(self):
    old = self.es
    st = contextlib.ExitStack()
    self.es_sems = old
    self.es = st
    try:
        yield
    finally:
        self.barrier()
        self.es = old
        st.close()


Prog.barrier = _barrier
Prog.phase = _phase


def _dma2(self, q, out, in_, r=(), w=(), sk=None, **kw):
    cur = self.es
    self.es = self.root_es
    try:
        return Prog._dma_orig(self, q, out, in_, r=r, w=w, sk=sk, **kw)
    finally:
        self.es = cur


Prog._dma_orig = Prog.dma
Prog.dma = _dma2
_old_init = Prog.__init__


def _init2(self):
    _old_init(self)
    self.root_es = self.es


Prog.__init__ = _init2
_old_finish = Prog.finish


def _finish2(self):
    self.es = self.root_es
    return _old_finish(self)


Prog.finish = _finish2


def consts_np():
    s = np.arange(128)[:, None]
    c = np.arange(128)[None, :]
    ident = np.eye(128, dtype=np.float32)
    tri = np.stack([(s <= c), (s >= c)]).astype(np.float32)
    strict = np.stack([(s > c), (s < c)]).astype(np.float32)
    return ident, tri, strict


def pos_tables():
    quarter = D // 4
    omega = (1.0 / (10000.0 ** (np.arange(quarter, dtype=np.float32) / np.float32(quarter)))).astype(np.float32)
    ang_r = np.arange(128, dtype=np.float32)[:, None] * omega
    ang_c = np.arange(64, dtype=np.float32)[:, None] * omega
    emb_r = np.concatenate([np.sin(ang_r), np.cos(ang_r)], -1).astype(np.float32)
    emb_c = np.concatenate([np.sin(ang_c), np.cos(ang_c)], -1).astype(np.float32)
    return emb_r, np.concatenate([emb_c, emb_c], 0)


def emit_hT(P, PS, n_chunks, src_fn, pos_fn, ab_fn, embr, embc2_d, ident, dst_fn, dst_key_fn, pfx="h"):
    xt = [P.sb(pfx + "xt%d" % b, [128, D]) for b in range(2)]
    pr = [P.sb(pfx + "pr%d" % b, [128, 1024]) for b in range(2)]
    pc = P.sb(pfx + "pc", [128, 1024])
    junk = P.sb(pfx + "junk", [128, D], BF16)
    xn = P.sb(pfx + "xn", [128, D])
    st = P.sb(pfx + "st", [128, 4])
    hTt = [P.sb(pfx + "hTo%d" % b, [128, 16, 128], BF16) for b in range(2)]
    if embc2_d is not None:
        P.dma("sp", pc[:], embc2_d[:, :], w=[pfx + "pc"])

    def load(n):
        b = n % 2
        src = src_fn(n)
        P.dma("sp", xt[b][:, 0:1024], src[:, 0:1024], w=[pfx + "xt%da" % b])
        P.dma("pool", xt[b][:, 1024:2048], src[:, 1024:2048], w=[pfx + "xt%db" % b])
        r0 = pos_fn(n)
        if r0 is not None:
            P.dma("sp", pr[b][0:64, :], embr[r0].partition_broadcast(64), w=[pfx + "pr%d" % b], sk=pfx + "pr%dlo" % b)
            P.dma("sp", pr[b][64:128, :], embr[r0 + 1].partition_broadcast(64), w=[pfx + "pr%dh" % b], sk=pfx + "pr%dhi" % b)

    import os
    dbg = int(os.environ.get("HTDBG", "99"))
    if dbg < 99:
        n_chunks = 3
    load(0)
    for n in range(n_chunks):
        b = n % 2
        if n + 1 < n_chunks:
            load(n + 1)
        ka, kb = pfx + "xt%da" % b, pfx + "xt%db" % b
        if pos_fn(n) is not None:
            P.op("pool", lambda e: e.tensor_tensor(out=xt[b][:, 0:1024], in0=xt[b][:, 0:1024], in1=pr[b][:], op=ALU.add),
                 r=[ka, pfx + "pr%d" % b, pfx + "pr%dh" % b], w=[ka])
            P.op("dve", lambda e: e.tensor_tensor(out=xt[b][:, 1024:2048], in0=xt[b][:, 1024:2048], in1=pc[:], op=ALU.add),
                 r=[kb, pfx + "pc"], w=[kb])
        if dbg < 1:
            continue
        P.op("act", lambda e: e.activation(out=junk[:], in_=xt[b][:], func=AF.Square, accum_out=st[:, 0:1]),
             r=[ka, kb], w=[pfx + "junk", pfx + "st0"])
        P.op("dve", lambda e: e.tensor_scalar(out=st[:, 1:2], in0=st[:, 0:1], scalar1=1.0 / D, scalar2=EPS, op0=ALU.mult, op1=ALU.add),
             r=[pfx + "st0"], w=[pfx + "st1"])
        P.op("act", lambda e: e.activation(out=st[:, 2:3], in_=st[:, 1:2], func=AF.Sqrt), r=[pfx + "st1"], w=[pfx + "st2"])
        P.op("dve", lambda e: e.reciprocal(out=st[:, 3:4], in_=st[:, 2:3]), r=[pfx + "st2"], w=[pfx + "st3"])
        P.op("pool", lambda e: e.tensor_scalar(out=xn[:], in0=xt[b][:], scalar1=st[:, 3:4], scalar2=0.0, op0=ALU.mult, op1=ALU.add),
             r=[ka, kb, pfx + "st3"], w=[pfx + "xn"])
        if dbg < 2:
            continue
        A, Bv, keyA, keyB = ab_fn(n)
        for k4 in range(4):
            bank = PS[k4]
            bk = "B%d" % k4
            for q in range(4):
                c = 4 * k4 + q
                P.op("pe", lambda e, c=c, q=q: e.transpose(out=bank[:, q * 128:(q + 1) * 128], in_=xn[:, c * 128:(c + 1) * 128], identity=ident[:]),
                     r=[pfx + "xn", "ident"], w=[bk])
            for q in range(4):
                c = 4 * k4 + q
                if k4 % 2 == 0:
                    P.op("act", lambda e, c=c, q=q: e.activation(out=hTt[b][:, c, :], in_=bank[:, q * 128:(q + 1) * 128], func=AF.Identity,
                                                                   scale=A[:, c:c + 1], bias=Bv[:, c:c + 1]),
                         r=[keyA, keyB], w=[bk, pfx + "hTo%d_%d" % (b, c)])
                else:
                    P.op("dve", lambda e, c=c, q=q: e.tensor_scalar(out=hTt[b][:, c, :], in0=bank[:, q * 128:(q + 1) * 128],
                                                                      scalar1=A[:, c:c + 1], scalar2=Bv[:, c:c + 1], op0=ALU.mult, op1=ALU.add),
                         r=[keyA, keyB], w=[bk, pfx + "hTo%d_%d" % (b, c)])
        if dbg < 3:
            continue
        P.dma("sp", dst_fn(n), hTt[b][:], r=[pfx + "hTo%d_%d" % (b, c) for c in range(16)], w=[dst_key_fn(n)], sk=pfx + "hTo%d" % b)


def emit_AB(P, gn_d, sc_d, sh_d, name):
    g = P.sb(name + "g", [128, 16])
    sc = P.sb(name + "sc", [128, 16])
    A = P.sb(name + "A", [128, 16])
    Bv = P.sb(name + "B", [128, 16])
    P.dma("sp", g[:], gn_d[:, :], w=[name + "g"])
    P.dma("sp", sc[:], sc_d[:, :], w=[name + "sc"])
    P.dma("sp", Bv[:], sh_d[:, :], w=[name + "B"])
    P.op("dve", lambda e: e.scalar_tensor_tensor(out=A[:], in0=sc[:], scalar=1.0, in1=g[:], op0=ALU.add, op1=ALU.mult),
         r=[name + "g", name + "sc"], w=[name + "A"])
    return A, Bv, name + "A", name + "B"


K1_COLS = 736
LSEGS = [(0, 256)] + [(256 + 512 * k, 512) for k in range(16)]


def xa_col(t):
    return 2 + t if t < 256 else 261 + (t - 256)


def build_k1(stop=9):
    P = Prog()
    xl = P.din("xl", [SEQ, D])
    xc = P.din("xc", [CTX, D])
    embr = P.din("embr", [128, 1024])
    embc2 = P.din("embc2", [128, 1024])
    gn = P.din("gn", [128, 16])
    scl = P.din("scl", [128, 16]); shl = P.din("shl", [128, 16])
    scc = P.din("scc", [128, 16]); shc = P.din("shc", [128, 16])
    win = P.din("win", [D, K1_COLS])
    convw = P.din("convw", [128, 4]); convb = P.din("convb", [128, 1])
    bd = P.din("bd", [4, 128, 128])
    gb = P.din("gb", [128, 4])
    lam = P.din("lam", [128, 2])
    wg = P.din("wg", [2, 16, 64]); bg = P.din("bg", [2, 64])
    gng = P.din("gng", [128])
    identd = P.din("ident", [128, 128]); trid = P.din("tri", [2, 128, 128]); strd = P.din("strict", [2, 128, 128])
    yaT = P.dout("yaT", [128, T_ALL], BF16)
    yb = P.dout("yb", [T_ALL, 128], BF16)
    hTd = P.dscr("hTd", [NCH, 128, 16, 128], BF16)

    PS = [P.ps("PS%d" % i, [128, 512]) for i in range(8)]
    ident = P.sb("ident", [128, 128])
    P.dma("sp", ident[:], identd[:, :], w=["ident"])
    Wb = P.sb("Wb", [128, 16, K1_COLS], BF16)
    winv = win.rearrange("(c p) n -> p c n", p=128)
    for g4 in range(4):
        P.dma("pool", Wb[:, 4 * g4:4 * g4 + 4, :], winv[:, 4 * g4:4 * g4 + 4, :], w=["Wb%d" % g4])
    WK = ["Wb%d" % g4 for g4 in range(4)]

    with P.phase():
        Al = emit_AB(P, gn, scl, shl, "l")
        Ac = emit_AB(P, gn, scc, shc, "c")
        emit_hT(P, PS, NCH,
                src_fn=lambda n: (xc[n * 128:(n + 1) * 128, :] if n < 2 else xl[(n - 2) * 128:(n - 1) * 128, :]),
                pos_fn=lambda n: (None if n < 2 else 2 * (n - 2)),
                ab_fn=lambda n: (Ac if n < 2 else Al),
                embr=embr, embc2_d=embc2, ident=ident,
                dst_fn=lambda n: hTd[n], dst_key_fn=lambda n: "d:hT%d" % n)

    if stop < 1:
        return P
    with P.phase():
        XW = 8454
        XA = P.sb("XA", [128, XW])
        GG = P.sb("GG", [128, T_ALL], BF16)
        HF = P.sb("HF", [128, T_ALL])
        hTt = [P.sb("hTi%d" % b, [128, 16, 128], BF16) for b in range(2)]
        cw = P.sb("cw", [128, 4]); cb = P.sb("cb", [128, 1]); gbt = P.sb("gbt", [128, 4]); lamt = P.sb("lamt", [128, 2])
        cd = P.sb("cd", [128, 4])
        BD = P.sb("BD", [128, 4, 128], BF16)
        P.dma("sp", cw[:], convw[:, :], w=["cw"]); P.dma("sp", cb[:], convb[:, :], w=["cb"])
        P.dma("sp", gbt[:], gb[:, :], w=["gbt"]); P.dma("sp", lamt[:], lam[:, :], w=["lamt"])
        for i in range(4):
            P.dma("pool", BD[:, i, :], bd[i], w=["BD%d" % i])
        tmp2 = P.sb("tmp2", [128, 2])
        P.op("act", lambda e: e.activation(out=tmp2[:], in_=lamt[:], func=AF.Exp, scale=-1.0), r=["lamt"], w=["tmp2"])
        P.op("act", lambda e: e.activation(out=tmp2[:], in_=tmp2[:], func=AF.Ln, bias=1.0), r=["tmp2"], w=["tmp2"])
        P.op("dve", lambda e: e.tensor_scalar(out=cd[:, 0:2], in0=tmp2[:], scalar1=-8.0, scalar2=None, op0=ALU.mult), r=["tmp2"], w=["cd"])
        P.op("dve", lambda e: e.tensor_scalar(out=cd[:, 2:4], in0=tmp2[:], scalar1=-16.0, scalar2=None, op0=ALU.mult), r=["tmp2", "cd"], w=["cd"])
        P.op("pool", lambda e: e.memset(XA[:], 0.0), w=["XA"])
        gx = P.sb("gx", [128, 128]); gx2 = P.sb("gx2", [128, 128]); gs = P.sb("gs", [128, 128])

        def loadh(n):
            P.dma("sp", hTt[n % 2][:], hTd[n], r=["d:hT%d" % n], w=["hTi%d" % (n % 2)])
        loadh(0)
        for n in range(NCH):
            b = n % 2
            if n + 1 < NCH:
                loadh(n + 1)
            pa, pg = PS[2 * b], PS[2 * b + 1]
            for c in range(16):
                P.op("pe", lambda e, c=c: e.matmul(pa[:, 0:128], lhsT=Wb[:, c, 0:128], rhs=hTt[b][:, c, :], start=(c == 0), stop=(c == 15)),
                     r=["hTi%d" % b, WK[c // 4]], w=["PS%d" % (2 * b)])
            for c in range(16):
                P.op("pe", lambda e, c=c: e.matmul(pg[:, 0:128], lhsT=Wb[:, c, 128:256], rhs=hTt[b][:, c, :], start=(c == 0), stop=(c == 15)),
                     r=["hTi%d" % b, WK[c // 4]], w=["PS%d" % (2 * b + 1)])
            col = xa_col(n * 128)
            P.op("act", lambda e: e.activation(out=XA[:, col:col + 128], in_=pa[:, 0:128], func=AF.Copy), r=["PS%d" % (2 * b)], w=["XA"])
            P.op("act", lambda e: e.activation(out=gx[:], in_=pg[:, 0:128], func=AF.Copy), r=["PS%d" % (2 * b + 1)], w=["gx"])
            P.op("dve", lambda e: e.tensor_tensor(out=gx2[:], in0=gx[:], in1=gx[:], op=ALU.mult), r=["gx"], w=["gx2"])
            P.op("dve", lambda e: e.tensor_scalar(out=gx2[:], in0=gx2[:], scalar1=0.044715, scalar2=1.0, op0=ALU.mult, op1=ALU.add), r=["gx2"], w=["gx2"])
            P.op("dve", lambda e: e.tensor_tensor(out=gx2[:], in0=gx2[:], in1=gx[:], op=ALU.mult), r=["gx2", "gx"], w=["gx2"])
            P.op("act", lambda e: e.activation(out=gs[:], in_=gx2[:], func=AF.Sigmoid, scale=1.5957691216057308), r=["gx2"], w=["gs"])
            P.op("dve", lambda e: e.tensor_tensor(out=GG[:, n * 128:(n + 1) * 128], in0=gs[:], in1=gx[:], op=ALU.mult), r=["gs", "gx"], w=["GG"])
        L = 512
        xcv = P.sb("xcv", [128, L]); xcb = P.sb("xcb", [128, L], BF16)
        rr = P.sb("rr", [128, L]); ii = P.sb("ii", [128, L]); aa = P.sb("aa", [128, L]); tt = P.sb("tt", [128, L]); bb = P.sb("bb", [128, L])
        hb = P.sb("hb", [128, L]); yo = [P.sb("yo%d" % b, [128, L], BF16) for b in range(2)]
        carry = P.sb("carry", [128, 1])
        for d in range(2):
            order = LSEGS if d == 0 else [LSEGS[0]] + LSEGS[:0:-1]
            for si, (t0, Ls) in enumerate(order):
                c0 = xa_col(t0)
                P.op("dve", lambda e: e.tensor_scalar(out=xcv[:, :Ls], in0=XA[:, c0 - 2:c0 - 2 + Ls], scalar1=cw[:, 0:1], scalar2=cb[:, 0:1], op0=ALU.mult, op1=ALU.add),
                     r=["XA", "cw", "cb"], w=["xcv"])
                for j in range(1, 4):
                    P.op("dve", lambda e, j=j: e.scalar_tensor_tensor(out=xcv[:, :Ls], in0=XA[:, c0 - 2 + j:c0 - 2 + j + Ls], scalar=cw[:, j:j + 1], in1=xcv[:, :Ls], op0=ALU.mult, op1=ALU.add),
                         r=["XA", "cw", "xcv"], w=["xcv"])
                P.op("pool", lambda e: e.tensor_copy(out=xcb[:, :Ls], in_=xcv[:, :Ls]), r=["xcv"], w=["xcb"])
                P.op("pe", lambda e: e.matmul(PS[0][:, :Ls], lhsT=BD[:, d, :], rhs=xcb[:, :Ls], start=True, stop=True), r=["xcb", "BD%d" % d], w=["PS0"])
                P.op("pe", lambda e: e.matmul(PS[1][:, :Ls], lhsT=BD[:, 2 + d, :], rhs=xcb[:, :Ls], start=True, stop=True), r=["xcb", "BD%d" % (2 + d)], w=["PS1"])
                P.op("act", lambda e: e.activation(out=rr[:, :Ls], in_=PS[0][:, :Ls], func=AF.Sigmoid, bias=gbt[:, d:d + 1]), r=["PS0", "gbt"], w=["rr"])
                P.op("act", lambda e: e.activation(out=ii[:, :Ls], in_=PS[1][:, :Ls], func=AF.Sigmoid, bias=gbt[:, 2 + d:3 + d]), r=["PS1", "gbt"], w=["ii"])
                P.op("act", lambda e: e.activation(out=aa[:, :Ls], in_=rr[:, :Ls], func=AF.Exp, scale=cd[:, d:d + 1]), r=["rr", "cd"], w=["aa"])
                P.op("act", lambda e: e.activation(out=tt[:, :Ls], in_=rr[:, :Ls], func=AF.Exp, scale=cd[:, 2 + d:3 + d]), r=["rr", "cd"], w=["tt"])
                P.op("act", lambda e: e.activation(out=tt[:, :Ls], in_=tt[:, :Ls], func=AF.Sqrt, scale=-1.0, bias=1.0), r=["tt"], w=["tt"])
                P.op("dve", lambda e: e.tensor_tensor(out=bb[:, :Ls], in0=tt[:, :Ls], in1=ii[:, :Ls], op=ALU.mult), r=["tt", "ii"], w=["bb"])
                P.op("dve", lambda e: e.tensor_tensor(out=bb[:, :Ls], in0=bb[:, :Ls], in1=xcv[:, :Ls], op=ALU.mult), r=["bb", "xcv"], w=["bb"])
                if d == 0:
                    init = 0.0 if si == 0 else HF[:, t0 - 1:t0]
                    P.op("dve", lambda e: e.tensor_tensor_scan(out=HF[:, t0:t0 + Ls], data0=aa[:, :Ls], data1=bb[:, :Ls], initial=init, op0=ALU.mult, op1=ALU.add),
                         r=["aa", "bb", "HF"], w=["HF"])
                else:
                    init = 0.0 if si == 0 else carry[:, 0:1]
                    P.op("dve", lambda e: e.tensor_tensor_scan(out=hb[:, slice(Ls - 1, None, -1)], data0=aa[:, slice(Ls - 1, None, -1)], data1=bb[:, slice(Ls - 1, None, -1)],
                                                               initial=init, op0=ALU.mult, op1=ALU.add),
                         r=["aa", "bb", "carry"], w=["hb"])
                    P.op("dve", lambda e: e.tensor_copy(out=carry[:], in_=hb[:, 0:1]), r=["hb"], w=["carry"])
                    P.op("dve", lambda e: e.tensor_tensor(out=hb[:, :Ls], in0=hb[:, :Ls], in1=HF[:, t0:t0 + Ls], op=ALU.add), r=["hb", "HF"], w=["hb"])
                    yb_ = yo[si % 2]
                    P.op("dve", lambda e: e.tensor_tensor(out=yb_[:, :Ls], in0=hb[:, :Ls], in1=GG[:, t0:t0 + Ls], op=ALU.mult), r=["hb", "GG"], w=["yo%d" % (si % 2)])
                    P.dma("sp", yaT[:, t0:t0 + Ls], yb_[:, :Ls], r=["yo%d" % (si % 2)], w=["out:yaT%d" % t0])
    if stop < 2:
        return P
    emit_gla(P, PS, Wb, WK, hTd, wg, bg, gng, trid, strd, yb)
    return P


def emit_gla(P, PS, Wb, WK, hTd, wg, bg, gng, trid, strd, yb):
    with P.phase():
        hTt = [P.sb("hTg%d" % b, [128, 16, 128], BF16) for b in range(2)]
        OF = P.sb("OF", [128, NCH, 128])
        tri = P.sb("tri", [128, 2, 128]); stri = P.sb("stri", [128, 2, 128])
        for d in range(2):
            P.dma("sp", tri[:, d, :], trid[d], w=["tri%d" % d]); P.dma("sp", stri[:, d, :], strd[d], w=["stri%d" % d])
        wgt = P.sb("wgt", [16, 2, 64]); bgb = P.sb("bgb", [128, 2, 64]); gngb = P.sb("gngb", [128, 128])
        for d in range(2):
            P.dma("sp", wgt[:, d, :], wg[d], w=["wgt%d" % d])
            P.dma("sp", bgb[:, d, :], bg[d].partition_broadcast(128), w=["bgb%d" % d])
        P.dma("sp", gngb[:], gng.partition_broadcast(128), w=["gngb"])
        S = P.sb("S", [64, 128]); Sb = P.sb("Sb", [64, 128], BF16)
        adT = P.sb("adT", [16, 128]); xg = P.sb("xg", [128, 64]); lg = P.sb("lg", [128, 64])
        E1 = P.sb("E1", [64, 128]); E2 = P.sb("E2", [64, 128]); E3 = P.sb("E3", [128, 64])
        qd = P.sb("qd", [64, 128], BF16); ki = P.sb("ki", [64, 128], BF16); ke = P.sb("ke", [128, 64], BF16)
        vb = P.sb("vb", [128, 128], BF16); sog = P.sb("sog", [128, 128]); scm = P.sb("scm", [128, 128], BF16)
        ot = P.sb("ot", [128, 128]); oj = P.sb("oj", [128, 128]); gst = P.sb("gst", [128, 4]); ybt = [P.sb("ybt%d" % b, [128, 128], BF16) for b in range(2)]
        QK, TK, LG, CT, SC, OO, UP = PS[0], PS[1], PS[2], PS[3], PS[4], PS[5], PS[6]

        def loadh(n, slot):
            P.dma("sp", hTt[slot][:], hTd[n], r=["d:hT%d" % n], w=["hTg%d" % slot])
        for d in range(2):
            order = list(range(NCH)) if d == 0 else [1, 0] + list(range(NCH - 1, 1, -1))
            cl = 127 if d == 0 else 0
            P.op("dve", lambda e: e.memset(S[:], 0.0), w=["S"])
            P.op("dve", lambda e: e.memset(Sb[:], 0.0), w=["Sb"])
            loadh(order[0], 0)
            for i, n in enumerate(order):
                b = i % 2
                if i + 1 < len(order):
                    loadh(order[i + 1], 1 - b)
                hk = "hTg%d" % b
                h = hTt[b]
                for (c0, c1, o0, M, key) in ((256, 320, 0, 64, "QKq"), (320, 384, 128, 64, "QKk"), (384 + 16 * d, 400 + 16 * d, 256, 16, "QKa")):
                    for c in range(16):
                        P.op("pe", lambda e, c=c: e.matmul(QK[0:M, o0:o0 + 128], lhsT=Wb[:, c, c0:c1], rhs=h[:, c, :], start=(c == 0), stop=(c == 15)),
                             r=[hk, WK[c // 4]], w=["B_QK"])
                for c in range(16):
                    P.op("pe", lambda e, c=c: e.matmul(TK[:, 0:320], lhsT=h[:, c, :], rhs=Wb[:, c, 416:736], start=(c == 0), stop=(c == 15)),
                         r=[hk, WK[c // 4]], w=["B_TK"])
                P.op("act", lambda e: e.activation(out=adT[:], in_=QK[0:16, 256:384], func=AF.Copy), r=[], w=["B_QK", "adT"])
                P.op("pe", lambda e: e.matmul(LG[:, 0:64], lhsT=adT[:], rhs=wgt[:, d, :], start=True, stop=True), r=["adT", "wgt%d" % d], w=["B_LG"])
                P.op("dve", lambda e: e.tensor_tensor(out=xg[:], in0=LG[:, 0:64], in1=bgb[:, d, :], op=ALU.add), r=["bgb%d" % d], w=["B_LG", "xg"])
                P.op("act", lambda e: e.activation(out=xg[:], in_=xg[:], func=AF.Exp, scale=-1.0), r=["xg"], w=["xg"])
                P.op("act", lambda e: e.activation(out=xg[:], in_=xg[:], func=AF.Ln, bias=1.0), r=["xg"], w=["xg"])
                P.op("dve", lambda e: e.tensor_scalar(out=lg[:], in0=xg[:], scalar1=-1.0 / 16.0, scalar2=None, op0=ALU.mult), r=["xg"], w=["lg"])
                P.op("pe", lambda e: e.matmul(LG[:, 128:192], lhsT=stri[:, d, :], rhs=lg[:], start=True, stop=True), r=["lg", "stri%d" % d], w=["B_LG"])
                P.op("pe", lambda e: e.matmul(CT[0:64, 0:128], lhsT=lg[:], rhs=tri[:, d, :], start=True, stop=True), r=["lg", "tri%d" % d], w=["B_CT"])
                P.op("act", lambda e: e.activation(out=E1[:], in_=CT[0:64, 0:128], func=AF.Exp), r=[], w=["B_CT", "E1"])
                P.op("act", lambda e: e.activation(out=E2[:], in_=CT[0:64, 0:128], func=AF.Exp, scale=-1.0), r=[], w=["B_CT", "E2"])
                P.op("act", lambda e: e.activation(out=E3[:], in_=LG[:, 128:192], func=AF.Exp), r=[], w=["B_LG", "E3"])
                P.op("dve", lambda e: e.scalar_tensor_tensor(out=qd[:], in0=E1[:], scalar=0.125, in1=QK[0:64, 0:128], op0=ALU.mult, op1=ALU.mult), r=["E1"], w=["B_QK", "qd"])
                P.op("dve", lambda e: e.tensor_tensor(out=ki[:], in0=E2[:], in1=QK[0:64, 128:256], op=ALU.mult), r=["E2"], w=["B_QK", "ki"])
                P.op("dve", lambda e: e.tensor_tensor(out=ke[:], in0=E3[:], in1=TK[:, 0:64], op=ALU.mult), r=["E3"], w=["B_TK", "ke"])
                P.op("act", lambda e: e.activation(out=vb[:], in_=TK[:, 64:192], func=AF.Copy), r=[], w=["B_TK", "vb"])
                if d == 1:
                    P.op("act", lambda e: e.activation(out=sog[:], in_=TK[:, 192:320], func=AF.Silu), r=[], w=["B_TK", "sog"])
                P.op("pe", lambda e: e.matmul(SC[:, 0:128], lhsT=ki[:], rhs=qd[:], start=True, stop=True), r=["ki", "qd"], w=["B_SC"])
                P.op("dve", lambda e: e.tensor_tensor(out=scm[:], in0=SC[:, 0:128], in1=tri[:, d, :], op=ALU.mult), r=["tri%d" % d], w=["B_SC", "scm"])
                P.op("pe", lambda e: e.matmul(OO[:, 0:128], lhsT=scm[:], rhs=vb[:], start=True, stop=False), r=["scm", "vb"], w=["B_OO"])
                P.op("pe", lambda e: e.matmul(OO[:, 0:128], lhsT=qd[:], rhs=Sb[:], start=False, stop=True), r=["qd", "Sb"], w=["B_OO"])
                P.op("pe", lambda e: e.matmul(UP[0:64, 0:128], lhsT=ke[:], rhs=vb[:], start=True, stop=True), r=["ke", "vb"], w=["B_UP"])
                P.op("dve", lambda e: e.scalar_tensor_tensor(out=S[:], in0=S[:], scalar=E1[:, cl:cl + 1], in1=UP[0:64, 0:128], op0=ALU.mult, op1=ALU.add),
                     r=["E1"], w=["B_UP", "S"])
                P.op("pool", lambda e: e.tensor_copy(out=Sb[:], in_=S[:]), r=["S"], w=["Sb"])
                if d == 0:
                    P.op("act", lambda e: e.activation(out=OF[:, n, :], in_=OO[:, 0:128], func=AF.Copy), r=[], w=["B_OO", "OF%d" % n])
                else:
                    P.op("dve", lambda e: e.tensor_tensor(out=ot[:], in0=OO[:, 0:128], in1=OF[:, n, :], op=ALU.add), r=["OF%d" % n], w=["B_OO", "ot"])
                    P.op("act", lambda e: e.activation(out=oj[:], in_=ot[:], func=AF.Square, accum_out=gst[:, 0:1]), r=["ot"], w=["oj", "gst0"])
                    P.op("dve", lambda e: e.tensor_scalar(out=gst[:, 1:2], in0=gst[:, 0:1], scalar1=1.0 / 128, scalar2=EPS, op0=ALU.mult, op1=ALU.add), r=["gst0"], w=["gst1"])
                    P.op("act", lambda e: e.activation(out=gst[:, 2:3], in_=gst[:, 1:2], func=AF.Sqrt), r=["gst1"], w=["gst2"])
                    P.op("dve", lambda e: e.reciprocal(out=gst[:, 3:4], in_=gst[:, 2:3]), r=["gst2"], w=["gst3"])
                    P.op("dve", lambda e: e.scalar_tensor_tensor(out=ot[:], in0=ot[:], scalar=gst[:, 3:4], in1=gngb[:], op0=ALU.mult, op1=ALU.mult), r=["ot", "gst3", "gngb"], w=["ot"])
                    yt = ybt[i % 2]
                    P.op("dve", lambda e: e.tensor_tensor(out=yt[:], in0=ot[:], in1=sog[:], op=ALU.mult), r=["ot", "sog"], w=["ybt%d" % (i % 2)])
                    P.dma("sp", yb[n * 128:(n + 1) * 128, :], yt[:], r=["ybt%d" % (i % 2)], w=["out:yb%d" % n])


def k1_inputs(x, ctx, modv0, norm1_g0, ev_w_in, ev_conv_w, ev_conv_b, lru_wa, lru_ba, lru_wx, lru_bx, lru_lambda, gla_wg_up, gla_bg, gla_norm_g):
    ident, tri, strict = consts_np()
    embr, embc2 = pos_tables()
    xl = np.ascontiguousarray(x[0]); xc = np.ascontiguousarray(ctx[0])
    base = {"xl": xl, "xc": xc, "embr": embr, "embc2": embc2, "gn": feat_pc(norm1_g0),
            "shl": feat_pc(modv0[0, 0:2048]), "scl": feat_pc(modv0[0, 2048:4096]),
            "shc": feat_pc(modv0[1, 0:2048]), "scc": feat_pc(modv0[1, 2048:4096]),
            "ident": ident, "tri": tri, "strict": strict}
    maps = []
    W = ev_w_in[0]
    for j in range(NCORES):
        s128 = slice(128 * j, 128 * j + 128)
        cols = np.concatenate([np.arange(128 * j, 128 * j + 128), 1024 + np.arange(128 * j, 128 * j + 128),
                               2048 + np.arange(64 * j, 64 * j + 64), 2560 + np.arange(64 * j, 64 * j + 64),
                               5120 + np.arange(32),
                               2560 + np.arange(64 * j, 64 * j + 64), 3072 + np.arange(128 * j, 128 * j + 128),
                               4096 + np.arange(128 * j, 128 * j + 128)])
        bdm = np.zeros((4, 128, 128), np.float32)
        for i, (arr, d) in enumerate([(lru_wa[0], 0), (lru_wa[0], 1), (lru_wx[0], 0), (lru_wx[0], 1)]):
            for m in range(2):
                bdm[i, 64 * m:64 * m + 64, 64 * m:64 * m + 64] = arr[d, 2 * j + m]
        m = dict(base)
        m.update({"win": np.ascontiguousarray(W[:, cols]),
                  "convw": np.ascontiguousarray(ev_conv_w[0][:, s128].T), "convb": np.ascontiguousarray(ev_conv_b[0][s128, None]),
                  "bd": bdm,
                  "gb": np.ascontiguousarray(np.stack([lru_ba[0][0, s128], lru_ba[0][1, s128], lru_bx[0][0, s128], lru_bx[0][1, s128]], -1)),
                  "lam": np.ascontiguousarray(lru_lambda[0][:, s128].T),
                  "wg": np.ascontiguousarray(gla_wg_up[0][:, :, 64 * j:64 * j + 64]), "bg": np.ascontiguousarray(gla_bg[0][:, 64 * j:64 * j + 64]),
                  "gng": np.ascontiguousarray(gla_norm_g[0][s128])})
        maps.append(m)
    return maps


def run_k1(maps):
    P = build_k1()
    res = run_prog(P, maps)
    preT = np.concatenate([r["yaT"] for r in res] + [np.ascontiguousarray(r["yb"].T) for r in res], axis=0)
    return preT


def build_post(layer):
    P = Prog()
    NT = 9 if layer == 0 else 8
    KC = 16 if layer == 0 else 32
    xin = P.din("xin", [NT, 128, D])
    yT = P.din("yT", [KC, 128, NT * 128], BF16)
    wout = P.din("wout", [KC * 128, D])
    g1 = P.din("g1", [2, D])
    gn = P.din("gn", [128, 16])
    sc2l = P.din("sc2l", [128, 16]); sh2l = P.din("sh2l", [128, 16])
    sc2c = P.din("sc2c", [128, 16]); sh2c = P.din("sh2c", [128, 16])
    rw = P.din("rw", [128, 16, 32])
    rb = P.din("rb", [32])
    identd = P.din("ident", [128, 128])
    if layer == 0:
        embr = P.din("embr", [16, 1024]); embc2 = P.din("embc2", [128, 1024])
    else:
        ssqp = P.din("ssqp", [NT, 128, 8])
    x1o = P.dout("x1o", [NT, 128, D])
    h2To = P.dout("h2To", [16, 128, NT * 128], BF16)
    gto = P.dout("gto", [NT, 128, 32])

    PS = [P.ps("PS%d" % i, [128, 512]) for i in range(8)]
    ident = P.sb("ident", [128, 128]); P.dma("sp", ident[:], identd[:, :], w=["ident"])
    Wb = P.sb("Wb", [128, KC, D], BF16)
    wv = wout.rearrange("(c p) n -> p c n", p=128)
    for c in range(KC):
        P.dma("pool", Wb[:, c, :], wv[:, c, :], w=["Wb%d" % c])
    Al = emit_AB(P, gn, sc2l, sh2l, "l")
    Ac = emit_AB(P, gn, sc2c, sh2c, "c") if layer == 0 else None
    G1l = P.sb("G1l", [128, D]); P.dma("sp", G1l[:], g1[0].partition_broadcast(128), w=["G1l"])
    if layer == 0:
        G1c = P.sb("G1c", [128, D]); P.dma("sp", G1c[:], g1[1].partition_broadcast(128), w=["G1c"])
        pc = P.sb("pc", [128, 1024]); P.dma("sp", pc[:], embc2[:, :], w=["pc"])
        pr = P.sb("pr", [128, 1024])
    rwt = P.sb("rwt", [128, 16, 32]); P.dma("sp", rwt[:], rw[:, :, :], w=["rwt"])
    rbb = P.sb("rbb", [128, 32]); P.dma("sp", rbb[:], rb.partition_broadcast(128), w=["rbb"])
    xt = P.sb("xt", [128, D]); tmp = P.sb("tmp", [128, D]); junk = P.sb("junk", [128, D], BF16)
    yt = [P.sb("yt%d" % b, [128, KC, 128], BF16) for b in range(2)]
    st = P.sb("st", [128, 8])
    hq = [P.sb("hq%d" % i, [128, 128]) for i in range(4)]
    h2b = P.sb("h2b", [128, 16, 128], BF16)
    sc = P.sb("sc", [128, 32]); sel = P.sb("sel", [128, 32]); sel2 = P.sb("sel2", [128, 32]); eq = P.sb("eq", [128, 32])
    m1 = P.sb("m1", [128, 4]); m2 = P.sb("m2", [128, 4]); gsx = P.sb("gsx", [128, 4]); gmask = P.sb("gmask", [128, 4]); gmax = P.sb("gmax", [128, 4])
    gout = P.sb("gout", [128, 32])

    def loady(i):
        P.dma("sp", yt[i % 2][:], yT[:, :, i * 128:(i + 1) * 128].rearrange("c p t -> p c t"), w=["yt%d" % (i % 2)])
    loady(0)
    for i in range(NT):
        b = i % 2
        if i + 1 < NT:
            loady(i + 1)
        isctx = (layer == 0 and i == 0)
        P.dma("pool", xt[:], xin[i], w=["xt"])
        if layer == 0 and not isctx:
            r0 = 2 * (i - 1)
            P.dma("sp", pr[0:64, :], embr[r0].partition_broadcast(64), w=["pr"], sk="prlo")
            P.dma("sp", pr[64:128, :], embr[r0 + 1].partition_broadcast(64), w=["prh"], sk="prhi")
            P.op("pool", lambda e: e.tensor_tensor(out=xt[:, 0:1024], in0=xt[:, 0:1024], in1=pr[:], op=ALU.add), r=["pr", "prh"], w=["xt"])
            P.op("dve", lambda e: e.tensor_tensor(out=xt[:, 1024:2048], in0=xt[:, 1024:2048], in1=pc[:], op=ALU.add), r=["pc"], w=["xt"])
        if layer == 1:
            ssq = P.sb("ssq%d" % i, [128, 8])
            P.dma("sp", ssq[:], ssqp[i], w=["ssq%d" % i])
            P.op("dve", lambda e: e.tensor_reduce(out=st[:, 4:5], in_=ssq[:], axis=AX.X, op=ALU.add), r=["ssq%d" % i], w=["st4"])
            P.op("dve", lambda e: e.tensor_scalar(out=st[:, 5:6], in0=st[:, 4:5], scalar1=1.0 / 4096, scalar2=EPS, op0=ALU.mult, op1=ALU.add), r=["st4"], w=["st5"])
            P.op("act", lambda e: e.activation(out=st[:, 6:7], in_=st[:, 5:6], func=AF.Sqrt), r=["st5"], w=["st6"])
            P.op("dve", lambda e: e.reciprocal(out=st[:, 7:8], in_=st[:, 6:7]), r=["st6"], w=["st7"])
        for nb in range(4):
            for c in range(KC):
                P.op("pe", lambda e, c=c, nb=nb: e.matmul(PS[nb][:], lhsT=yt[b][:, c, :], rhs=Wb[:, c, nb * 512:(nb + 1) * 512], start=(c == 0), stop=(c == KC - 1)),
                     r=["yt%d" % b, "Wb%d" % c], w=["B%d" % nb])
        G1 = G1c if isctx else G1l
        for nb in range(4):
            sl = slice(nb * 512, (nb + 1) * 512)
            P.op("dve", lambda e, nb=nb, sl=sl: e.tensor_tensor(out=tmp[:, sl], in0=PS[nb][:], in1=G1[:, sl], op=ALU.mult), r=["G1l", "G1c"], w=["B%d" % nb, "tmp"])
            if layer == 1:
                P.op("dve", lambda e, sl=sl: e.scalar_tensor_tensor(out=xt[:, sl], in0=tmp[:, sl], scalar=st[:, 7:8], in1=xt[:, sl], op0=ALU.mult, op1=ALU.add), r=["tmp", "st7"], w=["xt"])
            else:
                P.op("pool", lambda e, sl=sl: e.tensor_tensor(out=xt[:, sl], in0=tmp[:, sl], in1=xt[:, sl], op=ALU.add), r=["tmp"], w=["xt"])
        P.dma("sp", x1o[i], xt[:], r=["xt"], w=["out:x1o%d" % i])
        P.op("act", lambda e: e.activation(out=junk[:], in_=xt[:], func=AF.Square, accum_out=st[:, 0:1]), r=["xt"], w=["junk", "st0"])
        P.op("dve", lambda e: e.tensor_scalar(out=st[:, 1:2], in0=st[:, 0:1], scalar1=1.0 / D, scalar2=EPS, op0=ALU.mult, op1=ALU.add), r=["st0"], w=["st1"])
        P.op("act", lambda e: e.activation(out=st[:, 2:3], in_=st[:, 1:2], func=AF.Sqrt), r=["st1"], w=["st2"])
        P.op("dve", lambda e: e.reciprocal(out=st[:, 3:4], in_=st[:, 2:3]), r=["st2"], w=["st3"])
        P.op("pool", lambda e: e.tensor_scalar(out=tmp[:], in0=xt[:], scalar1=st[:, 3:4], scalar2=0.0, op0=ALU.mult, op1=ALU.add), r=["xt", "st3"], w=["tmp"])
        A, Bv, keyA, keyB = (Ac if isctx else Al)
        for k4 in range(4):
            bank = PS[4 + k4]; bk = "B%d" % (4 + k4)
            for q in range(4):
                c = 4 * k4 + q
                P.op("pe", lambda e, c=c, q=q: e.transpose(out=bank[:, q * 128:(q + 1) * 128], in_=tmp[:, c * 128:(c + 1) * 128], identity=ident[:]), r=["tmp", "ident"], w=[bk])
            for q in range(4):
                c = 4 * k4 + q
                P.op("act", lambda e, c=c, q=q: e.activation(out=hq[q][:], in_=bank[:, q * 128:(q + 1) * 128], func=AF.Identity, scale=A[:, c:c + 1], bias=Bv[:, c:c + 1]),
                     r=[keyA, keyB], w=[bk, "hq%d" % q])
                P.op("pe", lambda e, c=c, q=q: e.matmul(PS[0][:, 0:32], lhsT=hq[q][:], rhs=rwt[:, c, :], start=(c == 0), stop=(c == 15)), r=["hq%d" % q, "rwt"], w=["B0"])
                P.op("dve", lambda e, c=c, q=q: e.tensor_copy(out=h2b[:, c, :], in_=hq[q][:]), r=["hq%d" % q], w=["h2b"])
        P.dma("sp", h2To[:, :, i * 128:(i + 1) * 128].rearrange("c p t -> p c t"), h2b[:], r=["h2b"], w=["out:h2T%d" % i])
        P.op("act", lambda e: e.activation(out=sc[:], in_=PS[0][:, 0:32], func=AF.Sigmoid), r=[], w=["B0", "sc"])
        P.op("dve", lambda e: e.tensor_tensor(out=sel[:], in0=sc[:], in1=rbb[:], op=ALU.add), r=["sc", "rbb"], w=["sel"])
        sel3 = sel[:].rearrange("p (g k) -> p g k", g=4); sel23 = sel2[:].rearrange("p (g k) -> p g k", g=4); eq3 = eq[:].rearrange("p (g k) -> p g k", g=4)
        P.op("dve", lambda e: e.tensor_reduce(out=m1[:], in_=sel3, axis=AX.X, op=ALU.max), r=["sel"], w=["m1"])
        P.op("dve", lambda e: e.tensor_tensor(out=eq3, in0=sel3, in1=m1[:].unsqueeze(2).to_broadcast([128, 4, 8]), op=ALU.is_equal), r=["sel", "m1"], w=["eq"])
        P.op("dve", lambda e: e.scalar_tensor_tensor(out=sel2[:], in0=eq[:], scalar=-1.0e9, in1=sel[:], op0=ALU.mult, op1=ALU.add), r=["eq", "sel"], w=["sel2"])
        P.op("dve", lambda e: e.tensor_reduce(out=m2[:], in_=sel23, axis=AX.X, op=ALU.max), r=["sel2"], w=["m2"])
        P.op("dve", lambda e: e.tensor_tensor(out=gsx[:], in0=m1[:], in1=m2[:], op=ALU.add), r=["m1", "m2"], w=["gsx"])
        P.op("dve", lambda e: e.tensor_reduce(out=gmax[:, 0:1], in_=gsx[:], axis=AX.X, op=ALU.max), r=["gsx"], w=["gmax"])
        P.op("dve", lambda e: e.tensor_scalar(out=gmask[:], in0=gsx[:], scalar1=gmax[:, 0:1], scalar2=None, op0=ALU.is_equal), r=["gsx", "gmax"], w=["gmask"])
        P.op("dve", lambda e: e.tensor_tensor(out=eq3, in0=sel3, in1=m2[:].unsqueeze(2).to_broadcast([128, 4, 8]), op=ALU.is_ge), r=["sel", "m2"], w=["eq"])
        P.op("dve", lambda e: e.tensor_tensor(out=eq3, in0=eq3, in1=gmask[:].unsqueeze(2).to_broadcast([128, 4, 8]), op=ALU.mult), r=["eq", "gmask"], w=["eq"])
        P.op("dve", lambda e: e.tensor_tensor(out=sel2[:], in0=eq[:], in1=sc[:], op=ALU.mult), r=["eq", "sc"], w=["sel2"])
        P.op("dve", lambda e: e.tensor_reduce(out=gmax[:, 1:2], in_=sel2[:], axis=AX.X, op=ALU.add), r=["sel2"], w=["gmax1"])
        P.op("dve", lambda e: e.reciprocal(out=gmax[:, 2:3], in_=gmax[:, 1:2]), r=["gmax1"], w=["gmax2"])
        P.op("dve", lambda e: e.tensor_scalar(out=gout[:], in0=sel2[:], scalar1=gmax[:, 2:3], scalar2=None, op0=ALU.mult), r=["sel2", "gmax2"], w=["gout"])
        P.dma("sp", gto[i], gout[:], r=["gout"], w=["out:gt%d" % i])
    return P


def tok_tiles(arr_c, arr_l, j, layer):
    F = arr_l.shape[-1]
    lat = arr_l[1024 * j:1024 * (j + 1)].reshape(8, 128, F)
    if layer == 0:
        t0 = np.zeros((1, 128, F), arr_l.dtype)
        t0[0, :32] = arr_c[32 * j:32 * (j + 1)]
        return np.ascontiguousarray(np.concatenate([t0, lat], 0))
    return np.ascontiguousarray(lat)


def featT_tiles(aT, j, layer):
    lat = aT[:, :, 256 + 1024 * j:256 + 1024 * (j + 1)]
    if layer == 0:
        t0 = np.zeros(aT.shape[:2] + (128,), aT.dtype)
        t0[:, :, :32] = aT[:, :, 32 * j:32 * (j + 1)]
        return np.ascontiguousarray(np.concatenate([t0, lat], -1))
    return np.ascontiguousarray(lat)


def untile(tiles_per_core, layer):
    if layer == 0:
        c = np.concatenate([t[0, :32] for t in tiles_per_core], 0)
        l = np.concatenate([t[1:].reshape(1024, -1) for t in tiles_per_core], 0)
        return c, l
    return None, np.concatenate([t.reshape(1024, -1) for t in tiles_per_core], 0)


def run_post(layer, x_c, x_l, preT, wout, modv_l, norm2_g, router_w, router_b, ssq_parts=None):
    P = build_post(layer)
    ident, _, _ = consts_np()
    embr, embc2 = pos_tables()
    KC = preT.shape[0] // 128
    aT = preT.reshape(KC, 128, T_ALL)
    base = {"wout": np.ascontiguousarray(wout, np.float32),
            "g1": np.ascontiguousarray(np.stack([modv_l[0, 4096:6144], modv_l[1, 4096:6144]])),
            "gn": feat_pc(norm2_g), "sh2l": feat_pc(modv_l[0, 6144:8192]), "sc2l": feat_pc(modv_l[0, 8192:10240]),
            "sh2c": feat_pc(modv_l[1, 6144:8192]), "sc2c": feat_pc(modv_l[1, 8192:10240]),
            "rw": np.ascontiguousarray(np.asarray(router_w, np.float32).reshape(16, 128, 32).transpose(1, 0, 2)),
            "rb": np.asarray(router_b, np.float32), "ident": ident}
    maps = []
    for j in range(NCORES):
        m = dict(base)
        m["xin"] = tok_tiles(x_c, x_l, j, layer)
        m["yT"] = featT_tiles(aT, j, layer)
        if layer == 0:
            m["embr"] = np.ascontiguousarray(embr[16 * j:16 * j + 16]); m["embc2"] = embc2
        else:
            m["ssqp"] = np.ascontiguousarray(ssq_parts[1024 * j:1024 * (j + 1)].reshape(8, 128, 8))
        maps.append(m)
    res = run_prog(P, maps)
    x1c, x1l = untile([r["x1o"] for r in res], layer)
    gc, gl = untile([r["gto"] for r in res], layer)
    if layer == 0:
        h2T = np.concatenate([r["h2To"][:, :, :32] for r in res] + [r["h2To"][:, :, 128:] for r in res], -1)
        gates = np.concatenate([gc, gl], 0)
    else:
        h2T = np.concatenate([r["h2To"] for r in res], -1)
        gates = gl
    return x1c, x1l, np.ascontiguousarray(h2T), gates


def build_moe(T):
    P = Prog()
    NT = T // 128
    FE = 1024
    h2T = P.din("h2T", [16, 128, T], BF16)
    gt = P.din("gt", [128, NT, 4])
    wg = P.din("wg", [4, D, FE]); wu = P.din("wu", [4, D, FE]); wd = P.din("wd", [4, FE, D])
    part = P.dout("part", [T, D])
    ye = [P.dscr("ye%d" % q, [T, D]) for q in range(4)]
    PS = [P.ps("PS%d" % i, [128, 512]) for i in range(8)]
    gtt = P.sb("gtt", [128, NT, 4]); P.dma("sp", gtt[:], gt[:, :, :], w=["gtt"])
    groups = [(t0, min(512, T - t0)) for t0 in range(0, T, 512)]
    with P.phase():
        Wg = P.sb("Wg", [128, 16, FE], BF16); Wu = P.sb("Wu", [128, 16, FE], BF16); Wd = P.sb("Wd", [128, 8, D], BF16)
        hg = [P.sb("hg%d" % b, [128, 16, 512], BF16) for b in range(2)]
        AT = P.sb("AT", [128, 8, 512], BF16)
        sgt = [P.sb("sgt%d" % b, [128, 512], BF16) for b in range(2)]
        Yt = [P.sb("Yt%d" % b, [128, D]) for b in range(2)]
        h2v = h2T.rearrange("c p t -> p c t")
        ycount = 0
        for e_ in range(4):
            wgv = wg[e_].rearrange("(c p) n -> p c n", p=128); wuv = wu[e_].rearrange("(c p) n -> p c n", p=128)
            wdv = wd[e_].rearrange("(c p) n -> p c n", p=128)
            for c in range(16):
                P.dma("pool", Wg[:, c, :], wgv[:, c, :], w=["Wg%d" % c])
                P.dma("pool", Wu[:, c, :], wuv[:, c, :], w=["Wu%d" % c])
            for c in range(8):
                P.dma("pool", Wd[:, c, :], wdv[:, c, :], w=["Wd%d" % c])

            def loadh(gi):
                t0, Lg = groups[gi]
                P.dma("sp", hg[gi % 2][:, :, :Lg], h2v[:, :, t0:t0 + Lg], w=["hg%d" % (gi % 2)])
            loadh(0)
            for gi, (t0, Lg) in enumerate(groups):
                b = gi % 2
                if gi + 1 < len(groups):
                    loadh(gi + 1)
                for fc in range(8):
                    pb = fc % 2
                    Gb, Ub = PS[2 * pb], PS[2 * pb + 1]
                    for c in range(16):
                        P.op("pe", lambda e, c=c: e.matmul(Gb[:, :Lg], lhsT=Wg[:, c, fc * 128:(fc + 1) * 128], rhs=hg[b][:, c, :Lg], start=(c == 0), stop=(c == 15)),
                             r=["hg%d" % b, "Wg%d" % c], w=["B%d" % (2 * pb)])
                    for c in range(16):
                        P.op("pe", lambda e, c=c: e.matmul(Ub[:, :Lg], lhsT=Wu[:, c, fc * 128:(fc + 1) * 128], rhs=hg[b][:, c, :Lg], start=(c == 0), stop=(c == 15)),
                             r=["hg%d" % b, "Wu%d" % c], w=["B%d" % (2 * pb + 1)])
                    P.op("act", lambda e: e.activation(out=sgt[pb][:, :Lg], in_=Gb[:, :Lg], func=AF.Silu), r=[], w=["B%d" % (2 * pb), "sgt%d" % pb])
                    P.op("dve", lambda e: e.tensor_tensor(out=AT[:, fc, :Lg], in0=Ub[:, :Lg], in1=sgt[pb][:, :Lg], op=ALU.mult), r=["sgt%d" % pb], w=["B%d" % (2 * pb + 1), "AT%d" % fc])
                for tt in range(Lg // 128):
                    tile = (t0 // 128) + tt
                    yb_ = Yt[ycount % 2]; yk = "Yt%d" % (ycount % 2); ycount += 1
                    for dmb in range(4):
                        bank = PS[4 + dmb]; bk = "B%d" % (4 + dmb)
                        for fc in range(8):
                            P.op("pe", lambda e, fc=fc: e.matmul(bank[:], lhsT=AT[:, fc, tt * 128:(tt + 1) * 128], rhs=Wd[:, fc, dmb * 512:(dmb + 1) * 512], start=(fc == 0), stop=(fc == 7)),
                                 r=["AT%d" % fc, "Wd%d" % fc], w=[bk])
                        sl = slice(dmb * 512, (dmb + 1) * 512)
                        if dmb % 2 == 0:
                            P.op("act", lambda e: e.activation(out=yb_[:, sl], in_=bank[:], func=AF.Copy, scale=gtt[:, tile, e_:e_ + 1]), r=["gtt"], w=[bk, yk + "_%d" % dmb])
                        else:
                            P.op("dve", lambda e: e.tensor_scalar(out=yb_[:, sl], in0=bank[:], scalar1=gtt[:, tile, e_:e_ + 1], scalar2=None, op0=ALU.mult), r=["gtt"], w=[bk, yk + "_%d" % dmb])
                    P.dma("sp", ye[e_][tile * 128:(tile + 1) * 128, :], yb_[:], r=[yk + "_%d" % q for q in range(4)], w=["d:ye%d_%d" % (e_, tile)], sk=yk)
    with P.phase():
        yin = [[P.sb("yin%d_%d" % (b, e_), [128, D]) for e_ in range(4)] for b in range(2)]

        def loady(tile):
            b = tile % 2
            for e_ in range(4):
                P.dma("sp" if e_ % 2 == 0 else "pool", yin[b][e_][:], ye[e_][tile * 128:(tile + 1) * 128, :], r=["d:ye%d_%d" % (e_, tile)], w=["yin%d_%d" % (b, e_)])
        loady(0)
        for tile in range(NT):
            b = tile % 2
            if tile + 1 < NT:
                loady(tile + 1)
            P.op("dve", lambda e: e.tensor_tensor(out=yin[b][0][:], in0=yin[b][0][:], in1=yin[b][1][:], op=ALU.add), r=["yin%d_1" % b], w=["yin%d_0" % b])
            P.op("pool", lambda e: e.tensor_tensor(out=yin[b][2][:], in0=yin[b][2][:], in1=yin[b][3][:], op=ALU.add), r=["yin%d_3" % b], w=["yin%d_2" % b])
            P.op("dve", lambda e: e.tensor_tensor(out=yin[b][0][:], in0=yin[b][0][:], in1=yin[b][2][:], op=ALU.add), r=["yin%d_2" % b], w=["yin%d_0" % b])
            P.dma("sp", part[tile * 128:(tile + 1) * 128, :], yin[b][0][:], r=["yin%d_0" % b], w=["out:part%d" % tile])
    return P


def run_moe(h2T, gates, wgate, wup, wdown):
    T = h2T.shape[-1]
    P = build_moe(T)
    maps = []
    for j in range(NCORES):
        g = np.ascontiguousarray(gates[:, 4 * j:4 * j + 4].reshape(T // 128, 128, 4).transpose(1, 0, 2))
        maps.append({"h2T": h2T, "gt": g, "wg": np.ascontiguousarray(wgate[4 * j:4 * j + 4]), "wu": np.ascontiguousarray(wup[4 * j:4 * j + 4]),
                     "wd": np.ascontiguousarray(wdown[4 * j:4 * j + 4])})
    res = run_prog(P, maps)
    return [r["part"] for r in res]


def build_combine(layer):
    P = Prog()
    NT = 9 if layer == 0 else 8
    x1 = P.din("x1", [NT, 128, D])
    parts = P.din("parts", [8, NT, 128, D])
    g2 = P.din("g2", [2, D])
    xo = P.dout("xo", [NT, 128, D])
    G2l = P.sb("G2l", [128, D]); P.dma("sp", G2l[:], g2[0].partition_broadcast(128), w=["G2l"])
    if layer == 0:
        G2c = P.sb("G2c", [128, D]); P.dma("sp", G2c[:], g2[1].partition_broadcast(128), w=["G2c"])
    else:
        fg = P.din("fg", [D])
        FG = P.sb("FG", [128, D]); P.dma("sp", FG[:], fg.partition_broadcast(128), w=["FG"])
        junk = P.sb("junk", [128, D], BF16); st = P.sb("st", [128, 4])
    pb = [P.sb("pb%d" % q, [128, D]) for q in range(8)]
    xt = [P.sb("xt%d" % b, [128, D]) for b in range(2)]
    for i in range(NT):
        b = i % 2
        P.dma("sp", xt[b][:], x1[i], w=["xt%d" % b])
        for q in range(8):
            P.dma("sp" if q % 2 == 0 else "pool", pb[q][:], parts[q, i], w=["pb%d" % q])
        for (a, c, eng) in ((0, 1, "dve"), (2, 3, "pool"), (4, 5, "dve"), (6, 7, "pool"), (0, 2, "dve"), (4, 6, "pool"), (0, 4, "dve")):
            P.op(eng, lambda e, a=a, c=c: e.tensor_tensor(out=pb[a][:], in0=pb[a][:], in1=pb[c][:], op=ALU.add), r=["pb%d" % c], w=["pb%d" % a])
        G2 = G2c if (layer == 0 and i == 0) else G2l
        P.op("dve", lambda e: e.tensor_tensor(out=pb[0][:], in0=pb[0][:], in1=G2[:], op=ALU.mult), r=["G2l", "G2c"], w=["pb0"])
        P.op("pool", lambda e: e.tensor_tensor(out=xt[b][:], in0=xt[b][:], in1=pb[0][:], op=ALU.add), r=["pb0"], w=["xt%d" % b])
        if layer == 1:
            P.op("act", lambda e: e.activation(out=junk[:], in_=xt[b][:], func=AF.Square, accum_out=st[:, 0:1]), r=["xt%d" % b], w=["junk", "st0"])
            P.op("dve", lambda e: e.tensor_scalar(out=st[:, 1:2], in0=st[:, 0:1], scalar1=1.0 / D, scalar2=EPS, op0=ALU.mult, op1=ALU.add), r=["st0"], w=["st1"])
            P.op("act", lambda e: e.activation(out=st[:, 2:3], in_=st[:, 1:2], func=AF.Sqrt), r=["st1"], w=["st2"])
            P.op("dve", lambda e: e.reciprocal(out=st[:, 3:4], in_=st[:, 2:3]), r=["st2"], w=["st3"])
            P.op("dve", lambda e: e.scalar_tensor_tensor(out=xt[b][:], in0=xt[b][:], scalar=st[:, 3:4], in1=FG[:], op0=ALU.mult, op1=ALU.mult), r=["st3", "FG"], w=["xt%d" % b])
        P.dma("sp", xo[i], xt[b][:], r=["xt%d" % b], w=["out:xo%d" % i])
    return P


def run_combine(layer, x1c, x1l, parts, modv_l, final_g=None):
    P = build_combine(layer)
    maps = []
    g2 = np.ascontiguousarray(np.stack([modv_l[0, 10240:12288], modv_l[1, 10240:12288]]))
    for j in range(NCORES):
        if layer == 0:
            pj = np.stack([tok_tiles(p[:256], p[256:], j, 0) for p in parts])
        else:
            pj = np.stack([tok_tiles(None, p, j, 1) for p in parts])
        m = {"x1": tok_tiles(x1c, x1l, j, layer), "parts": np.ascontiguousarray(pj), "g2": g2}
        if layer == 1:
            m["fg"] = np.asarray(final_g, np.float32)
        maps.append(m)
    res = run_prog(P, maps)
    return untile([r["xo"] for r in res], layer)


K5_COLS = 1296
XW = 8454


def build_k5():
    P = Prog()
    xl = P.din("xl", [SEQ, D]); xc = P.din("xc", [CTX, D])
    gn = P.din("gn", [128, 16])
    scl = P.din("scl", [128, 16]); shl = P.din("shl", [128, 16]); scc = P.din("scc", [128, 16]); shc = P.din("shc", [128, 16])
    win = P.din("win", [D, K5_COLS])
    convw = P.din("convw", [128, 6, 4]); convb = P.din("convb", [128, 6])
    alog = P.din("alog", [16]); dtb = P.din("dtb", [16]); dsk = P.din("dsk", [8]); ng = P.din("ng", [512])
    identd = P.din("ident", [128, 128]); trid = P.din("tri", [2, 128, 128]); strd = P.din("strict", [2, 128, 128])
    seld = P.din("sel", [8, 1024])
    yzo = P.dout("yzo", [SEQ, 512], BF16)
    ssqo = P.dout("ssqo", [SEQ, 1])
    hTd = P.dscr("hTd", [128, 16, XW], BF16)
    YF = P.dscr("YF", [SEQ, 512])

    PS = [P.ps("PS%d" % i, [128, 512]) for i in range(8)]
    ident = P.sb("ident", [128, 128]); P.dma("sp", ident[:], identd[:, :], w=["ident"])
    Wb = P.sb("Wb", [128, 16, K5_COLS], BF16)
    winv = win.rearrange("(c p) n -> p c n", p=128)
    for c in range(16):
        P.dma("pool", Wb[:, c, :], winv[:, c, :], w=["Wb%d" % c])
    with P.phase():
        zt = P.sb("zt", [128, 16, 4], BF16)
        P.op("dve", lambda e: e.memset(zt[:], 0.0), w=["zt"])
        for (a, b_) in ((0, 2), (258, 261), (8453, 8454)):
            P.dma("sp", hTd[:, :, a:b_], zt[:, :, 0:b_ - a], r=["zt"], w=["d:pad%d" % a], sk="zt", allow_slow_non_contiguous=True)
        Al = emit_AB(P, gn, scl, shl, "l")
        Ac = emit_AB(P, gn, scc, shc, "c")
        emit_hT(P, PS, NCH,
                src_fn=lambda n: (xc[n * 128:(n + 1) * 128, :] if n < 2 else xl[(n - 2) * 128:(n - 1) * 128, :]),
                pos_fn=lambda n: None, ab_fn=lambda n: (Ac if n < 2 else Al), embr=None, embc2_d=None, ident=ident,
                dst_fn=lambda n: hTd[:, :, xa_col(128 * n):xa_col(128 * n) + 128], dst_key_fn=lambda n: "d:hT%d" % n)
    with P.phase():
        hw = [P.sb("hw%d" % b, [128, 16, 131], BF16) for b in range(2)]
        tri = P.sb("tri", [128, 2, 128]); stri = P.sb("stri", [128, 2, 128]); ones = P.sb("ones", [128, 128])
        for d in range(2):
            P.dma("sp", tri[:, d, :], trid[d], w=["tri%d" % d]); P.dma("sp", stri[:, d, :], strd[d], w=["stri%d" % d])
        P.op("dve", lambda e: e.memset(ones[:], 1.0), w=["ones"])
        SEL = P.sb("SEL", [8, 1024]); P.dma("sp", SEL[:], seld[:, :], w=["SEL"])
        cw = P.sb("cw", [128, 6, 4]); cbv = P.sb("cbv", [128, 6])
        P.dma("sp", cw[:], convw[:, :, :], w=["cw"]); P.dma("sp", cbv[:], convb[:, :], w=["cbv"])
        aneg = P.sb("aneg", [128, 16]); dtbb = P.sb("dtbb", [128, 16]); dskb = P.sb("dskb", [128, 8]); ngb = P.sb("ngb", [128, 512])
        P.dma("sp", aneg[:], alog.partition_broadcast(128), w=["aneg"]); P.dma("sp", dtbb[:], dtb.partition_broadcast(128), w=["dtbb"])
        P.dma("sp", dskb[:], dsk.partition_broadcast(128), w=["dskb"]); P.dma("sp", ngb[:], ng.partition_broadcast(128), w=["ngb"])
        P.op("act", lambda e: e.activation(out=aneg[:], in_=aneg[:], func=AF.Exp), r=[], w=["aneg"])
        P.op("dve", lambda e: e.tensor_scalar(out=aneg[:], in0=aneg[:], scalar1=-1.0, scalar2=None, op0=ALU.mult), r=[], w=["aneg"])
        X = P.sb("X", [128, 786]); U = P.sb("U", [128, 6, 128]); BCb = P.sb("BCb", [128, 2, 128], BF16)
        Btk = P.sb("Btk", [128, 128], BF16); xst = P.sb("xst", [128, 512]); sz = P.sb("sz", [128, 512])
        dtp = P.sb("dtp", [128, 8]); la = P.sb("la", [128, 8]); ecum = P.sb("ecum", [128, 8]); erest = P.sb("erest", [128, 8]); dec = P.sb("dec", [128, 8])
        ncT = P.sb("ncT", [8, 128]); cT = P.sb("cT", [8, 128]); BDc = P.sb("BDc", [8, 1024]); cbm = P.sb("cbm", [128, 128])
        dmin = P.sb("dmin", [128, 1024]); M = P.sb("M", [128, 8, 128], BF16)
        xdt32 = P.sb("xdt32", [128, 512]); xdtb = P.sb("xdtb", [128, 512], BF16); xdte = P.sb("xdte", [128, 512], BF16)
        yt = P.sb("yt", [128, 512]); yf = [P.sb("yf%d" % b, [128, 512]) for b in range(2)]; tmpd = P.sb("tmpd", [128, 512])
        ST = P.sb("ST", [128, 512]); STb = P.sb("STb", [128, 512], BF16)
        yo = [P.sb("yo%d" % b, [128, 512], BF16) for b in range(2)]; sq = [P.sb("sq%d" % b, [128, 2]) for b in range(2)]; junk = P.sb("junk", [128, 512], BF16)

        def v3(t, h=8):
            return t[:].rearrange("p (h q) -> p h q", h=h)

        def loadw(n, slot):
            c0 = xa_col(128 * n) - 2
            P.dma("sp", hw[slot][:], hTd[:, :, c0:c0 + 131], r=["d:hT%d" % n], w=["hw%d" % slot])
        for d in range(2):
            order = list(range(NCH)) if d == 0 else [1, 0] + list(range(NCH - 1, 1, -1))
            P.op("dve", lambda e: e.memset(ST[:], 0.0), w=["ST"])
            P.op("dve", lambda e: e.memset(STb[:], 0.0), w=["STb"])
            loadw(order[0], 0)
            for i, n in enumerate(order):
                b = i % 2
                if i + 1 < len(order):
                    loadw(order[i + 1], 1 - b)
                h = hw[b]; hk = "hw%d" % b
                lat = n >= 2
                row0 = (n - 2) * 128
                if d == 1 and lat:
                    P.dma("pool", yf[b][:], YF[row0:row0 + 128, :], r=["d:YF%d" % n], w=["yf%d" % b])
                for k in range(6):
                    bank = PS[k // 3]; o0 = (k % 3) * 131
                    for c in range(16):
                        P.op("pe", lambda e, c=c: e.matmul(bank[:, o0:o0 + 131], lhsT=Wb[:, c, k * 128:(k + 1) * 128], rhs=h[:, c, :], start=(c == 0), stop=(c == 15)),
                             r=[hk, "Wb%d" % c], w=["B%d" % (k // 3)])
                for c in range(16):
                    P.op("pe", lambda e, c=c: e.matmul(PS[3][:, 0:16], lhsT=h[:, c, 2:130], rhs=Wb[:, c, 1280:1296], start=(c == 0), stop=(c == 15)), r=[hk, "Wb%d" % c], w=["B3"])
                if d == 1 and lat:
                    for c in range(16):
                        P.op("pe", lambda e, c=c: e.matmul(PS[2][:, 0:512], lhsT=h[:, c, 2:130], rhs=Wb[:, c, 768:1280], start=(c == 0), stop=(c == 15)), r=[hk, "Wb%d" % c], w=["B2"])
                    P.op("act", lambda e: e.activation(out=sz[:], in_=PS[2][:, 0:512], func=AF.Silu), r=[], w=["B2", "sz"])
                P.op("act", lambda e: e.activation(out=X[:, 0:393], in_=PS[0][:, 0:393], func=AF.Copy), r=[], w=["B0", "X0"])
                P.op("act", lambda e: e.activation(out=X[:, 393:786], in_=PS[1][:, 0:393], func=AF.Copy), r=[], w=["B1", "X1"])
                for k in range(6):
                    xk = "X%d" % (k // 3); x0 = k * 131
                    P.op("dve", lambda e: e.tensor_scalar(out=U[:, k, :], in0=X[:, x0:x0 + 128], scalar1=cw[:, k, 0:1], scalar2=cbv[:, k:k + 1], op0=ALU.mult, op1=ALU.add),
                         r=[xk, "cw", "cbv"], w=["U"])
                    for t_ in range(1, 4):
                        P.op("dve", lambda e, t_=t_: e.scalar_tensor_tensor(out=U[:, k, :], in0=X[:, x0 + t_:x0 + t_ + 128], scalar=cw[:, k, t_:t_ + 1], in1=U[:, k, :], op0=ALU.mult, op1=ALU.add),
                             r=[xk, "cw"], w=["U"])
                P.op("act", lambda e: e.activation(out=U[:], in_=U[:], func=AF.Silu), r=[], w=["U"])
                for k in range(4):
                    P.op("pe", lambda e, k=k: e.transpose(out=PS[4][:, k * 128:(k + 1) * 128], in_=U[:, k, :], identity=ident[:]), r=["U", "ident"], w=["B4"])
                P.op("pe", lambda e: e.transpose(out=PS[5][:, 0:128], in_=U[:, 4, :], identity=ident[:]), r=["U", "ident"], w=["B5"])
                P.op("pool", lambda e: e.tensor_copy(out=BCb[:], in_=U[:, 4:6, :]), r=["U"], w=["BCb"])
                P.op("act", lambda e: e.activation(out=xst[:], in_=PS[4][:, 0:512], func=AF.Copy), r=[], w=["B4", "xst"])
                P.op("act", lambda e: e.activation(out=Btk[:], in_=PS[5][:, 0:128], func=AF.Copy), r=[], w=["B5", "Btk"])
                P.op("dve", lambda e: e.tensor_tensor(out=dtp[:], in0=PS[3][:, 8 * d:8 * d + 8], in1=dtbb[:, 8 * d:8 * d + 8], op=ALU.add), r=["dtbb"], w=["B3", "dtp"])
                P.op("act", lambda e: e.activation(out=dtp[:], in_=dtp[:], func=AF.Exp), r=[], w=["dtp"])
                P.op("act", lambda e: e.activation(out=dtp[:], in_=dtp[:], func=AF.Ln, bias=1.0), r=[], w=["dtp"])
                P.op("dve", lambda e: e.tensor_tensor(out=la[:], in0=dtp[:], in1=aneg[:, 8 * d:8 * d + 8], op=ALU.mult), r=["dtp", "aneg"], w=["la"])
                P.op("pe", lambda e: e.matmul(PS[3][:, 16:24], lhsT=tri[:, d, :], rhs=la[:], start=True, stop=True), r=["la", "tri%d" % d], w=["B3"])
                P.op("pe", lambda e: e.matmul(PS[3][:, 24:32], lhsT=stri[:, d, :], rhs=la[:], start=True, stop=True), r=["la", "stri%d" % d], w=["B3"])
                P.op("pe", lambda e: e.matmul(PS[3][:, 32:40], lhsT=ones[:], rhs=la[:], start=True, stop=True), r=["la", "ones"], w=["B3"])
                P.op("pe", lambda e: e.matmul(PS[3][0:8, 64:192], lhsT=la[:], rhs=tri[:, d, :], start=True, stop=True), r=["la", "tri%d" % d], w=["B3"])
                P.op("pe", lambda e: e.matmul(PS[3][:, 192:320], lhsT=BCb[:, 0, :], rhs=BCb[:, 1, :], start=True, stop=True), r=["BCb"], w=["B3"])
                P.op("act", lambda e: e.activation(out=ecum[:], in_=PS[3][:, 16:24], func=AF.Exp), r=[], w=["B3", "ecum"])
                P.op("act", lambda e: e.activation(out=erest[:], in_=PS[3][:, 24:32], func=AF.Exp), r=[], w=["B3", "erest"])
                P.op("act", lambda e: e.activation(out=dec[:], in_=PS[3][:, 32:40], func=AF.Exp), r=[], w=["B3", "dec"])
                P.op("act", lambda e: e.activation(out=cT[:], in_=PS[3][0:8, 64:192], func=AF.Copy), r=[], w=["B3", "cT"])
                P.op("dve", lambda e: e.tensor_scalar(out=ncT[:], in0=cT[:], scalar1=-1.0, scalar2=None, op0=ALU.mult), r=["cT"], w=["ncT"])
                P.op("dve", lambda e: e.tensor_tensor(out=BDc[:].rearrange("p (h c) -> p h c", h=8), in0=SEL[:].rearrange("p (h c) -> p h c", h=8),
                                                      in1=cT[:].unsqueeze(1).to_broadcast([8, 8, 128]), op=ALU.mult), r=["cT", "SEL"], w=["BDc"])
                P.op("dve", lambda e: e.tensor_tensor(out=cbm[:], in0=PS[3][:, 192:320], in1=tri[:, d, :], op=ALU.mult), r=["tri%d" % d], w=["B3", "cbm"])
                for hf in range(2):
                    P.op("pe", lambda e: e.matmul(PS[6 + hf][:, 0:512], lhsT=ncT[:], rhs=SEL[:, hf * 512:(hf + 1) * 512], start=True, stop=False), r=["ncT", "SEL"], w=["B%d" % (6 + hf)])
                    P.op("pe", lambda e: e.matmul(PS[6 + hf][:, 0:512], lhsT=ones[0:8, :], rhs=BDc[:, hf * 512:(hf + 1) * 512], start=False, stop=True), r=["ones", "BDc"], w=["B%d" % (6 + hf)])
                    P.op("dve", lambda e: e.tensor_scalar(out=dmin[:, hf * 512:(hf + 1) * 512], in0=PS[6 + hf][:, 0:512], scalar1=0.0, scalar2=None, op0=ALU.min), r=[], w=["B%d" % (6 + hf), "dmin"])
                P.op("act", lambda e: e.activation(out=dmin[:], in_=dmin[:], func=AF.Exp), r=[], w=["dmin"])
                P.op("dve", lambda e: e.tensor_tensor(out=M[:], in0=dmin[:].rearrange("p (h c) -> p h c", h=8), in1=cbm[:].unsqueeze(1).to_broadcast([128, 8, 128]), op=ALU.mult),
                     r=["dmin", "cbm"], w=["M"])
                P.op("dve", lambda e: e.tensor_tensor(out=v3(xdt32), in0=v3(xst), in1=dtp[:].unsqueeze(2).to_broadcast([128, 8, 64]), op=ALU.mult), r=["xst", "dtp"], w=["xdt32"])
                P.op("pool", lambda e: e.tensor_copy(out=xdtb[:], in_=xdt32[:]), r=["xdt32"], w=["xdtb"])
                P.op("dve", lambda e: e.tensor_tensor(out=v3(xdte), in0=v3(xdt32), in1=erest[:].unsqueeze(2).to_broadcast([128, 8, 64]), op=ALU.mult), r=["xdt32", "erest"], w=["xdte"])
                for hh in range(8):
                    P.op("pe", lambda e, hh=hh: e.matmul(PS[0][:, hh * 64:(hh + 1) * 64], lhsT=M[:, hh, :], rhs=xdtb[:, hh * 64:(hh + 1) * 64], start=True, stop=True), r=["M", "xdtb"], w=["B0"])
                P.op("pe", lambda e: e.matmul(PS[1][:, 0:512], lhsT=BCb[:, 1, :], rhs=STb[:], start=True, stop=True), r=["BCb", "STb"], w=["B1"])
                P.op("pe", lambda e: e.matmul(PS[2][:, 0:512], lhsT=Btk[:], rhs=xdte[:], start=True, stop=True), r=["Btk", "xdte"], w=["B2"])
                P.op("dve", lambda e: e.tensor_tensor(out=v3(yt), in0=PS[1][:, 0:512].rearrange("p (h q) -> p h q", h=8), in1=ecum[:].unsqueeze(2).to_broadcast([128, 8, 64]), op=ALU.mult),
                     r=["ecum"], w=["B1", "yt"])
                P.op("dve", lambda e: e.tensor_tensor(out=yt[:], in0=PS[0][:, 0:512], in1=yt[:], op=ALU.add), r=[], w=["B0", "yt"])
                P.op("dve", lambda e: e.tensor_tensor(out=v3(ST), in0=v3(ST), in1=dec[:].unsqueeze(2).to_broadcast([128, 8, 64]), op=ALU.mult), r=["dec"], w=["ST"])
                P.op("dve", lambda e: e.tensor_tensor(out=ST[:], in0=PS[2][:, 0:512], in1=ST[:], op=ALU.add), r=[], w=["B2", "ST"])
                P.op("pool", lambda e: e.tensor_copy(out=STb[:], in_=ST[:]), r=["ST"], w=["STb"])
                if not lat:
                    continue
                if d == 0:
                    P.dma("sp", YF[row0:row0 + 128, :], yt[:], r=["yt"], w=["d:YF%d" % n])
                else:
                    P.op("pool", lambda e: e.tensor_tensor(out=yt[:], in0=yt[:], in1=yf[b][:], op=ALU.add), r=["yf%d" % b], w=["yt"])
                    P.op("dve", lambda e: e.tensor_tensor(out=v3(tmpd), in0=v3(xst), in1=dskb[:].unsqueeze(2).to_broadcast([128, 8, 64]), op=ALU.mult), r=["xst", "dskb"], w=["tmpd"])
                    P.op("pool", lambda e: e.tensor_tensor(out=yt[:], in0=yt[:], in1=tmpd[:], op=ALU.add), r=["tmpd"], w=["yt"])
                    P.op("dve", lambda e: e.tensor_tensor(out=yt[:], in0=yt[:], in1=sz[:], op=ALU.mult), r=["sz"], w=["yt"])
                    P.op("act", lambda e: e.activation(out=junk[:], in_=yt[:], func=AF.Square, accum_out=sq[b][:, 0:1]), r=["yt"], w=["junk", "sq%d" % b])
                    P.op("dve", lambda e: e.tensor_tensor(out=yo[b][:], in0=yt[:], in1=ngb[:], op=ALU.mult), r=["yt", "ngb"], w=["yo%d" % b])
                    P.dma("sp", yzo[row0:row0 + 128, :], yo[b][:], r=["yo%d" % b], w=["out:yz%d" % n])
                    P.dma("sp", ssqo[row0:row0 + 128, :], sq[b][:, 0:1], r=["sq%d" % b], w=["out:sq%d" % n])
    return P


def k5_inputs(x2c, x2l, modv1, norm1_g1, od_w_in, od_conv_w, od_conv_b, ssd_a_log, ssd_dt_bias, ssd_d, ssd_norm_g):
    ident, tri, strict = consts_np()
    sel = np.zeros((8, 8, 128), np.float32)
    for hh in range(8):
        sel[hh, hh, :] = 1.0
    base = {"xl": np.ascontiguousarray(x2l), "xc": np.ascontiguousarray(x2c), "gn": feat_pc(norm1_g1),
            "shl": feat_pc(modv1[0, 0:2048]), "scl": feat_pc(modv1[0, 2048:4096]),
            "shc": feat_pc(modv1[1, 0:2048]), "scc": feat_pc(modv1[1, 2048:4096]),
            "ident": ident, "tri": tri, "strict": strict, "sel": sel.reshape(8, 1024)}
    W = od_w_in[0]; cwf = od_conv_w[0]; cbf = od_conv_b[0]
    maps = []
    for j in range(NCORES):
        cols = np.concatenate([4096 + 512 * j + np.arange(512), 8192 + 128 * j + np.arange(128), 9216 + 128 * j + np.arange(128),
                               512 * j + np.arange(512), 10240 + 8 * j + np.arange(8), 10304 + 8 * j + np.arange(8)])
        ch = np.stack([512 * j + 128 * k + np.arange(128) for k in range(4)] + [4096 + 128 * j + np.arange(128), 5120 + 128 * j + np.arange(128)], 1)
        m = dict(base)
        m.update({"win": np.ascontiguousarray(W[:, cols]),
                  "convw": np.ascontiguousarray(cwf[:, ch].transpose(1, 2, 0)), "convb": np.ascontiguousarray(cbf[ch]),
                  "alog": np.concatenate([ssd_a_log[0][0, 8 * j:8 * j + 8], ssd_a_log[0][1, 8 * j:8 * j + 8]]).astype(np.float32),
                  "dtb": np.concatenate([ssd_dt_bias[0][0, 8 * j:8 * j + 8], ssd_dt_bias[0][1, 8 * j:8 * j + 8]]).astype(np.float32),
                  "dsk": np.ascontiguousarray(ssd_d[0][8 * j:8 * j + 8]), "ng": np.ascontiguousarray(ssd_norm_g[0][512 * j:512 * j + 512])})
        maps.append(m)
    return maps


def run_k5(maps):
    P = build_k5()
    res = run_prog(P, maps)
    pre1T = np.zeros((4096, T_ALL), NPBF)
    for j, r in enumerate(res):
        pre1T[512 * j:512 * (j + 1), 256:] = r["yzo"].T
    ssq = np.concatenate([r["ssqo"] for r in res], 1)
    return pre1T, np.ascontiguousarray(ssq)


def kernel(x, c, ctx, c_ctx, mod_w, mod_b, norm1_g, norm2_g, ev_w_in, ev_conv_w, ev_conv_b, lru_wa, lru_ba, lru_wx, lru_bx,
           lru_lambda, gla_wg_up, gla_bg, gla_norm_g, ev_w_out, od_w_in, od_conv_w, od_conv_b, ssd_a_log, ssd_dt_bias, ssd_d,
           ssd_norm_g, od_w_out, router_w, router_b, exp_w_gate, exp_w_up, exp_w_down, final_norm_g):
    f = lambda a: np.asarray(a, np.float32)
    x = f(x); ctx = f(ctx)
    modv = run_k0(f(c), f(c_ctx), f(mod_w), f(mod_b))
    maps = k1_inputs(x, ctx, modv[0], f(norm1_g)[0], f(ev_w_in), f(ev_conv_w), f(ev_conv_b), f(lru_wa), f(lru_ba), f(lru_wx), f(lru_bx),
                     f(lru_lambda), f(gla_wg_up), f(gla_bg), f(gla_norm_g))
    preT = run_k1(maps)
    del maps
    x1c, x1l, h2T, gates = run_post(0, ctx[0], x[0], preT, f(ev_w_out)[0], modv[0], f(norm2_g)[0], f(router_w), f(router_b))
    parts = run_moe(h2T, gates, f(exp_w_gate)[0], f(exp_w_up)[0], f(exp_w_down)[0])
    x2c, x2l = run_combine(0, x1c, x1l, parts, modv[0])
    del parts, preT, h2T
    maps = k5_inputs(x2c, x2l, modv[1], f(norm1_g)[1], f(od_w_in), f(od_conv_w), f(od_conv_b), f(ssd_a_log), f(ssd_dt_bias), f(ssd_d), f(ssd_norm_g))
    pre1T, ssq = run_k5(maps)
    del maps
    _, x3l, h2T, gates = run_post(1, None, x2l, pre1T, f(od_w_out)[0], modv[1], f(norm2_g)[1], f(router_w), f(router_b), ssq_parts=ssq)
    parts = run_moe(h2T, gates, f(exp_w_gate)[1], f(exp_w_up)[1], f(exp_w_down)[1])
    _, out = run_combine(1, None, x3l, parts, modv[1], final_g=f(final_norm_g))
    return out.reshape(1, SEQ, D).astype(np.float32)
```

```python
import contextlib
import math
import numpy as np
import ml_dtypes
import concourse.bass as bass
import concourse.mybir as mybir
from concourse.bass_utils import run_bass_kernel_spmd

F32 = mybir.dt.float32
BF16 = mybir.dt.bfloat16
I32 = mybir.dt.int32
AF = mybir.ActivationFunctionType
ALU = mybir.AluOpType
AX = mybir.AxisListType
NPBF = ml_dtypes.bfloat16

NCORES = 8
D = 2048
SEQ = 8192
CTX = 256
T_ALL = SEQ + CTX
NCH = T_ALL // 128
EPS = 1e-6

import os
SAME_ENGINE_SYNC = os.environ.get("SES", "1") == "1"


class Prog:
    def __init__(self):
        self.nc = bass.Bass("TRN2", target_bir_lowering=False)
        nc = self.nc
        self.es = contextlib.ExitStack()
        self.eng = {"pe": nc.tensor, "act": nc.scalar, "dve": nc.vector, "pool": nc.gpsimd, "sp": nc.sync}
        self.esem = {}
        self.ecnt = {}
        self.seen = {}
        for e in self.eng:
            self.esem[e] = self.es.enter_context(nc.semaphore("sem_" + e))
            self.ecnt[e] = 0
            self.seen[e] = {}
        self.lastw = {}
        self.readers = {}
        self.dsem = {}
        self.out_tokens = []
        self.nuniq = 0

    def _uid(self):
        self.nuid = getattr(self, 'nuid', 0) + 1
        return self.nuid

    def din(self, name, shape, dt=F32):
        return self.nc.dram_tensor(name, list(shape), dt, kind="ExternalInput").ap()

    def dout(self, name, shape, dt=F32):
        return self.nc.dram_tensor(name, list(shape), dt, kind="ExternalOutput").ap()

    def dscr(self, name, shape, dt=F32):
        return self.nc.dram_tensor(name, list(shape), dt, kind="Internal").ap()

    def sb(self, name, shape, dt=F32):
        return self.es.enter_context(self.nc.sbuf_tensor("s%d_" % self._uid() + name, list(shape), dt))

    def ps(self, name, shape, dt=F32):
        return self.es.enter_context(self.nc.psum_tensor("p%d_" % self._uid() + name, list(shape), dt))

    def _waits(self, e, r, w):
        toks = []
        for k in r:
            t = self.lastw.get(k)
            if t is not None:
                toks.append(t)
        for k in w:
            t = self.lastw.get(k)
            if t is not None:
                toks.append(t)
            toks.extend(self.readers.get(k, ()))
        eng = self.eng[e]
        best = {}
        for (sem, val, sid, owner) in toks:
            if owner == e and (e == "pe" or not SAME_ENGINE_SYNC):
                continue
            if self.seen[e].get(sid, 0) >= val:
                continue
            if sid not in best or best[sid][1] < val:
                best[sid] = (sem, val)
        for sid, (sem, val) in best.items():
            eng.wait_ge(sem, val)
            self.seen[e][sid] = val

    def _update(self, r, w, tok):
        for k in r:
            self.readers.setdefault(k, []).append(tok)
        for k in w:
            self.lastw[k] = tok
            self.readers[k] = []

    def op(self, e, fn, r=(), w=()):
        self._waits(e, r, w)
        inst = fn(self.eng[e])
        self.ecnt[e] += 1
        inst.then_inc(self.esem[e], 1)
        tok = (self.esem[e], self.ecnt[e], "E" + e, e)
        self._update(r, w, tok)
        return tok

    def dma(self, q, out, in_, r=(), w=(), sk=None, **kw):
        if sk is None:
            sk = (w[0] if (w and not str(w[0]).startswith("out:") and not str(w[0]).startswith("d:")) else r[0])
        self._waits(q, r, w)
        if sk not in self.dsem:
            self.nuniq += 1
            self.dsem[sk] = [self.es.enter_context(self.nc.semaphore("dq%d" % self.nuniq)), 0]
        ent = self.dsem[sk]
        ent[1] += 16
        self.eng[q].dma_start(out=out, in_=in_, **kw).then_inc(ent[0], 16)
        tok = (ent[0], ent[1], "D" + str(sk), None)
        self._update(r, w, tok)
        for k in w:
            if str(k).startswith("out:"):
                self.out_tokens.append(tok)
        return tok

    def finish(self):
        eng = self.eng["sp"]
        best = {}
        for (sem, val, sid, owner) in self.out_tokens:
            if sid not in best or best[sid][1] < val:
                best[sid] = (sem, val)
        for sid, (sem, val) in best.items():
            eng.wait_ge(sem, val)
        self.es.close()
        return self.nc


def run_prog(P, in_maps):
    nc = P.finish()
    res = run_bass_kernel_spmd(nc, in_maps, core_ids=list(range(NCORES)))
    return res.results


def feat_pc(v):
    return np.ascontiguousarray(np.asarray(v, np.float32).reshape(-1, 128).T)


def build_k0():
    P = Prog()
    NC_ = 1536
    cvT = P.din("cvT", [128, 16, 2])
    w = P.din("w", [2, D, NC_])
    b = P.din("b", [2, NC_])
    o = P.dout("o", [2, 2, NC_])
    s_raw = P.sb("s_raw", [128, 16, 2])
    s = P.sb("s", [128, 16, 2])
    W = P.sb("W", [128, 16, NC_])
    bt = P.sb("bt", [2, 2, NC_])
    ot = P.sb("ot", [2, 2, NC_])
    pss = [P.ps("ps%d" % i, [2, 512]) for i in range(3)]
    P.dma("sp", s_raw[:], cvT[:, :, :], w=["s_raw"])
    P.op("act", lambda e: e.activation(out=s[:], in_=s_raw[:], func=AF.Silu), r=["s_raw"], w=["s"])
    for i in range(2):
        P.dma("sp", bt[:, i, :], b[i].partition_broadcast(2), w=["bt%d" % i])
    for i in range(2):
        wv = w[i].rearrange("(c p) n -> p c n", p=128)
        for g in range(4):
            P.dma("sp" if g % 2 == 0 else "pool", W[:, 4 * g:4 * g + 4, :], wv[:, 4 * g:4 * g + 4, :], w=["W%d" % g])
        for nb in range(3):
            for c in range(16):
                P.op("pe", lambda e, c=c, nb=nb: e.matmul(pss[nb][:], lhsT=s[:, c, :], rhs=W[:, c, nb * 512:(nb + 1) * 512],
                                                          start=(c == 0), stop=(c == 15)),
                     r=["s", "W%d" % (c // 4)], w=["ps%d" % nb])
            P.op("dve", lambda e, nb=nb, i=i: e.tensor_tensor(out=ot[:, i, nb * 512:(nb + 1) * 512], in0=pss[nb][:],
                                                              in1=bt[:, i, nb * 512:(nb + 1) * 512], op=ALU.add),
                 r=["ps%d" % nb, "bt%d" % i], w=["ot%d_%d" % (i, nb)])
    for i in range(2):
        P.dma("sp", o[i], ot[:, i, :], r=["ot%d_%d" % (i, nb) for nb in range(3)], w=["out:o%d" % i], sk="ot%d" % i)
    return P


def run_k0(c, c_ctx, mod_w, mod_b):
    P = build_k0()
    cv = np.stack([np.asarray(c, np.float32).reshape(-1), np.asarray(c_ctx, np.float32).reshape(-1)], -1)
    cvT = np.ascontiguousarray(cv.reshape(16, 128, 2).transpose(1, 0, 2))
    maps = []
    for j in range(NCORES):
        sl = slice(j * 1536, (j + 1) * 1536)
        maps.append({"cvT": cvT, "w": np.ascontiguousarray(mod_w[:, :, sl]), "b": np.ascontiguousarray(mod_b[:, sl])})
    res = run_prog(P, maps)
    return np.concatenate([r["o"] for r in res], axis=-1)


def _barrier(self):
    for e, eng in self.eng.items():
        for e2 in self.eng:
            if e2 == e or self.ecnt[e2] == 0:
                continue
            if self.seen[e].get("E" + e2, 0) < self.ecnt[e2]:
                eng.wait_ge(self.esem[e2], self.ecnt[e2])
                self.seen[e]["E" + e2] = self.ecnt[e2]
        for sk, (sem, cnt) in self.dsem.items():
            if cnt and self.seen[e].get("D" + str(sk), 0) < cnt:
                eng.wait_ge(sem, cnt)
                self.seen[e]["D" + str(sk)] = cnt


@contextlib.contextmanager
def _phase(self):
    old = self.es
    st = contextlib.ExitStack()
    self.es_sems = old
    self.es = st
    try:
        yield
    finally:
        self.barrier()
        self.es = old
        st.close()


Prog.barrier = _barrier
Prog.phase = _phase


def _dma2(self, q, out, in_, r=(), w=(), sk=None, **kw):
    cur = self.es
    self.es = self.root_es
    try:
        return Prog._dma_orig(self, q, out, in_, r=r, w=w, sk=sk, **kw)
    finally:
        self.es = cur


Prog._dma_orig = Prog.dma
Prog.dma = _dma2
_old_init = Prog.__init__


def _init2(self):
    _old_init(self)
    self.root_es = self.es


Prog.__init__ = _init2
_old_finish = Prog.finish


def _finish2(self):
    self.es = self.root_es
    return _old_finish(self)


Prog.finish = _finish2


def consts_np():
    s = np.arange(128)[:, None]
    c = np.arange(128)[None, :]
    ident = np.eye(128, dtype=np.float32)
    tri = np.stack([(s <= c), (s >= c)]).astype(np.float32)
    strict = np.stack([(s > c), (s < c)]).astype(np.float32)
    return ident, tri, strict


def pos_tables():
    quarter = D // 4
    omega = (1.0 / (10000.0 ** (np.arange(quarter, dtype=np.float32) / np.float32(quarter)))).astype(np.float32)
    ang_r = np.arange(128, dtype=np.float32)[:, None] * omega
    ang_c = np.arange(64, dtype=np.float32)[:, None] * omega
    emb_r = np.concatenate([np.sin(ang_r), np.cos(ang_r)], -1).astype(np.float32)
    emb_c = np.concatenate([np.sin(ang_c), np.cos(ang_c)], -1).astype(np.float32)
    return emb_r, np.concatenate([emb_c, emb_c], 0)


def emit_hT(P, PS, n_chunks, src_fn, pos_fn, ab_fn, embr, embc2_d, ident, dst_fn, dst_key_fn, pfx="h"):
    xt = [P.sb(pfx + "xt%d" % b, [128, D]) for b in range(2)]
    pr = [P.sb(pfx + "pr%d" % b, [128, 1024]) for b in range(2)]
    pc = P.sb(pfx + "pc", [128, 1024])
    junk = P.sb(pfx + "junk", [128, D], BF16)
    xn = P.sb(pfx + "xn", [128, D])
    st = P.sb(pfx + "st", [128, 4])
    hTt = [P.sb(pfx + "hTo%d" % b, [128, 16, 128], BF16) for b in range(2)]
    if embc2_d is not None:
        P.dma("sp", pc[:], embc2_d[:, :], w=[pfx + "pc"])

    def load(n):
        b = n % 2
        src = src_fn(n)
        P.dma("sp", xt[b][:, 0:1024], src[:, 0:1024], w=[pfx + "xt%da" % b])
        P.dma("pool", xt[b][:, 1024:2048], src[:, 1024:2048], w=[pfx + "xt%db" % b])
        r0 = pos_fn(n)
        if r0 is not None:
            P.dma("sp", pr[b][0:64, :], embr[r0].partition_broadcast(64), w=[pfx + "pr%d" % b], sk=pfx + "pr%dlo" % b)
            P.dma("sp", pr[b][64:128, :], embr[r0 + 1].partition_broadcast(64), w=[pfx + "pr%dh" % b], sk=pfx + "pr%dhi" % b)

    import os
    dbg = int(os.environ.get("HTDBG", "99"))
    if dbg < 99:
        n_chunks = 3
    load(0)
    for n in range(n_chunks):
        b = n % 2
        if n + 1 < n_chunks:
            load(n + 1)
        ka, kb = pfx + "xt%da" % b, pfx + "xt%db" % b
        if pos_fn(n) is not None:
            P.op("pool", lambda e: e.tensor_tensor(out=xt[b][:, 0:1024], in0=xt[b][:, 0:1024], in1=pr[b][:], op=ALU.add),
                 r=[ka, pfx + "pr%d" % b, pfx + "pr%dh" % b], w=[ka])
            P.op("dve", lambda e: e.tensor_tensor(out=xt[b][:, 1024:2048], in0=xt[b][:, 1024:2048], in1=pc[:], op=ALU.add),
                 r=[kb, pfx + "pc"], w=[kb])
        if dbg < 1:
            continue
        P.op("act", lambda e: e.activation(out=junk[:], in_=xt[b][:], func=AF.Square, accum_out=st[:, 0:1]),
             r=[ka, kb], w=[pfx + "junk", pfx + "st0"])
        P.op("dve", lambda e: e.tensor_scalar(out=st[:, 1:2], in0=st[:, 0:1], scalar1=1.0 / D, scalar2=EPS, op0=ALU.mult, op1=ALU.add),
             r=[pfx + "st0"], w=[pfx + "st1"])
        P.op("act", lambda e: e.activation(out=st[:, 2:3], in_=st[:, 1:2], func=AF.Sqrt), r=[pfx + "st1"], w=[pfx + "st2"])
        P.op("dve", lambda e: e.reciprocal(out=st[:, 3:4], in_=st[:, 2:3]), r=[pfx + "st2"], w=[pfx + "st3"])
        P.op("pool", lambda e: e.tensor_scalar(out=xn[:], in0=xt[b][:], scalar1=st[:, 3:4], scalar2=0.0, op0=ALU.mult, op1=ALU.add),
             r=[ka, kb, pfx + "st3"], w=[pfx + "xn"])
        if dbg < 2:
            continue
        A, Bv, keyA, keyB = ab_fn(n)
        for k4 in range(4):
            bank = PS[k4]
            bk = "B%d" % k4
            for q in range(4):
                c = 4 * k4 + q
                P.op("pe", lambda e, c=c, q=q: e.transpose(out=bank[:, q * 128:(q + 1) * 128], in_=xn[:, c * 128:(c + 1) * 128], identity=ident[:]),
                     r=[pfx + "xn", "ident"], w=[bk])
            for q in range(4):
                c = 4 * k4 + q
                if k4 % 2 == 0:
                    P.op("act", lambda e, c=c, q=q: e.activation(out=hTt[b][:, c, :], in_=bank[:, q * 128:(q + 1) * 128], func=AF.Identity,
                                                                   scale=A[:, c:c + 1], bias=Bv[:, c:c + 1]),
                         r=[keyA, keyB], w=[bk, pfx + "hTo%d_%d" % (b, c)])
                else:
                    P.op("dve", lambda e, c=c, q=q: e.tensor_scalar(out=hTt[b][:, c, :], in0=bank[:, q * 128:(q + 1) * 128],
                                                                      scalar1=A[:, c:c + 1], scalar2=Bv[:, c:c + 1], op0=ALU.mult, op1=ALU.add),
                         r=[keyA, keyB], w=[bk, pfx + "hTo%d_%d" % (b, c)])
        if dbg < 3:
            continue
        P.dma("sp", dst_fn(n), hTt[b][:], r=[pfx + "hTo%d_%d" % (b, c) for c in range(16)], w=[dst_key_fn(n)], sk=pfx + "hTo%d" % b)


def emit_AB(P, gn_d, sc_d, sh_d, name):
    g = P.sb(name + "g", [128, 16])
    sc = P.sb(name + "sc", [128, 16])
    A = P.sb(name + "A", [128, 16])
    Bv = P.sb(name + "B", [128, 16])
    P.dma("sp", g[:], gn_d[:, :], w=[name + "g"])
    P.dma("sp", sc[:], sc_d[:, :], w=[name + "sc"])
    P.dma("sp", Bv[:], sh_d[:, :], w=[name + "B"])
    P.op("dve", lambda e: e.scalar_tensor_tensor(out=A[:], in0=sc[:], scalar=1.0, in1=g[:], op0=ALU.add, op1=ALU.mult),
         r=[name + "g", name + "sc"], w=[name + "A"])
    return A, Bv, name + "A", name + "B"


K1_COLS = 736
LSEGS = [(0, 256)] + [(256 + 512 * k, 512) for k in range(16)]


def xa_col(t):
    return 2 + t if t < 256 else 261 + (t - 256)


def build_k1(stop=9):
    P = Prog()
    xl = P.din("xl", [SEQ, D])
    xc = P.din("xc", [CTX, D])
    embr = P.din("embr", [128, 1024])
    embc2 = P.din("embc2", [128, 1024])
    gn = P.din("gn", [128, 16])
    scl = P.din("scl", [128, 16]); shl = P.din("shl", [128, 16])
    scc = P.din("scc", [128, 16]); shc = P.din("shc", [128, 16])
    win = P.din("win", [D, K1_COLS])
    convw = P.din("convw", [128, 4]); convb = P.din("convb", [128, 1])
    bd = P.din("bd", [4, 128, 128])
    gb = P.din("gb", [128, 4])
    lam = P.din("lam", [128, 2])
    wg = P.din("wg", [2, 16, 64]); bg = P.din("bg", [2, 64])
    gng = P.din("gng", [128])
    identd = P.din("ident", [128, 128]); trid = P.din("tri", [2, 128, 128]); strd = P.din("strict", [2, 128, 128])
    yaT = P.dout("yaT", [128, T_ALL], BF16)
    yb = P.dout("yb", [T_ALL, 128], BF16)
    hTd = P.dscr("hTd", [NCH, 128, 16, 128], BF16)

    PS = [P.ps("PS%d" % i, [128, 512]) for i in range(8)]
    ident = P.sb("ident", [128, 128])
    P.dma("sp", ident[:], identd[:, :], w=["ident"])
    Wb = P.sb("Wb", [128, 16, K1_COLS], BF16)
    winv = win.rearrange("(c p) n -> p c n", p=128)
    for g4 in range(4):
        P.dma("pool", Wb[:, 4 * g4:4 * g4 + 4, :], winv[:, 4 * g4:4 * g4 + 4, :], w=["Wb%d" % g4])
    WK = ["Wb%d" % g4 for g4 in range(4)]

    with P.phase():
        Al = emit_AB(P, gn, scl, shl, "l")
        Ac = emit_AB(P, gn, scc, shc, "c")
        emit_hT(P, PS, NCH,
                src_fn=lambda n: (xc[n * 128:(n + 1) * 128, :] if n < 2 else xl[(n - 2) * 128:(n - 1) * 128, :]),
                pos_fn=lambda n: (None if n < 2 else 2 * (n - 2)),
                ab_fn=lambda n: (Ac if n < 2 else Al),
                embr=embr, embc2_d=embc2, ident=ident,
                dst_fn=lambda n: hTd[n], dst_key_fn=lambda n: "d:hT%d" % n)

    if stop < 1:
        return P
    with P.phase():
        XW = 8454
        XA = P.sb("XA", [128, XW])
        GG = P.sb("GG", [128, T_ALL], BF16)
        HF = P.sb("HF", [128, T_ALL])
        hTt = [P.sb("hTi%d" % b, [128, 16, 128], BF16) for b in range(2)]
        cw = P.sb("cw", [128, 4]); cb = P.sb("cb", [128, 1]); gbt = P.sb("gbt", [128, 4]); lamt = P.sb("lamt", [128, 2])
        cd = P.sb("cd", [128, 4])
        BD = P.sb("BD", [128, 4, 128], BF16)
        P.dma("sp", cw[:], convw[:, :], w=["cw"]); P.dma("sp", cb[:], convb[:, :], w=["cb"])
        P.dma("sp", gbt[:], gb[:, :], w=["gbt"]); P.dma("sp", lamt[:], lam[:, :], w=["lamt"])
        for i in range(4):
            P.dma("pool", BD[:, i, :], bd[i], w=["BD%d" % i])
        tmp2 = P.sb("tmp2", [128, 2])
        P.op("act", lambda e: e.activation(out=tmp2[:], in_=lamt[:], func=AF.Exp, scale=-1.0), r=["lamt"], w=["tmp2"])
        P.op("act", lambda e: e.activation(out=tmp2[:], in_=tmp2[:], func=AF.Ln, bias=1.0), r=["tmp2"], w=["tmp2"])
        P.op("dve", lambda e: e.tensor_scalar(out=cd[:, 0:2], in0=tmp2[:], scalar1=-8.0, scalar2=None, op0=ALU.mult), r=["tmp2"], w=["cd"])
        P.op("dve", lambda e: e.tensor_scalar(out=cd[:, 2:4], in0=tmp2[:], scalar1=-16.0, scalar2=None, op0=ALU.mult), r=["tmp2", "cd"], w=["cd"])
        P.op("pool", lambda e: e.memset(XA[:], 0.0), w=["XA"])
        gx = P.sb("gx", [128, 128]); gx2 = P.sb("gx2", [128, 128]); gs = P.sb("gs", [128, 128])

        def loadh(n):
            P.dma("sp", hTt[n % 2][:], hTd[n], r=["d:hT%d" % n], w=["hTi%d" % (n % 2)])
        loadh(0)
        for n in range(NCH):
            b = n % 2
            if n + 1 < NCH:
                loadh(n + 1)
            pa, pg = PS[2 * b], PS[2 * b + 1]
            for c in range(16):
                P.op("pe", lambda e, c=c: e.matmul(pa[:, 0:128], lhsT=Wb[:, c, 0:128], rhs=hTt[b][:, c, :], start=(c == 0), stop=(c == 15)),
                     r=["hTi%d" % b, WK[c // 4]], w=["PS%d" % (2 * b)])
            for c in range(16):
                P.op("pe", lambda e, c=c: e.matmul(pg[:, 0:128], lhsT=Wb[:, c, 128:256], rhs=hTt[b][:, c, :], start=(c == 0), stop=(c == 15)),
                     r=["hTi%d" % b, WK[c // 4]], w=["PS%d" % (2 * b + 1)])
            col = xa_col(n * 128)
            P.op("act", lambda e: e.activation(out=XA[:, col:col + 128], in_=pa[:, 0:128], func=AF.Copy), r=["PS%d" % (2 * b)], w=["XA"])
            P.op("act", lambda e: e.activation(out=gx[:], in_=pg[:, 0:128], func=AF.Copy), r=["PS%d" % (2 * b + 1)], w=["gx"])
            P.op("dve", lambda e: e.tensor_tensor(out=gx2[:], in0=gx[:], in1=gx[:], op=ALU.mult), r=["gx"], w=["gx2"])
            P.op("dve", lambda e: e.tensor_scalar(out=gx2[:], in0=gx2[:], scalar1=0.044715, scalar2=1.0, op0=ALU.mult, op1=ALU.add), r=["gx2"], w=["gx2"])
            P.op("dve", lambda e: e.tensor_tensor(out=gx2[:], in0=gx2[:], in1=gx[:], op=ALU.mult), r=["gx2", "gx"], w=["gx2"])
            P.op("act", lambda e: e.activation(out=gs[:], in_=gx2[:], func=AF.Sigmoid, scale=1.5957691216057308), r=["gx2"], w=["gs"])
            P.op("dve", lambda e: e.tensor_tensor(out=GG[:, n * 128:(n + 1) * 128], in0=gs[:], in1=gx[:], op=ALU.mult), r=["gs", "gx"], w=["GG"])
        L = 512
        xcv = P.sb("xcv", [128, L]); xcb = P.sb("xcb", [128, L], BF16)
        rr = P.sb("rr", [128, L]); ii = P.sb("ii", [128, L]); aa = P.sb("aa", [128, L]); tt = P.sb("tt", [128, L]); bb = P.sb("bb", [128, L])
        hb = P.sb("hb", [128, L]); yo = [P.sb("yo%d" % b, [128, L], BF16) for b in range(2)]
        carry = P.sb("carry", [128, 1])
        for d in range(2):
            order = LSEGS if d == 0 else [LSEGS[0]] + LSEGS[:0:-1]
            for si, (t0, Ls) in enumerate(order):
                c0 = xa_col(t0)
                P.op("dve", lambda e: e.tensor_scalar(out=xcv[:, :Ls], in0=XA[:, c0 - 2:c0 - 2 + Ls], scalar1=cw[:, 0:1], scalar2=cb[:, 0:1], op0=ALU.mult, op1=ALU.add),
                     r=["XA", "cw", "cb"], w=["xcv"])
                for j in range(1, 4):
                    P.op("dve", lambda e, j=j: e.scalar_tensor_tensor(out=xcv[:, :Ls], in0=XA[:, c0 - 2 + j:c0 - 2 + j + Ls], scalar=cw[:, j:j + 1], in1=xcv[:, :Ls], op0=ALU.mult, op1=ALU.add),
                         r=["XA", "cw", "xcv"], w=["xcv"])
                P.op("pool", lambda e: e.tensor_copy(out=xcb[:, :Ls], in_=xcv[:, :Ls]), r=["xcv"], w=["xcb"])
                P.op("pe", lambda e: e.matmul(PS[0][:, :Ls], lhsT=BD[:, d, :], rhs=xcb[:, :Ls], start=True, stop=True), r=["xcb", "BD%d" % d], w=["PS0"])
                P.op("pe", lambda e: e.matmul(PS[1][:, :Ls], lhsT=BD[:, 2 + d, :], rhs=xcb[:, :Ls], start=True, stop=True), r=["xcb", "BD%d" % (2 + d)], w=["PS1"])
                P.op("act", lambda e: e.activation(out=rr[:, :Ls], in_=PS[0][:, :Ls], func=AF.Sigmoid, bias=gbt[:, d:d + 1]), r=["PS0", "gbt"], w=["rr"])
                P.op("act", lambda e: e.activation(out=ii[:, :Ls], in_=PS[1][:, :Ls], func=AF.Sigmoid, bias=gbt[:, 2 + d:3 + d]), r=["PS1", "gbt"], w=["ii"])
                P.op("act", lambda e: e.activation(out=aa[:, :Ls], in_=rr[:, :Ls], func=AF.Exp, scale=cd[:, d:d + 1]), r=["rr", "cd"], w=["aa"])
                P.op("act", lambda e: e.activation(out=tt[:, :Ls], in_=rr[:, :Ls], func=AF.Exp, scale=cd[:, 2 + d:3 + d]), r=["rr", "cd"], w=["tt"])
                P.op("act", lambda e: e.activation(out=tt[:, :Ls], in_=tt[:, :Ls], func=AF.Sqrt, scale=-1.0, bias=1.0), r=["tt"], w=["tt"])
                P.op("dve", lambda e: e.tensor_tensor(out=bb[:, :Ls], in0=tt[:, :Ls], in1=ii[:, :Ls], op=ALU.mult), r=["tt", "ii"], w=["bb"])
                P.op("dve", lambda e: e.tensor_tensor(out=bb[:, :Ls], in0=bb[:, :Ls], in1=xcv[:, :Ls], op=ALU.mult), r=["bb", "xcv"], w=["bb"])
                if d == 0:
                    init = 0.0 if si == 0 else HF[:, t0 - 1:t0]
                    P.op("dve", lambda e: e.tensor_tensor_scan(out=HF[:, t0:t0 + Ls], data0=aa[:, :Ls], data1=bb[:, :Ls], initial=init, op0=ALU.mult, op1=ALU.add),
                         r=["aa", "bb", "HF"], w=["HF"])
                else:
                    init = 0.0 if si == 0 else carry[:, 0:1]
                    P.op("dve", lambda e: e.tensor_tensor_scan(out=hb[:, slice(Ls - 1, None, -1)], data0=aa[:, slice(Ls - 1, None, -1)], data1=bb[:, slice(Ls - 1, None, -1)],
                                                               initial=init, op0=ALU.mult, op1=ALU.add),
                         r=["aa", "bb", "carry"], w=["hb"])
                    P.op("dve", lambda e: e.tensor_copy(out=carry[:], in_=hb[:, 0:1]), r=["hb"], w=["carry"])
                    P.op("dve", lambda e: e.tensor_tensor(out=hb[:, :Ls], in0=hb[:, :Ls], in1=HF[:, t0:t0 + Ls], op=ALU.add), r=["hb", "HF"], w=["hb"])
                    yb_ = yo[si % 2]
                    P.op("dve", lambda e: e.tensor_tensor(out=yb_[:, :Ls], in0=hb[:, :Ls], in1=GG[:, t0:t0 + Ls], op=ALU.mult), r=["hb", "GG"], w=["yo%d" % (si % 2)])
                    P.dma("sp", yaT[:, t0:t0 + Ls], yb_[:, :Ls], r=["yo%d" % (si % 2)], w=["out:yaT%d" % t0])
    if stop < 2:
        return P
    emit_gla(P, PS, Wb, WK, hTd, wg, bg, gng, trid, strd, yb)
    return P


def emit_gla(P, PS, Wb, WK, hTd, wg, bg, gng, trid, strd, yb):
    with P.phase():
        OF = P.sb("OF", [128, NCH, 128])
        tri = P.sb("tri", [128, 2, 128]); stri = P.sb("stri", [128, 2, 128])
        for d in range(2):
            P.dma("sp", tri[:, d, :], trid[d], w=["tri%d" % d]); P.dma("sp", stri[:, d, :], strd[d], w=["stri%d" % d])
        wgt = P.sb("wgt", [16, 2, 64]); bgb = P.sb("bgb", [128, 2, 64]); gngb = P.sb("gngb", [128, 128])
        for d in range(2):
            P.dma("sp", wgt[:, d, :], wg[d], w=["wgt%d" % d])
            P.dma("sp", bgb[:, d, :], bg[d].partition_broadcast(128), w=["bgb%d" % d])
        P.dma("sp", gngb[:], gng.partition_broadcast(128), w=["gngb"])

        class R:
            pass
        RS = []
        for d in range(2):
            r = R(); s_ = "_%d" % d
            r.h = [P.sb("hTg%d" % b + s_, [128, 16, 128], BF16) for b in range(2)]
            r.S = P.sb("S" + s_, [64, 128]); r.Sb = P.sb("Sb" + s_, [64, 128], BF16)
            r.adT = P.sb("adT" + s_, [16, 128]); r.xg = P.sb("xg" + s_, [128, 64]); r.lg = P.sb("lg" + s_, [128, 64])
            r.E1 = P.sb("E1" + s_, [64, 128]); r.E2 = P.sb("E2" + s_, [64, 128]); r.E3 = P.sb("E3" + s_, [128, 64])
            r.qd = P.sb("qd" + s_, [64, 128], BF16); r.ki = P.sb("ki" + s_, [64, 128], BF16); r.ke = P.sb("ke" + s_, [128, 64], BF16)
            r.vb = P.sb("vb" + s_, [128, 128], BF16); r.sog = P.sb("sog" + s_, [128, 128]); r.scm = P.sb("scm" + s_, [128, 128], BF16)
            r.ot = P.sb("ot" + s_, [128, 128]); r.oj = P.sb("oj" + s_, [128, 128]); r.gst = P.sb("gst" + s_, [128, 4])
            r.ybt = [P.sb("ybt%d" % b + s_, [128, 128], BF16) for b in range(2)]
            r.order = list(range(NCH)) if d == 0 else [1, 0] + list(range(NCH - 1, 1, -1))
            r.arr = {n: i for i, n in enumerate(r.order)}
            RS.append(r)
            P.op("dve", lambda e: e.memset(r.S[:], 0.0), w=["S" + s_])
            P.op("dve", lambda e: e.memset(r.Sb[:], 0.0), w=["Sb" + s_])

        def loadh(d, i):
            r = RS[d]; n = r.order[i]
            P.dma("sp", r.h[i % 2][:], hTd[n], r=["d:hT%d" % n], w=["hTg%d_%d" % (i % 2, d)])

        def step(d, i):
            r = RS[d]; s_ = "_%d" % d; n = r.order[i]; b = i % 2
            K_ = lambda name: name + s_
            QK, TKC, LS, OU = PS[4 * d], PS[4 * d + 1], PS[4 * d + 2], PS[4 * d + 3]
            bQK, bTK, bLS, bOU = ["B%d" % (4 * d + k) for k in range(4)]
            cl = 127 if d == 0 else 0
            final = r.arr[n] > RS[1 - d].arr[n]
            if i + 1 < NCH:
                loadh(d, i + 1)
            hk = "hTg%d_%d" % (b, d); h = r.h[b]
            for (c0, c1, o0, M) in ((256, 320, 0, 64), (320, 384, 128, 64), (384 + 16 * d, 400 + 16 * d, 256, 16)):
                for c in range(16):
                    P.op("pe", lambda e, c=c: e.matmul(QK[0:M, o0:o0 + 128], lhsT=Wb[:, c, c0:c1], rhs=h[:, c, :], start=(c == 0), stop=(c == 15)),
                         r=[hk, WK[c // 4]], w=[bQK])
            for c in range(16):
                P.op("pe", lambda e, c=c: e.matmul(TKC[:, 0:320], lhsT=h[:, c, :], rhs=Wb[:, c, 416:736], start=(c == 0), stop=(c == 15)),
                     r=[hk, WK[c // 4]], w=[bTK])
            yield
            P.op("act", lambda e: e.activation(out=r.adT[:], in_=QK[0:16, 256:384], func=AF.Copy), r=[], w=[bQK, K_("adT")])
            P.op("pe", lambda e: e.matmul(LS[:, 0:64], lhsT=r.adT[:], rhs=wgt[:, d, :], start=True, stop=True), r=[K_("adT"), "wgt%d" % d], w=[bLS])
            P.op("dve", lambda e: e.tensor_tensor(out=r.xg[:], in0=LS[:, 0:64], in1=bgb[:, d, :], op=ALU.add), r=["bgb%d" % d], w=[bLS, K_("xg")])
            P.op("act", lambda e: e.activation(out=r.xg[:], in_=r.xg[:], func=AF.Exp, scale=-1.0), r=[], w=[K_("xg")])
            P.op("act", lambda e: e.activation(out=r.xg[:], in_=r.xg[:], func=AF.Ln, bias=1.0), r=[], w=[K_("xg")])
            P.op("dve", lambda e: e.tensor_scalar(out=r.lg[:], in0=r.xg[:], scalar1=-1.0 / 16.0, scalar2=None, op0=ALU.mult), r=[K_("xg")], w=[K_("lg")])
            yield
            P.op("pe", lambda e: e.matmul(LS[:, 64:128], lhsT=stri[:, d, :], rhs=r.lg[:], start=True, stop=True), r=[K_("lg"), "stri%d" % d], w=[bLS])
            P.op("pe", lambda e: e.matmul(TKC[0:64, 320:448], lhsT=r.lg[:], rhs=tri[:, d, :], start=True, stop=True), r=[K_("lg"), "tri%d" % d], w=[bTK])
            P.op("act", lambda e: e.activation(out=r.E1[:], in_=TKC[0:64, 320:448], func=AF.Exp), r=[], w=[bTK, K_("E1")])
            P.op("act", lambda e: e.activation(out=r.E2[:], in_=TKC[0:64, 320:448], func=AF.Exp, scale=-1.0), r=[], w=[bTK, K_("E2")])
            P.op("act", lambda e: e.activation(out=r.E3[:], in_=LS[:, 64:128], func=AF.Exp), r=[], w=[bLS, K_("E3")])
            P.op("dve", lambda e: e.scalar_tensor_tensor(out=r.qd[:], in0=r.E1[:], scalar=0.125, in1=QK[0:64, 0:128], op0=ALU.mult, op1=ALU.mult), r=[K_("E1")], w=[bQK, K_("qd")])
            P.op("dve", lambda e: e.tensor_tensor(out=r.ki[:], in0=r.E2[:], in1=QK[0:64, 128:256], op=ALU.mult), r=[K_("E2")], w=[bQK, K_("ki")])
            P.op("dve", lambda e: e.tensor_tensor(out=r.ke[:], in0=r.E3[:], in1=TKC[:, 0:64], op=ALU.mult), r=[K_("E3")], w=[bTK, K_("ke")])
            P.op("act", lambda e: e.activation(out=r.vb[:], in_=TKC[:, 64:192], func=AF.Copy), r=[], w=[bTK, K_("vb")])
            if final:
                P.op("act", lambda e: e.activation(out=r.sog[:], in_=TKC[:, 192:320], func=AF.Silu), r=[], w=[bTK, K_("sog")])
            yield
            P.op("pe", lambda e: e.matmul(LS[:, 128:256], lhsT=r.ki[:], rhs=r.qd[:], start=True, stop=True), r=[K_("ki"), K_("qd")], w=[bLS])
            P.op("dve", lambda e: e.tensor_tensor(out=r.scm[:], in0=LS[:, 128:256], in1=tri[:, d, :], op=ALU.mult), r=["tri%d" % d], w=[bLS, K_("scm")])
            P.op("pe", lambda e: e.matmul(OU[:, 0:128], lhsT=r.scm[:], rhs=r.vb[:], start=True, stop=False), r=[K_("scm"), K_("vb")], w=[bOU])
            P.op("pe", lambda e: e.matmul(OU[:, 0:128], lhsT=r.qd[:], rhs=r.Sb[:], start=False, stop=True), r=[K_("qd"), K_("Sb")], w=[bOU])
            P.op("pe", lambda e: e.matmul(OU[0:64, 128:256], lhsT=r.ke[:], rhs=r.vb[:], start=True, stop=True), r=[K_("ke"), K_("vb")], w=[bOU])
            yield
            P.op("dve", lambda e: e.scalar_tensor_tensor(out=r.S[:], in0=r.S[:], scalar=r.E1[:, cl:cl + 1], in1=OU[0:64, 128:256], op0=ALU.mult, op1=ALU.add),
                 r=[K_("E1")], w=[bOU, K_("S")])
            P.op("pool", lambda e: e.tensor_copy(out=r.Sb[:], in_=r.S[:]), r=[K_("S")], w=[K_("Sb")])
            if not final:
                P.op("act", lambda e: e.activation(out=OF[:, n, :], in_=OU[:, 0:128], func=AF.Copy), r=[], w=[bOU, "OF%d" % n])
            else:
                assert ("OF%d" % n) in P.lastw
                P.op("dve", lambda e: e.tensor_tensor(out=r.ot[:], in0=OU[:, 0:128], in1=OF[:, n, :], op=ALU.add), r=["OF%d" % n], w=[bOU, K_("ot")])
                P.op("act", lambda e: e.activation(out=r.oj[:], in_=r.ot[:], func=AF.Square, accum_out=r.gst[:, 0:1]), r=[K_("ot")], w=[K_("oj"), K_("gst0")])
                P.op("dve", lambda e: e.tensor_scalar(out=r.gst[:, 1:2], in0=r.gst[:, 0:1], scalar1=1.0 / 128, scalar2=EPS, op0=ALU.mult, op1=ALU.add), r=[K_("gst0")], w=[K_("gst1")])
                P.op("act", lambda e: e.activation(out=r.gst[:, 2:3], in_=r.gst[:, 1:2], func=AF.Sqrt), r=[K_("gst1")], w=[K_("gst2")])
                P.op("dve", lambda e: e.reciprocal(out=r.gst[:, 3:4], in_=r.gst[:, 2:3]), r=[K_("gst2")], w=[K_("gst3")])
                P.op("dve", lambda e: e.scalar_tensor_tensor(out=r.ot[:], in0=r.ot[:], scalar=r.gst[:, 3:4], in1=gngb[:], op0=ALU.mult, op1=ALU.mult), r=[K_("gst3"), "gngb"], w=[K_("ot")])
                yt = r.ybt[b]
                P.op("dve", lambda e: e.tensor_tensor(out=yt[:], in0=r.ot[:], in1=r.sog[:], op=ALU.mult), r=[K_("ot"), K_("sog")], w=[K_("ybt%d" % b)])
                P.dma("sp", yb[n * 128:(n + 1) * 128, :], yt[:], r=[K_("ybt%d" % b)], w=["out:yb%d" % n])
            yield

        loadh(0, 0); loadh(1, 0)

        def chain(d):
            for i in range(NCH):
                yield from step(d, i)
        ga, gb = chain(0), chain(1)
        for _ in range(2):
            next(ga)
        alive = [ga, gb]
        while alive:
            for g_ in list(alive):
                try:
                    next(g_)
                except StopIteration:
                    alive.remove(g_)


def k1_inputs(x, ctx, modv0, norm1_g0, ev_w_in, ev_conv_w, ev_conv_b, lru_wa, lru_ba, lru_wx, lru_bx, lru_lambda, gla_wg_up, gla_bg, gla_norm_g):
    ident, tri, strict = consts_np()
    embr, embc2 = pos_tables()
    xl = np.ascontiguousarray(x[0]); xc = np.ascontiguousarray(ctx[0])
    base = {"xl": xl, "xc": xc, "embr": embr, "embc2": embc2, "gn": feat_pc(norm1_g0),
            "shl": feat_pc(modv0[0, 0:2048]), "scl": feat_pc(modv0[0, 2048:4096]),
            "shc": feat_pc(modv0[1, 0:2048]), "scc": feat_pc(modv0[1, 2048:4096]),
            "ident": ident, "tri": tri, "strict": strict}
    maps = []
    W = ev_w_in[0]
    for j in range(NCORES):
        s128 = slice(128 * j, 128 * j + 128)
        cols = np.concatenate([np.arange(128 * j, 128 * j + 128), 1024 + np.arange(128 * j, 128 * j + 128),
                               2048 + np.arange(64 * j, 64 * j + 64), 2560 + np.arange(64 * j, 64 * j + 64),
                               5120 + np.arange(32),
                               2560 + np.arange(64 * j, 64 * j + 64), 3072 + np.arange(128 * j, 128 * j + 128),
                               4096 + np.arange(128 * j, 128 * j + 128)])
        bdm = np.zeros((4, 128, 128), np.float32)
        for i, (arr, d) in enumerate([(lru_wa[0], 0), (lru_wa[0], 1), (lru_wx[0], 0), (lru_wx[0], 1)]):
            for m in range(2):
                bdm[i, 64 * m:64 * m + 64, 64 * m:64 * m + 64] = arr[d, 2 * j + m]
        m = dict(base)
        m.update({"win": np.ascontiguousarray(W[:, cols]),
                  "convw": np.ascontiguousarray(ev_conv_w[0][:, s128].T), "convb": np.ascontiguousarray(ev_conv_b[0][s128, None]),
                  "bd": bdm,
                  "gb": np.ascontiguousarray(np.stack([lru_ba[0][0, s128], lru_ba[0][1, s128], lru_bx[0][0, s128], lru_bx[0][1, s128]], -1)),
                  "lam": np.ascontiguousarray(lru_lambda[0][:, s128].T),
                  "wg": np.ascontiguousarray(gla_wg_up[0][:, :, 64 * j:64 * j + 64]), "bg": np.ascontiguousarray(gla_bg[0][:, 64 * j:64 * j + 64]),
                  "gng": np.ascontiguousarray(gla_norm_g[0][s128])})
        maps.append(m)
    return maps


def run_k1(maps):
    P = build_k1()
    res = run_prog(P, maps)
    preT = np.concatenate([r["yaT"] for r in res] + [np.ascontiguousarray(r["yb"].T) for r in res], axis=0)
    return preT


def build_post(layer):
    P = Prog()
    NT = 9 if layer == 0 else 8
    KC = 16 if layer == 0 else 32
    xin = P.din("xin", [NT, 128, D])
    yT = P.din("yT", [KC, 128, NT * 128], BF16)
    wout = P.din("wout", [KC * 128, D])
    g1 = P.din("g1", [2, D])
    gn = P.din("gn", [128, 16])
    sc2l = P.din("sc2l", [128, 16]); sh2l = P.din("sh2l", [128, 16])
    sc2c = P.din("sc2c", [128, 16]); sh2c = P.din("sh2c", [128, 16])
    rw = P.din("rw", [128, 16, 32])
    rb = P.din("rb", [32])
    identd = P.din("ident", [128, 128])
    if layer == 0:
        embr = P.din("embr", [16, 1024]); embc2 = P.din("embc2", [128, 1024])
    else:
        ssqp = P.din("ssqp", [NT, 128, 8])
    x1o = P.dout("x1o", [NT, 128, D])
    h2To = P.dout("h2To", [16, 128, NT * 128], BF16)
    gto = P.dout("gto", [NT, 128, 32])

    PS = [P.ps("PS%d" % i, [128, 512]) for i in range(8)]
    ident = P.sb("ident", [128, 128]); P.dma("sp", ident[:], identd[:, :], w=["ident"])
    Wb = P.sb("Wb", [128, KC, D], BF16)
    wv = wout.rearrange("(c p) n -> p c n", p=128)
    for c in range(KC):
        P.dma("pool", Wb[:, c, :], wv[:, c, :], w=["Wb%d" % c])
    Al = emit_AB(P, gn, sc2l, sh2l, "l")
    Ac = emit_AB(P, gn, sc2c, sh2c, "c") if layer == 0 else None
    G1l = P.sb("G1l", [128, D]); P.dma("sp", G1l[:], g1[0].partition_broadcast(128), w=["G1l"])
    if layer == 0:
        G1c = P.sb("G1c", [128, D]); P.dma("sp", G1c[:], g1[1].partition_broadcast(128), w=["G1c"])
        pc = P.sb("pc", [128, 1024]); P.dma("sp", pc[:], embc2[:, :], w=["pc"])
        pr = P.sb("pr", [128, 1024])
    rwt = P.sb("rwt", [128, 16, 32]); P.dma("sp", rwt[:], rw[:, :, :], w=["rwt"])
    rbb = P.sb("rbb", [128, 32]); P.dma("sp", rbb[:], rb.partition_broadcast(128), w=["rbb"])
    xt = P.sb("xt", [128, D]); tmp = P.sb("tmp", [128, D]); junk = P.sb("junk", [128, D], BF16)
    yt = [P.sb("yt%d" % b, [128, KC, 128], BF16) for b in range(2)]
    st = P.sb("st", [128, 8])
    hq = [P.sb("hq%d" % i, [128, 128]) for i in range(4)]
    h2b = P.sb("h2b", [128, 16, 128], BF16)
    sc = P.sb("sc", [128, 32]); sel = P.sb("sel", [128, 32]); sel2 = P.sb("sel2", [128, 32]); eq = P.sb("eq", [128, 32])
    m1 = P.sb("m1", [128, 4]); m2 = P.sb("m2", [128, 4]); gsx = P.sb("gsx", [128, 4]); gmask = P.sb("gmask", [128, 4]); gmax = P.sb("gmax", [128, 4])
    gout = P.sb("gout", [128, 32])

    def loady(i):
        P.dma("sp", yt[i % 2][:], yT[:, :, i * 128:(i + 1) * 128].rearrange("c p t -> p c t"), w=["yt%d" % (i % 2)])
    loady(0)
    for i in range(NT):
        b = i % 2
        if i + 1 < NT:
            loady(i + 1)
        isctx = (layer == 0 and i == 0)
        P.dma("pool", xt[:], xin[i], w=["xt"])
        if layer == 0 and not isctx:
            r0 = 2 * (i - 1)
            P.dma("sp", pr[0:64, :], embr[r0].partition_broadcast(64), w=["pr"], sk="prlo")
            P.dma("sp", pr[64:128, :], embr[r0 + 1].partition_broadcast(64), w=["prh"], sk="prhi")
            P.op("pool", lambda e: e.tensor_tensor(out=xt[:, 0:1024], in0=xt[:, 0:1024], in1=pr[:], op=ALU.add), r=["pr", "prh"], w=["xt"])
            P.op("dve", lambda e: e.tensor_tensor(out=xt[:, 1024:2048], in0=xt[:, 1024:2048], in1=pc[:], op=ALU.add), r=["pc"], w=["xt"])
        if layer == 1:
            ssq = P.sb("ssq%d" % i, [128, 8])
            P.dma("sp", ssq[:], ssqp[i], w=["ssq%d" % i])
            P.op("dve", lambda e: e.tensor_reduce(out=st[:, 4:5], in_=ssq[:], axis=AX.X, op=ALU.add), r=["ssq%d" % i], w=["st4"])
            P.op("dve", lambda e: e.tensor_scalar(out=st[:, 5:6], in0=st[:, 4:5], scalar1=1.0 / 4096, scalar2=EPS, op0=ALU.mult, op1=ALU.add), r=["st4"], w=["st5"])
            P.op("act", lambda e: e.activation(out=st[:, 6:7], in_=st[:, 5:6], func=AF.Sqrt), r=["st5"], w=["st6"])
            P.op("dve", lambda e: e.reciprocal(out=st[:, 7:8], in_=st[:, 6:7]), r=["st6"], w=["st7"])
        for nb in range(4):
            for c in range(KC):
                P.op("pe", lambda e, c=c, nb=nb: e.matmul(PS[nb][:], lhsT=yt[b][:, c, :], rhs=Wb[:, c, nb * 512:(nb + 1) * 512], start=(c == 0), stop=(c == KC - 1)),
                     r=["yt%d" % b, "Wb%d" % c], w=["B%d" % nb])
        G1 = G1c if isctx else G1l
        for nb in range(4):
            sl = slice(nb * 512, (nb + 1) * 512)
            P.op("dve", lambda e, nb=nb, sl=sl: e.tensor_tensor(out=tmp[:, sl], in0=PS[nb][:], in1=G1[:, sl], op=ALU.mult), r=["G1l", "G1c"], w=["B%d" % nb, "tmp"])
            if layer == 1:
                P.op("dve", lambda e, sl=sl: e.scalar_tensor_tensor(out=xt[:, sl], in0=tmp[:, sl], scalar=st[:, 7:8], in1=xt[:, sl], op0=ALU.mult, op1=ALU.add), r=["tmp", "st7"], w=["xt"])
            else:
                P.op("pool", lambda e, sl=sl: e.tensor_tensor(out=xt[:, sl], in0=tmp[:, sl], in1=xt[:, sl], op=ALU.add), r=["tmp"], w=["xt"])
        P.dma("sp", x1o[i], xt[:], r=["xt"], w=["out:x1o%d" % i])
        P.op("act", lambda e: e.activation(out=junk[:], in_=xt[:], func=AF.Square, accum_out=st[:, 0:1]), r=["xt"], w=["junk", "st0"])
        P.op("dve", lambda e: e.tensor_scalar(out=st[:, 1:2], in0=st[:, 0:1], scalar1=1.0 / D, scalar2=EPS, op0=ALU.mult, op1=ALU.add), r=["st0"], w=["st1"])
        P.op("act", lambda e: e.activation(out=st[:, 2:3], in_=st[:, 1:2], func=AF.Sqrt), r=["st1"], w=["st2"])
        P.op("dve", lambda e: e.reciprocal(out=st[:, 3:4], in_=st[:, 2:3]), r=["st2"], w=["st3"])
        P.op("pool", lambda e: e.tensor_scalar(out=tmp[:], in0=xt[:], scalar1=st[:, 3:4], scalar2=0.0, op0=ALU.mult, op1=ALU.add), r=["xt", "st3"], w=["tmp"])
        A, Bv, keyA, keyB = (Ac if isctx else Al)
        for k4 in range(4):
            bank = PS[4 + k4]; bk = "B%d" % (4 + k4)
            for q in range(4):
                c = 4 * k4 + q
                P.op("pe", lambda e, c=c, q=q: e.transpose(out=bank[:, q * 128:(q + 1) * 128], in_=tmp[:, c * 128:(c + 1) * 128], identity=ident[:]), r=["tmp", "ident"], w=[bk])
            for q in range(4):
                c = 4 * k4 + q
                P.op("act", lambda e, c=c, q=q: e.activation(out=hq[q][:], in_=bank[:, q * 128:(q + 1) * 128], func=AF.Identity, scale=A[:, c:c + 1], bias=Bv[:, c:c + 1]),
                     r=[keyA, keyB], w=[bk, "hq%d" % q])
                P.op("pe", lambda e, c=c, q=q: e.matmul(PS[0][:, 0:32], lhsT=hq[q][:], rhs=rwt[:, c, :], start=(c == 0), stop=(c == 15)), r=["hq%d" % q, "rwt"], w=["B0"])
                P.op("dve", lambda e, c=c, q=q: e.tensor_copy(out=h2b[:, c, :], in_=hq[q][:]), r=["hq%d" % q], w=["h2b"])
        P.dma("sp", h2To[:, :, i * 128:(i + 1) * 128].rearrange("c p t -> p c t"), h2b[:], r=["h2b"], w=["out:h2T%d" % i])
        P.op("act", lambda e: e.activation(out=sc[:], in_=PS[0][:, 0:32], func=AF.Sigmoid), r=[], w=["B0", "sc"])
        P.op("dve", lambda e: e.tensor_tensor(out=sel[:], in0=sc[:], in1=rbb[:], op=ALU.add), r=["sc", "rbb"], w=["sel"])
        sel3 = sel[:].rearrange("p (g k) -> p g k", g=4); sel23 = sel2[:].rearrange("p (g k) -> p g k", g=4); eq3 = eq[:].rearrange("p (g k) -> p g k", g=4)
        P.op("dve", lambda e: e.tensor_reduce(out=m1[:], in_=sel3, axis=AX.X, op=ALU.max), r=["sel"], w=["m1"])
        P.op("dve", lambda e: e.tensor_tensor(out=eq3, in0=sel3, in1=m1[:].unsqueeze(2).to_broadcast([128, 4, 8]), op=ALU.is_equal), r=["sel", "m1"], w=["eq"])
        P.op("dve", lambda e: e.scalar_tensor_tensor(out=sel2[:], in0=eq[:], scalar=-1.0e9, in1=sel[:], op0=ALU.mult, op1=ALU.add), r=["eq", "sel"], w=["sel2"])
        P.op("dve", lambda e: e.tensor_reduce(out=m2[:], in_=sel23, axis=AX.X, op=ALU.max), r=["sel2"], w=["m2"])
        P.op("dve", lambda e: e.tensor_tensor(out=gsx[:], in0=m1[:], in1=m2[:], op=ALU.add), r=["m1", "m2"], w=["gsx"])
        P.op("dve", lambda e: e.tensor_reduce(out=gmax[:, 0:1], in_=gsx[:], axis=AX.X, op=ALU.max), r=["gsx"], w=["gmax"])
        P.op("dve", lambda e: e.tensor_scalar(out=gmask[:], in0=gsx[:], scalar1=gmax[:, 0:1], scalar2=None, op0=ALU.is_equal), r=["gsx", "gmax"], w=["gmask"])
        P.op("dve", lambda e: e.tensor_tensor(out=eq3, in0=sel3, in1=m2[:].unsqueeze(2).to_broadcast([128, 4, 8]), op=ALU.is_ge), r=["sel", "m2"], w=["eq"])
        P.op("dve", lambda e: e.tensor_tensor(out=eq3, in0=eq3, in1=gmask[:].unsqueeze(2).to_broadcast([128, 4, 8]), op=ALU.mult), r=["eq", "gmask"], w=["eq"])
        P.op("dve", lambda e: e.tensor_tensor(out=sel2[:], in0=eq[:], in1=sc[:], op=ALU.mult), r=["eq", "sc"], w=["sel2"])
        P.op("dve", lambda e: e.tensor_reduce(out=gmax[:, 1:2], in_=sel2[:], axis=AX.X, op=ALU.add), r=["sel2"], w=["gmax1"])
        P.op("dve", lambda e: e.reciprocal(out=gmax[:, 2:3], in_=gmax[:, 1:2]), r=["gmax1"], w=["gmax2"])
        P.op("dve", lambda e: e.tensor_scalar(out=gout[:], in0=sel2[:], scalar1=gmax[:, 2:3], scalar2=None, op0=ALU.mult), r=["sel2", "gmax2"], w=["gout"])
        P.dma("sp", gto[i], gout[:], r=["gout"], w=["out:gt%d" % i])
    return P


def tok_tiles(arr_c, arr_l, j, layer):
    F = arr_l.shape[-1]
    lat = arr_l[1024 * j:1024 * (j + 1)].reshape(8, 128, F)
    if layer == 0:
        t0 = np.zeros((1, 128, F), arr_l.dtype)
        t0[0, :32] = arr_c[32 * j:32 * (j + 1)]
        return np.ascontiguousarray(np.concatenate([t0, lat], 0))
    return np.ascontiguousarray(lat)


def featT_tiles(aT, j, layer):
    lat = aT[:, :, 256 + 1024 * j:256 + 1024 * (j + 1)]
    if layer == 0:
        t0 = np.zeros(aT.shape[:2] + (128,), aT.dtype)
        t0[:, :, :32] = aT[:, :, 32 * j:32 * (j + 1)]
        return np.ascontiguousarray(np.concatenate([t0, lat], -1))
    return np.ascontiguousarray(lat)


def untile(tiles_per_core, layer):
    if layer == 0:
        c = np.concatenate([t[0, :32] for t in tiles_per_core], 0)
        l = np.concatenate([t[1:].reshape(1024, -1) for t in tiles_per_core], 0)
        return c, l
    return None, np.concatenate([t.reshape(1024, -1) for t in tiles_per_core], 0)


def run_post(layer, x_c, x_l, preT, wout, modv_l, norm2_g, router_w, router_b, ssq_parts=None):
    P = build_post(layer)
    ident, _, _ = consts_np()
    embr, embc2 = pos_tables()
    KC = preT.shape[0] // 128
    aT = preT.reshape(KC, 128, T_ALL)
    base = {"wout": np.ascontiguousarray(wout, np.float32),
            "g1": np.ascontiguousarray(np.stack([modv_l[0, 4096:6144], modv_l[1, 4096:6144]])),
            "gn": feat_pc(norm2_g), "sh2l": feat_pc(modv_l[0, 6144:8192]), "sc2l": feat_pc(modv_l[0, 8192:10240]),
            "sh2c": feat_pc(modv_l[1, 6144:8192]), "sc2c": feat_pc(modv_l[1, 8192:10240]),
            "rw": np.ascontiguousarray(np.asarray(router_w, np.float32).reshape(16, 128, 32).transpose(1, 0, 2)),
            "rb": np.asarray(router_b, np.float32), "ident": ident}
    maps = []
    for j in range(NCORES):
        m = dict(base)
        m["xin"] = tok_tiles(x_c, x_l, j, layer)
        m["yT"] = featT_tiles(aT, j, layer)
        if layer == 0:
            m["embr"] = np.ascontiguousarray(embr[16 * j:16 * j + 16]); m["embc2"] = embc2
        else:
            m["ssqp"] = np.ascontiguousarray(ssq_parts[1024 * j:1024 * (j + 1)].reshape(8, 128, 8))
        maps.append(m)
    res = run_prog(P, maps)
    x1c, x1l = untile([r["x1o"] for r in res], layer)
    gc, gl = untile([r["gto"] for r in res], layer)
    if layer == 0:
        h2T = np.concatenate([r["h2To"][:, :, :32] for r in res] + [r["h2To"][:, :, 128:] for r in res], -1)
        gates = np.concatenate([gc, gl], 0)
    else:
        h2T = np.concatenate([r["h2To"] for r in res], -1)
        gates = gl
    return x1c, x1l, np.ascontiguousarray(h2T), gates


def build_moe(T):
    P = Prog()
    NT = T // 128
    FE = 1024
    h2T = P.din("h2T", [16, 128, T], BF16)
    gt = P.din("gt", [128, NT, 4])
    wg = P.din("wg", [4, D, FE]); wu = P.din("wu", [4, D, FE]); wd = P.din("wd", [4, FE, D])
    part = P.dout("part", [T, D])
    ye = [P.dscr("ye%d" % q, [T, D]) for q in range(4)]
    PS = [P.ps("PS%d" % i, [128, 512]) for i in range(8)]
    gtt = P.sb("gtt", [128, NT, 4]); P.dma("sp", gtt[:], gt[:, :, :], w=["gtt"])
    groups = [(t0, min(512, T - t0)) for t0 in range(0, T, 512)]
    with P.phase():
        Wg = P.sb("Wg", [128, 16, FE], BF16); Wu = P.sb("Wu", [128, 16, FE], BF16); Wd = P.sb("Wd", [128, 8, D], BF16)
        hg = [P.sb("hg%d" % b, [128, 16, 512], BF16) for b in range(2)]
        AT = P.sb("AT", [128, 8, 512], BF16)
        sgt = [P.sb("sgt%d" % b, [128, 512], BF16) for b in range(2)]
        Yt = [P.sb("Yt%d" % b, [128, D]) for b in range(2)]
        h2v = h2T.rearrange("c p t -> p c t")
        ycount = 0
        for e_ in range(4):
            wgv = wg[e_].rearrange("(c p) n -> p c n", p=128); wuv = wu[e_].rearrange("(c p) n -> p c n", p=128)
            wdv = wd[e_].rearrange("(c p) n -> p c n", p=128)
            for c in range(16):
                P.dma("pool", Wg[:, c, :], wgv[:, c, :], w=["Wg%d" % c])
                P.dma("pool", Wu[:, c, :], wuv[:, c, :], w=["Wu%d" % c])
            for c in range(8):
                P.dma("pool", Wd[:, c, :], wdv[:, c, :], w=["Wd%d" % c])

            def loadh(gi):
                t0, Lg = groups[gi]
                P.dma("sp", hg[gi % 2][:, :, :Lg], h2v[:, :, t0:t0 + Lg], w=["hg%d" % (gi % 2)])
            loadh(0)
            for gi, (t0, Lg) in enumerate(groups):
                b = gi % 2
                if gi + 1 < len(groups):
                    loadh(gi + 1)
                for fc in range(8):
                    pb = fc % 2
                    Gb, Ub = PS[2 * pb], PS[2 * pb + 1]
                    for c in range(16):
                        P.op("pe", lambda e, c=c: e.matmul(Gb[:, :Lg], lhsT=Wg[:, c, fc * 128:(fc + 1) * 128], rhs=hg[b][:, c, :Lg], start=(c == 0), stop=(c == 15)),
                             r=["hg%d" % b, "Wg%d" % c], w=["B%d" % (2 * pb)])
                    for c in range(16):
                        P.op("pe", lambda e, c=c: e.matmul(Ub[:, :Lg], lhsT=Wu[:, c, fc * 128:(fc + 1) * 128], rhs=hg[b][:, c, :Lg], start=(c == 0), stop=(c == 15)),
                             r=["hg%d" % b, "Wu%d" % c], w=["B%d" % (2 * pb + 1)])
                    P.op("act", lambda e: e.activation(out=sgt[pb][:, :Lg], in_=Gb[:, :Lg], func=AF.Silu), r=[], w=["B%d" % (2 * pb), "sgt%d" % pb])
                    P.op("dve", lambda e: e.tensor_tensor(out=AT[:, fc, :Lg], in0=Ub[:, :Lg], in1=sgt[pb][:, :Lg], op=ALU.mult), r=["sgt%d" % pb], w=["B%d" % (2 * pb + 1), "AT%d" % fc])
                for tt in range(Lg // 128):
                    tile = (t0 // 128) + tt
                    yb_ = Yt[ycount % 2]; yk = "Yt%d" % (ycount % 2); ycount += 1
                    for dmb in range(4):
                        bank = PS[4 + dmb]; bk = "B%d" % (4 + dmb)
                        for fc in range(8):
                            P.op("pe", lambda e, fc=fc: e.matmul(bank[:], lhsT=AT[:, fc, tt * 128:(tt + 1) * 128], rhs=Wd[:, fc, dmb * 512:(dmb + 1) * 512], start=(fc == 0), stop=(fc == 7)),
                                 r=["AT%d" % fc, "Wd%d" % fc], w=[bk])
                        sl = slice(dmb * 512, (dmb + 1) * 512)
                        if dmb % 2 == 0:
                            P.op("act", lambda e: e.activation(out=yb_[:, sl], in_=bank[:], func=AF.Copy, scale=gtt[:, tile, e_:e_ + 1]), r=["gtt"], w=[bk, yk + "_%d" % dmb])
                        else:
                            P.op("dve", lambda e: e.tensor_scalar(out=yb_[:, sl], in0=bank[:], scalar1=gtt[:, tile, e_:e_ + 1], scalar2=None, op0=ALU.mult), r=["gtt"], w=[bk, yk + "_%d" % dmb])
                    P.dma("sp", ye[e_][tile * 128:(tile + 1) * 128, :], yb_[:], r=[yk + "_%d" % q for q in range(4)], w=["d:ye%d_%d" % (e_, tile)], sk=yk)
    with P.phase():
        yin = [[P.sb("yin%d_%d" % (b, e_), [128, D]) for e_ in range(4)] for b in range(2)]

        def loady(tile):
            b = tile % 2
            for e_ in range(4):
                P.dma("sp" if e_ % 2 == 0 else "pool", yin[b][e_][:], ye[e_][tile * 128:(tile + 1) * 128, :], r=["d:ye%d_%d" % (e_, tile)], w=["yin%d_%d" % (b, e_)])
        loady(0)
        for tile in range(NT):
            b = tile % 2
            if tile + 1 < NT:
                loady(tile + 1)
            P.op("dve", lambda e: e.tensor_tensor(out=yin[b][0][:], in0=yin[b][0][:], in1=yin[b][1][:], op=ALU.add), r=["yin%d_1" % b], w=["yin%d_0" % b])
            P.op("pool", lambda e: e.tensor_tensor(out=yin[b][2][:], in0=yin[b][2][:], in1=yin[b][3][:], op=ALU.add), r=["yin%d_3" % b], w=["yin%d_2" % b])
            P.op("dve", lambda e: e.tensor_tensor(out=yin[b][0][:], in0=yin[b][0][:], in1=yin[b][2][:], op=ALU.add), r=["yin%d_2" % b], w=["yin%d_0" % b])
            P.dma("sp", part[tile * 128:(tile + 1) * 128, :], yin[b][0][:], r=["yin%d_0" % b], w=["out:part%d" % tile])
    return P


def run_moe(h2T, gates, wgate, wup, wdown):
    T = h2T.shape[-1]
    P = build_moe(T)
    maps = []
    for j in range(NCORES):
        g = np.ascontiguousarray(gates[:, 4 * j:4 * j + 4].reshape(T // 128, 128, 4).transpose(1, 0, 2))
        maps.append({"h2T": h2T, "gt": g, "wg": np.ascontiguousarray(wgate[4 * j:4 * j + 4]), "wu": np.ascontiguousarray(wup[4 * j:4 * j + 4]),
                     "wd": np.ascontiguousarray(wdown[4 * j:4 * j + 4])})
    res = run_prog(P, maps)
    return [r["part"] for r in res]


def build_combine(layer):
    P = Prog()
    NT = 9 if layer == 0 else 8
    x1 = P.din("x1", [NT, 128, D])
    parts = P.din("parts", [8, NT, 128, D])
    g2 = P.din("g2", [2, D])
    xo = P.dout("xo", [NT, 128, D])
    G2l = P.sb("G2l", [128, D]); P.dma("sp", G2l[:], g2[0].partition_broadcast(128), w=["G2l"])
    if layer == 0:
        G2c = P.sb("G2c", [128, D]); P.dma("sp", G2c[:], g2[1].partition_broadcast(128), w=["G2c"])
    else:
        fg = P.din("fg", [D])
        FG = P.sb("FG", [128, D]); P.dma("sp", FG[:], fg.partition_broadcast(128), w=["FG"])
        junk = P.sb("junk", [128, D], BF16); st = P.sb("st", [128, 4])
    pb = [P.sb("pb%d" % q, [128, D]) for q in range(8)]
    xt = [P.sb("xt%d" % b, [128, D]) for b in range(2)]
    for i in range(NT):
        b = i % 2
        P.dma("sp", xt[b][:], x1[i], w=["xt%d" % b])
        for q in range(8):
            P.dma("sp" if q % 2 == 0 else "pool", pb[q][:], parts[q, i], w=["pb%d" % q])
        for (a, c, eng) in ((0, 1, "dve"), (2, 3, "pool"), (4, 5, "dve"), (6, 7, "pool"), (0, 2, "dve"), (4, 6, "pool"), (0, 4, "dve")):
            P.op(eng, lambda e, a=a, c=c: e.tensor_tensor(out=pb[a][:], in0=pb[a][:], in1=pb[c][:], op=ALU.add), r=["pb%d" % c], w=["pb%d" % a])
        G2 = G2c if (layer == 0 and i == 0) else G2l
        P.op("dve", lambda e: e.tensor_tensor(out=pb[0][:], in0=pb[0][:], in1=G2[:], op=ALU.mult), r=["G2l", "G2c"], w=["pb0"])
        P.op("pool", lambda e: e.tensor_tensor(out=xt[b][:], in0=xt[b][:], in1=pb[0][:], op=ALU.add), r=["pb0"], w=["xt%d" % b])
        if layer == 1:
            P.op("act", lambda e: e.activation(out=junk[:], in_=xt[b][:], func=AF.Square, accum_out=st[:, 0:1]), r=["xt%d" % b], w=["junk", "st0"])
            P.op("dve", lambda e: e.tensor_scalar(out=st[:, 1:2], in0=st[:, 0:1], scalar1=1.0 / D, scalar2=EPS, op0=ALU.mult, op1=ALU.add), r=["st0"], w=["st1"])
            P.op("act", lambda e: e.activation(out=st[:, 2:3], in_=st[:, 1:2], func=AF.Sqrt), r=["st1"], w=["st2"])
            P.op("dve", lambda e: e.reciprocal(out=st[:, 3:4], in_=st[:, 2:3]), r=["st2"], w=["st3"])
            P.op("dve", lambda e: e.scalar_tensor_tensor(out=xt[b][:], in0=xt[b][:], scalar=st[:, 3:4], in1=FG[:], op0=ALU.mult, op1=ALU.mult), r=["st3", "FG"], w=["xt%d" % b])
        P.dma("sp", xo[i], xt[b][:], r=["xt%d" % b], w=["out:xo%d" % i])
    return P


def run_combine(layer, x1c, x1l, parts, modv_l, final_g=None):
    P = build_combine(layer)
    maps = []
    g2 = np.ascontiguousarray(np.stack([modv_l[0, 10240:12288], modv_l[1, 10240:12288]]))
    for j in range(NCORES):
        if layer == 0:
            pj = np.stack([tok_tiles(p[:256], p[256:], j, 0) for p in parts])
        else:
            pj = np.stack([tok_tiles(None, p, j, 1) for p in parts])
        m = {"x1": tok_tiles(x1c, x1l, j, layer), "parts": np.ascontiguousarray(pj), "g2": g2}
        if layer == 1:
            m["fg"] = np.asarray(final_g, np.float32)
        maps.append(m)
    res = run_prog(P, maps)
    return untile([r["xo"] for r in res], layer)


K5_COLS = 1296
XW = 8454


def build_k5():
    P = Prog()
    xl = P.din("xl", [SEQ, D]); xc = P.din("xc", [CTX, D])
    gn = P.din("gn", [128, 16])
    scl = P.din("scl", [128, 16]); shl = P.din("shl", [128, 16]); scc = P.din("scc", [128, 16]); shc = P.din("shc", [128, 16])
    win = P.din("win", [D, K5_COLS])
    convw = P.din("convw", [128, 6, 4]); convb = P.din("convb", [128, 6])
    alog = P.din("alog", [16]); dtb = P.din("dtb", [16]); dsk = P.din("dsk", [8]); ng = P.din("ng", [512])
    identd = P.din("ident", [128, 128]); trid = P.din("tri", [2, 128, 128]); strd = P.din("strict", [2, 128, 128])
    seld = P.din("sel", [8, 1024])
    yzo = P.dout("yzo", [SEQ, 512], BF16)
    ssqo = P.dout("ssqo", [SEQ, 1])
    hTd = P.dscr("hTd", [128, 16, XW], BF16)
    YF = P.dscr("YF", [SEQ, 512])

    PS = [P.ps("PS%d" % i, [128, 512]) for i in range(8)]
    ident = P.sb("ident", [128, 128]); P.dma("sp", ident[:], identd[:, :], w=["ident"])
    Wb = P.sb("Wb", [128, 16, K5_COLS], BF16)
    winv = win.rearrange("(c p) n -> p c n", p=128)
    for c in range(16):
        P.dma("pool", Wb[:, c, :], winv[:, c, :], w=["Wb%d" % c])
    with P.phase():
        zt = P.sb("zt", [128, 16, 4], BF16)
        P.op("dve", lambda e: e.memset(zt[:], 0.0), w=["zt"])
        for (a, b_) in ((0, 2), (258, 261), (8453, 8454)):
            P.dma("sp", hTd[:, :, a:b_], zt[:, :, 0:b_ - a], r=["zt"], w=["d:pad%d" % a], sk="zt", allow_slow_non_contiguous=True)
        Al = emit_AB(P, gn, scl, shl, "l")
        Ac = emit_AB(P, gn, scc, shc, "c")
        emit_hT(P, PS, NCH,
                src_fn=lambda n: (xc[n * 128:(n + 1) * 128, :] if n < 2 else xl[(n - 2) * 128:(n - 1) * 128, :]),
                pos_fn=lambda n: None, ab_fn=lambda n: (Ac if n < 2 else Al), embr=None, embc2_d=None, ident=ident,
                dst_fn=lambda n: hTd[:, :, xa_col(128 * n):xa_col(128 * n) + 128], dst_key_fn=lambda n: "d:hT%d" % n)
    YB = P.dscr("YB", [SEQ, 512])
    emit_ssd(P, PS, Wb, hTd, ident, trid, strd, seld, convw, convb, alog, dtb, dsk, ng, [YF, YB], yzo, ssqo)
    return P


def emit_ssd(P, PS, Wb, hTd, ident, trid, strd, seld, convw, convb, alog, dtb, dsk, ng, YS, yzo, ssqo):
    with P.phase():
        tri = P.sb("tri", [128, 2, 128]); stri = P.sb("stri", [128, 2, 128]); ones = P.sb("ones", [128, 128])
        for d in range(2):
            P.dma("sp", tri[:, d, :], trid[d], w=["tri%d" % d]); P.dma("sp", stri[:, d, :], strd[d], w=["stri%d" % d])
        P.op("dve", lambda e: e.memset(ones[:], 1.0), w=["ones"])
        SEL = P.sb("SEL", [8, 1024]); P.dma("sp", SEL[:], seld[:, :], w=["SEL"])
        cw = P.sb("cw", [128, 6, 4]); cbv = P.sb("cbv", [128, 6])
        P.dma("sp", cw[:], convw[:, :, :], w=["cw"]); P.dma("sp", cbv[:], convb[:, :], w=["cbv"])
        aneg = P.sb("aneg", [128, 16]); dtbb = P.sb("dtbb", [128, 16]); dskb = P.sb("dskb", [128, 8]); ngb = P.sb("ngb", [128, 512])
        P.dma("sp", aneg[:], alog.partition_broadcast(128), w=["aneg"]); P.dma("sp", dtbb[:], dtb.partition_broadcast(128), w=["dtbb"])
        P.dma("sp", dskb[:], dsk.partition_broadcast(128), w=["dskb"]); P.dma("sp", ngb[:], ng.partition_broadcast(128), w=["ngb"])
        P.op("act", lambda e: e.activation(out=aneg[:], in_=aneg[:], func=AF.Exp), r=[], w=["aneg"])
        P.op("dve", lambda e: e.tensor_scalar(out=aneg[:], in0=aneg[:], scalar1=-1.0, scalar2=None, op0=ALU.mult), r=[], w=["aneg"])

        def v3(t, h=8):
            return t[:].rearrange("p (h q) -> p h q", h=h)

        class R:
            pass
        RS = []
        for d in range(2):
            r = R(); s_ = "_%d" % d
            r.hw = [P.sb("hw%d" % b + s_, [128, 16, 131], BF16) for b in range(2)]
            r.X = P.sb("X" + s_, [128, 786]); r.U = P.sb("U" + s_, [128, 6, 128]); r.BCb = P.sb("BCb" + s_, [128, 2, 128], BF16)
            r.Btk = P.sb("Btk" + s_, [128, 128], BF16); r.xst = P.sb("xst" + s_, [128, 512]); r.sz = P.sb("sz" + s_, [128, 512])
            r.dtp = P.sb("dtp" + s_, [128, 8]); r.la = P.sb("la" + s_, [128, 8]); r.ecum = P.sb("ecum" + s_, [128, 8]); r.erest = P.sb("erest" + s_, [128, 8]); r.dec = P.sb("dec" + s_, [128, 8])
            r.ncT = P.sb("ncT" + s_, [8, 128]); r.cT = P.sb("cT" + s_, [8, 128]); r.BDc = P.sb("BDc" + s_, [8, 1024]); r.cbm = P.sb("cbm" + s_, [128, 128])
            r.dmin = P.sb("dmin" + s_, [128, 1024]); r.M = P.sb("M" + s_, [128, 8, 128], BF16)
            r.xdt32 = P.sb("xdt32" + s_, [128, 512]); r.xdtb = P.sb("xdtb" + s_, [128, 512], BF16); r.xdte = P.sb("xdte" + s_, [128, 512], BF16)
            r.yt = P.sb("yt" + s_, [128, 512]); r.yf = [P.sb("yf%d" % b + s_, [128, 512]) for b in range(2)]; r.tmpd = P.sb("tmpd" + s_, [128, 512])
            r.ST = P.sb("ST" + s_, [128, 512]); r.STb = P.sb("STb" + s_, [128, 512], BF16)
            r.yo = [P.sb("yo%d" % b + s_, [128, 512], BF16) for b in range(2)]; r.sq = [P.sb("sq%d" % b + s_, [128, 2]) for b in range(2)]
            r.junk = P.sb("junk" + s_, [128, 512], BF16)
            r.ctmp = P.sb("ctmp" + s_, [128, 6, 128])
            r.order = list(range(NCH)) if d == 0 else [1, 0] + list(range(NCH - 1, 1, -1))
            RS.append(r)
            P.op("dve", lambda e: e.memset(r.ST[:], 0.0), w=["ST" + s_])
            P.op("dve", lambda e: e.memset(r.STb[:], 0.0), w=["STb" + s_])

        def loadw(d, i):
            r = RS[d]; n = r.order[i]
            c0 = xa_col(128 * n) - 2
            P.dma("sp", r.hw[i % 2][:], hTd[:, :, c0:c0 + 131], r=["d:hT%d" % n], w=["hw%d_%d" % (i % 2, d)])

        def step(d, i):
            r = RS[d]; s_ = "_%d" % d; n = r.order[i]; b = i % 2
            K_ = lambda name: name + s_
            B_ = lambda k: "B%d" % (4 * d + k)
            PB = lambda k: PS[4 * d + k]
            if i + 1 < NCH:
                loadw(d, i + 1)
            h = r.hw[b]; hk = "hw%d_%d" % (b, d)
            lat = n >= 2
            row0 = (n - 2) * 128
            final = lat and ((n >= 34) if d == 0 else (n <= 33))
            for k in range(6):
                bank = PB(k // 3); o0 = (k % 3) * 131
                for c in range(16):
                    P.op("pe", lambda e, c=c: e.matmul(bank[:, o0:o0 + 131], lhsT=Wb[:, c, k * 128:(k + 1) * 128], rhs=h[:, c, :], start=(c == 0), stop=(c == 15)),
                         r=[hk, "Wb%d" % c], w=[B_(k // 3)])
            for c in range(16):
                P.op("pe", lambda e, c=c: e.matmul(PB(3)[:, 0:16], lhsT=h[:, c, 2:130], rhs=Wb[:, c, 1280:1296], start=(c == 0), stop=(c == 15)), r=[hk, "Wb%d" % c], w=[B_(3)])
            if final:
                for c in range(16):
                    P.op("pe", lambda e, c=c: e.matmul(PB(2)[:, 0:512], lhsT=h[:, c, 2:130], rhs=Wb[:, c, 768:1280], start=(c == 0), stop=(c == 15)), r=[hk, "Wb%d" % c], w=[B_(2)])
                P.op("act", lambda e: e.activation(out=r.sz[:], in_=PB(2)[:, 0:512], func=AF.Silu), r=[], w=[B_(2), K_("sz")])
            yield
            P.op("act", lambda e: e.activation(out=r.X[:, 0:393], in_=PB(0)[:, 0:393], func=AF.Copy), r=[], w=[B_(0), K_("X0")])
            P.op("act", lambda e: e.activation(out=r.X[:, 393:786], in_=PB(1)[:, 0:393], func=AF.Copy), r=[], w=[B_(1), K_("X1")])
            X3 = r.X[:].rearrange("p (k t) -> p k t", k=6)
            xks = [K_("X0"), K_("X1")]
            P.op("dve", lambda e: e.tensor_tensor(out=r.U[:], in0=X3[:, :, 0:128], in1=cw[:, :, 0:1].to_broadcast([128, 6, 128]), op=ALU.mult), r=xks + ["cw"], w=[K_("U")])
            for t_ in range(1, 4):
                P.op("pool", lambda e, t_=t_: e.tensor_tensor(out=r.ctmp[:], in0=X3[:, :, t_:t_ + 128], in1=cw[:, :, t_:t_ + 1].to_broadcast([128, 6, 128]), op=ALU.mult), r=xks + ["cw"], w=[K_("ctmp")])
                P.op("dve", lambda e: e.tensor_tensor(out=r.U[:], in0=r.U[:], in1=r.ctmp[:], op=ALU.add), r=[K_("ctmp")], w=[K_("U")])
            P.op("dve", lambda e: e.tensor_tensor(out=r.U[:], in0=r.U[:], in1=cbv[:].unsqueeze(2).to_broadcast([128, 6, 128]), op=ALU.add), r=["cbv"], w=[K_("U")])
            yield
            P.op("act", lambda e: e.activation(out=r.U[:], in_=r.U[:], func=AF.Silu), r=[], w=[K_("U")])
            yield
            for k in range(4):
                P.op("pe", lambda e, k=k: e.transpose(out=PB(0)[:, k * 128:(k + 1) * 128], in_=r.U[:, k, :], identity=ident[:]), r=[K_("U"), "ident"], w=[B_(0)])
            P.op("pe", lambda e: e.transpose(out=PB(3)[:, 320:448], in_=r.U[:, 4, :], identity=ident[:]), r=[K_("U"), "ident"], w=[B_(3)])
            P.op("pool", lambda e: e.tensor_copy(out=r.BCb[:], in_=r.U[:, 4:6, :]), r=[K_("U")], w=[K_("BCb")])
            P.op("act", lambda e: e.activation(out=r.xst[:], in_=PB(0)[:, 0:512], func=AF.Copy), r=[], w=[B_(0), K_("xst")])
            P.op("act", lambda e: e.activation(out=r.Btk[:], in_=PB(3)[:, 320:448], func=AF.Copy), r=[], w=[B_(3), K_("Btk")])
            yield
            P.op("dve", lambda e: e.tensor_tensor(out=r.dtp[:], in0=PB(3)[:, 8 * d:8 * d + 8], in1=dtbb[:, 8 * d:8 * d + 8], op=ALU.add), r=["dtbb"], w=[B_(3), K_("dtp")])
            P.op("act", lambda e: e.activation(out=r.dtp[:], in_=r.dtp[:], func=AF.Exp), r=[], w=[K_("dtp")])
            P.op("act", lambda e: e.activation(out=r.dtp[:], in_=r.dtp[:], func=AF.Ln, bias=1.0), r=[], w=[K_("dtp")])
            P.op("dve", lambda e: e.tensor_tensor(out=r.la[:], in0=r.dtp[:], in1=aneg[:, 8 * d:8 * d + 8], op=ALU.mult), r=[K_("dtp"), "aneg"], w=[K_("la")])
            P.op("pe", lambda e: e.matmul(PB(3)[:, 16:24], lhsT=tri[:, d, :], rhs=r.la[:], start=True, stop=True), r=[K_("la"), "tri%d" % d], w=[B_(3)])
            P.op("pe", lambda e: e.matmul(PB(3)[:, 24:32], lhsT=stri[:, d, :], rhs=r.la[:], start=True, stop=True), r=[K_("la"), "stri%d" % d], w=[B_(3)])
            P.op("pe", lambda e: e.matmul(PB(3)[:, 32:40], lhsT=ones[:], rhs=r.la[:], start=True, stop=True), r=[K_("la"), "ones"], w=[B_(3)])
            P.op("pe", lambda e: e.matmul(PB(3)[0:8, 64:192], lhsT=r.la[:], rhs=tri[:, d, :], start=True, stop=True), r=[K_("la"), "tri%d" % d], w=[B_(3)])
            P.op("pe", lambda e: e.matmul(PB(3)[:, 192:320], lhsT=r.BCb[:, 0, :], rhs=r.BCb[:, 1, :], start=True, stop=True), r=[K_("BCb")], w=[B_(3)])
            P.op("act", lambda e: e.activation(out=r.ecum[:], in_=PB(3)[:, 16:24], func=AF.Exp), r=[], w=[B_(3), K_("ecum")])
            P.op("act", lambda e: e.activation(out=r.erest[:], in_=PB(3)[:, 24:32], func=AF.Exp), r=[], w=[B_(3), K_("erest")])
            P.op("act", lambda e: e.activation(out=r.dec[:], in_=PB(3)[:, 32:40], func=AF.Exp), r=[], w=[B_(3), K_("dec")])
            P.op("act", lambda e: e.activation(out=r.cT[:], in_=PB(3)[0:8, 64:192], func=AF.Copy), r=[], w=[B_(3), K_("cT")])
            P.op("dve", lambda e: e.tensor_scalar(out=r.ncT[:], in0=r.cT[:], scalar1=-1.0, scalar2=None, op0=ALU.mult), r=[K_("cT")], w=[K_("ncT")])
            P.op("dve", lambda e: e.tensor_tensor(out=r.BDc[:].rearrange("p (h c) -> p h c", h=8), in0=SEL[:].rearrange("p (h c) -> p h c", h=8),
                                                  in1=r.cT[:].unsqueeze(1).to_broadcast([8, 8, 128]), op=ALU.mult), r=[K_("cT"), "SEL"], w=[K_("BDc")])
            P.op("dve", lambda e: e.tensor_tensor(out=r.cbm[:], in0=PB(3)[:, 192:320], in1=tri[:, d, :], op=ALU.mult), r=["tri%d" % d], w=[B_(3), K_("cbm")])
            yield
            for hf in range(2):
                P.op("pe", lambda e: e.matmul(PB(1 + hf)[:, 0:512], lhsT=r.ncT[:], rhs=SEL[:, hf * 512:(hf + 1) * 512], start=True, stop=False), r=[K_("ncT"), "SEL"], w=[B_(1 + hf)])
                P.op("pe", lambda e: e.matmul(PB(1 + hf)[:, 0:512], lhsT=ones[0:8, :], rhs=r.BDc[:, hf * 512:(hf + 1) * 512], start=False, stop=True), r=["ones", K_("BDc")], w=[B_(1 + hf)])
                P.op("dve", lambda e: e.tensor_scalar(out=r.dmin[:, hf * 512:(hf + 1) * 512], in0=PB(1 + hf)[:, 0:512], scalar1=0.0, scalar2=None, op0=ALU.min), r=[], w=[B_(1 + hf), K_("dmin")])
            P.op("act", lambda e: e.activation(out=r.dmin[:], in_=r.dmin[:], func=AF.Exp), r=[], w=[K_("dmin")])
            P.op("dve", lambda e: e.tensor_tensor(out=r.M[:], in0=r.dmin[:].rearrange("p (h c) -> p h c", h=8), in1=r.cbm[:].unsqueeze(1).to_broadcast([128, 8, 128]), op=ALU.mult),
                 r=[K_("dmin"), K_("cbm")], w=[K_("M")])
            P.op("dve", lambda e: e.tensor_tensor(out=v3(r.xdt32), in0=v3(r.xst), in1=r.dtp[:].unsqueeze(2).to_broadcast([128, 8, 64]), op=ALU.mult), r=[K_("xst"), K_("dtp")], w=[K_("xdt32")])
            P.op("pool", lambda e: e.tensor_copy(out=r.xdtb[:], in_=r.xdt32[:]), r=[K_("xdt32")], w=[K_("xdtb")])
            P.op("dve", lambda e: e.tensor_tensor(out=v3(r.xdte), in0=v3(r.xdt32), in1=r.erest[:].unsqueeze(2).to_broadcast([128, 8, 64]), op=ALU.mult), r=[K_("xdt32"), K_("erest")], w=[K_("xdte")])
            yield
            for hh in range(8):
                P.op("pe", lambda e, hh=hh: e.matmul(PB(0)[:, hh * 64:(hh + 1) * 64], lhsT=r.M[:, hh, :], rhs=r.xdtb[:, hh * 64:(hh + 1) * 64], start=True, stop=True), r=[K_("M"), K_("xdtb")], w=[B_(0)])
            P.op("pe", lambda e: e.matmul(PB(1)[:, 0:512], lhsT=r.BCb[:, 1, :], rhs=r.STb[:], start=True, stop=True), r=[K_("BCb"), K_("STb")], w=[B_(1)])
            P.op("pe", lambda e: e.matmul(PB(2)[:, 0:512], lhsT=r.Btk[:], rhs=r.xdte[:], start=True, stop=True), r=[K_("Btk"), K_("xdte")], w=[B_(2)])
            yield
            if final:
                assert ("d:Y%d_%d" % (1 - d, n)) in P.lastw
                P.dma("pool", r.yf[b][:], YS[1 - d][row0:row0 + 128, :], r=["d:Y%d_%d" % (1 - d, n)], w=[K_("yf%d" % b)])
            P.op("dve", lambda e: e.tensor_tensor(out=v3(r.yt), in0=PB(1)[:, 0:512].rearrange("p (h q) -> p h q", h=8), in1=r.ecum[:].unsqueeze(2).to_broadcast([128, 8, 64]), op=ALU.mult),
                 r=[K_("ecum")], w=[B_(1), K_("yt")])
            P.op("dve", lambda e: e.tensor_tensor(out=r.yt[:], in0=PB(0)[:, 0:512], in1=r.yt[:], op=ALU.add), r=[], w=[B_(0), K_("yt")])
            P.op("dve", lambda e: e.tensor_tensor(out=v3(r.ST), in0=v3(r.ST), in1=r.dec[:].unsqueeze(2).to_broadcast([128, 8, 64]), op=ALU.mult), r=[K_("dec")], w=[K_("ST")])
            P.op("dve", lambda e: e.tensor_tensor(out=r.ST[:], in0=PB(2)[:, 0:512], in1=r.ST[:], op=ALU.add), r=[], w=[B_(2), K_("ST")])
            P.op("pool", lambda e: e.tensor_copy(out=r.STb[:], in_=r.ST[:]), r=[K_("ST")], w=[K_("STb")])
            if not lat:
                return
            if not final:
                P.dma("sp", YS[d][row0:row0 + 128, :], r.yt[:], r=[K_("yt")], w=["d:Y%d_%d" % (d, n)])
            else:
                P.op("pool", lambda e: e.tensor_tensor(out=r.yt[:], in0=r.yt[:], in1=r.yf[b][:], op=ALU.add), r=[K_("yf%d" % b)], w=[K_("yt")])
                P.op("dve", lambda e: e.tensor_tensor(out=v3(r.tmpd), in0=v3(r.xst), in1=dskb[:].unsqueeze(2).to_broadcast([128, 8, 64]), op=ALU.mult), r=[K_("xst"), "dskb"], w=[K_("tmpd")])
                P.op("pool", lambda e: e.tensor_tensor(out=r.yt[:], in0=r.yt[:], in1=r.tmpd[:], op=ALU.add), r=[K_("tmpd")], w=[K_("yt")])
                P.op("dve", lambda e: e.tensor_tensor(out=r.yt[:], in0=r.yt[:], in1=r.sz[:], op=ALU.mult), r=[K_("sz")], w=[K_("yt")])
                P.op("act", lambda e: e.activation(out=r.junk[:], in_=r.yt[:], func=AF.Square, accum_out=r.sq[b][:, 0:1]), r=[K_("yt")], w=[K_("junk"), K_("sq%d" % b)])
                P.op("dve", lambda e: e.tensor_tensor(out=r.yo[b][:], in0=r.yt[:], in1=ngb[:], op=ALU.mult), r=[K_("yt"), "ngb"], w=[K_("yo%d" % b)])
                P.dma("sp", yzo[row0:row0 + 128, :], r.yo[b][:], r=[K_("yo%d" % b)], w=["out:yz%d" % n])
                P.dma("sp", ssqo[row0:row0 + 128, :], r.sq[b][:, 0:1], r=[K_("sq%d" % b)], w=["out:sq%d" % n])

        loadw(0, 0); loadw(1, 0)

        def chain(d):
            for i in range(NCH):
                yield from step(d, i)
        ga, gb = chain(0), chain(1)
        for _ in range(3):
            next(ga)
        alive = [ga, gb]
        while alive:
            for g_ in list(alive):
                try:
                    next(g_)
                except StopIteration:
                    alive.remove(g_)


def k5_inputs(x2c, x2l, modv1, norm1_g1, od_w_in, od_conv_w, od_conv_b, ssd_a_log, ssd_dt_bias, ssd_d, ssd_norm_g):
    ident, tri, strict = consts_np()
    sel = np.zeros((8, 8, 128), np.float32)
    for hh in range(8):
        sel[hh, hh, :] = 1.0
    base = {"xl": np.ascontiguousarray(x2l), "xc": np.ascontiguousarray(x2c), "gn": feat_pc(norm1_g1),
            "shl": feat_pc(modv1[0, 0:2048]), "scl": feat_pc(modv1[0, 2048:4096]),
            "shc": feat_pc(modv1[1, 0:2048]), "scc": feat_pc(modv1[1, 2048:4096]),
            "ident": ident, "tri": tri, "strict": strict, "sel": sel.reshape(8, 1024)}
    W = od_w_in[0]; cwf = od_conv_w[0]; cbf = od_conv_b[0]
    maps = []
    for j in range(NCORES):
        cols = np.concatenate([4096 + 512 * j + np.arange(512), 8192 + 128 * j + np.arange(128), 9216 + 128 * j + np.arange(128),
                               512 * j + np.arange(512), 10240 + 8 * j + np.arange(8), 10304 + 8 * j + np.arange(8)])
        ch = np.stack([512 * j + 128 * k + np.arange(128) for k in range(4)] + [4096 + 128 * j + np.arange(128), 5120 + 128 * j + np.arange(128)], 1)
        m = dict(base)
        m.update({"win": np.ascontiguousarray(W[:, cols]),
                  "convw": np.ascontiguousarray(cwf[:, ch].transpose(1, 2, 0)), "convb": np.ascontiguousarray(cbf[ch]),
                  "alog": np.concatenate([ssd_a_log[0][0, 8 * j:8 * j + 8], ssd_a_log[0][1, 8 * j:8 * j + 8]]).astype(np.float32),
                  "dtb": np.concatenate([ssd_dt_bias[0][0, 8 * j:8 * j + 8], ssd_dt_bias[0][1, 8 * j:8 * j + 8]]).astype(np.float32),
                  "dsk": np.ascontiguousarray(ssd_d[0][8 * j:8 * j + 8]), "ng": np.ascontiguousarray(ssd_norm_g[0][512 * j:512 * j + 512])})
        maps.append(m)
    return maps


def run_k5(maps):
    P = build_k5()
    res = run_prog(P, maps)
    pre1T = np.zeros((4096, T_ALL), NPBF)
    for j, r in enumerate(res):
        pre1T[512 * j:512 * (j + 1), 256:] = r["yzo"].T
    ssq = np.concatenate([r["ssqo"] for r in res], 1)
    return pre1T, np.ascontiguousarray(ssq)


def kernel(x, c, ctx, c_ctx, mod_w, mod_b, norm1_g, norm2_g, ev_w_in, ev_conv_w, ev_conv_b, lru_wa, lru_ba, lru_wx, lru_bx,
           lru_lambda, gla_wg_up, gla_bg, gla_norm_g, ev_w_out, od_w_in, od_conv_w, od_conv_b, ssd_a_log, ssd_dt_bias, ssd_d,
           ssd_norm_g, od_w_out, router_w, router_b, exp_w_gate, exp_w_up, exp_w_down, final_norm_g):
    f = lambda a: np.asarray(a, np.float32)
    x = f(x); ctx = f(ctx)
    modv = run_k0(f(c), f(c_ctx), f(mod_w), f(mod_b))
    maps = k1_inputs(x, ctx, modv[0], f(norm1_g)[0], f(ev_w_in), f(ev_conv_w), f(ev_conv_b), f(lru_wa), f(lru_ba), f(lru_wx), f(lru_bx),
                     f(lru_lambda), f(gla_wg_up), f(gla_bg), f(gla_norm_g))
    preT = run_k1(maps)
    del maps
    x1c, x1l, h2T, gates = run_post(0, ctx[0], x[0], preT, f(ev_w_out)[0], modv[0], f(norm2_g)[0], f(router_w), f(router_b))
    parts = run_moe(h2T, gates, f(exp_w_gate)[0], f(exp_w_up)[0], f(exp_w_down)[0])
    x2c, x2l = run_combine(0, x1c, x1l, parts, modv[0])
    del parts, preT, h2T
    maps = k5_inputs(x2c, x2l, modv[1], f(norm1_g)[1], f(od_w_in), f(od_conv_w), f(od_conv_b), f(ssd_a_log), f(ssd_dt_bias), f(ssd_d), f(ssd_norm_g))
    pre1T, ssq = run_k5(maps)
    del maps
    _, x3l, h2T, gates = run_post(1, None, x2l, pre1T, f(od_w_out)[0], modv[1], f(norm2_g)[1], f(router_w), f(router_b), ssq_parts=ssq)
    parts = run_moe(h2T, gates, f(exp_w_gate)[1], f(exp_w_up)[1], f(exp_w_down)[1])
    _, out = run_combine(1, None, x3l, parts, modv[1], final_g=f(final_norm_g))
    return out.reshape(1, SEQ, D).astype(np.float32)
```

```python
import contextlib
import math
import numpy as np
import ml_dtypes
import concourse.bass as bass
import concourse.mybir as mybir
from concourse.bass_utils import run_bass_kernel_spmd

F32 = mybir.dt.float32
BF16 = mybir.dt.bfloat16
I32 = mybir.dt.int32
AF = mybir.ActivationFunctionType
ALU = mybir.AluOpType
AX = mybir.AxisListType
NPBF = ml_dtypes.bfloat16

NCORES = 8
D = 2048
SEQ = 8192
CTX = 256
T_ALL = SEQ + CTX
NCH = T_ALL // 128
EPS = 1e-6

import os
SAME_ENGINE_SYNC = os.environ.get("SES", "1") == "1"


class Prog:
    def __init__(self):
        self.nc = bass.Bass("TRN2", target_bir_lowering=False)
        nc = self.nc
        self.es = contextlib.ExitStack()
        self.eng = {"pe": nc.tensor, "act": nc.scalar, "dve": nc.vector, "pool": nc.gpsimd, "sp": nc.sync}
        self.esem = {}
        self.ecnt = {}
        self.seen = {}
        for e in self.eng:
            self.esem[e] = self.es.enter_context(nc.semaphore("sem_" + e))
            self.ecnt[e] = 0
            self.seen[e] = {}
        self.lastw = {}
        self.readers = {}
        self.dsem = {}
        self.out_tokens = []
        self.nuniq = 0

    def _uid(self):
        self.nuid = getattr(self, 'nuid', 0) + 1
        return self.nuid

    def din(self, name, shape, dt=F32):
        return self.nc.dram_tensor(name, list(shape), dt, kind="ExternalInput").ap()

    def dout(self, name, shape, dt=F32):
        return self.nc.dram_tensor(name, list(shape), dt, kind="ExternalOutput").ap()

    def dscr(self, name, shape, dt=F32):
        return self.nc.dram_tensor(name, list(shape), dt, kind="Internal").ap()

    def sb(self, name, shape, dt=F32):
        return self.es.enter_context(self.nc.sbuf_tensor("s%d_" % self._uid() + name, list(shape), dt))

    def ps(self, name, shape, dt=F32):
        return self.es.enter_context(self.nc.psum_tensor("p%d_" % self._uid() + name, list(shape), dt))

    def _waits(self, e, r, w):
        toks = []
        for k in r:
            t = self.lastw.get(k)
            if t is not None:
                toks.append(t)
        for k in w:
            t = self.lastw.get(k)
            if t is not None:
                toks.append(t)
            toks.extend(self.readers.get(k, ()))
        eng = self.eng[e]
        best = {}
        for (sem, val, sid, owner) in toks:
            if owner == e and (e == "pe" or not SAME_ENGINE_SYNC):
                continue
            if self.seen[e].get(sid, 0) >= val:
                continue
            if sid not in best or best[sid][1] < val:
                best[sid] = (sem, val)
        for sid, (sem, val) in best.items():
            eng.wait_ge(sem, val)
            self.seen[e][sid] = val

    def _update(self, r, w, tok):
        for k in r:
            self.readers.setdefault(k, []).append(tok)
        for k in w:
            self.lastw[k] = tok
            self.readers[k] = []

    def op(self, e, fn, r=(), w=()):
        self._waits(e, r, w)
        inst = fn(self.eng[e])
        self.ecnt[e] += 1
        inst.then_inc(self.esem[e], 1)
        tok = (self.esem[e], self.ecnt[e], "E" + e, e)
        self._update(r, w, tok)
        return tok

    def dma(self, q, out, in_, r=(), w=(), sk=None, **kw):
        if sk is None:
            sk = (w[0] if (w and not str(w[0]).startswith("out:") and not str(w[0]).startswith("d:")) else r[0])
        self._waits(q, r, w)
        if sk not in self.dsem:
            self.nuniq += 1
            self.dsem[sk] = [self.es.enter_context(self.nc.semaphore("dq%d" % self.nuniq)), 0]
        ent = self.dsem[sk]
        ent[1] += 16
        self.eng[q].dma_start(out=out, in_=in_, **kw).then_inc(ent[0], 16)
        tok = (ent[0], ent[1], "D" + str(sk), None)
        self._update(r, w, tok)
        for k in w:
            if str(k).startswith("out:"):
                self.out_tokens.append(tok)
        return tok

    def finish(self):
        eng = self.eng["sp"]
        best = {}
        for (sem, val, sid, owner) in self.out_tokens:
            if sid not in best or best[sid][1] < val:
                best[sid] = (sem, val)
        for sid, (sem, val) in best.items():
            eng.wait_ge(sem, val)
        self.es.close()
        return self.nc


def run_prog(P, in_maps):
    nc = P.finish()
    res = run_bass_kernel_spmd(nc, in_maps, core_ids=list(range(NCORES)))
    return res.results


def feat_pc(v):
    return np.ascontiguousarray(np.asarray(v, np.float32).reshape(-1, 128).T)


def build_k0():
    P = Prog()
    NC_ = 1536
    cvT = P.din("cvT", [128, 16, 2])
    w = P.din("w", [2, D, NC_])
    b = P.din("b", [2, NC_])
    o = P.dout("o", [2, 2, NC_])
    s_raw = P.sb("s_raw", [128, 16, 2])
    s = P.sb("s", [128, 16, 2])
    W = P.sb("W", [128, 16, NC_])
    bt = P.sb("bt", [2, 2, NC_])
    ot = P.sb("ot", [2, 2, NC_])
    pss = [P.ps("ps%d" % i, [2, 512]) for i in range(3)]
    P.dma("sp", s_raw[:], cvT[:, :, :], w=["s_raw"])
    P.op("act", lambda e: e.activation(out=s[:], in_=s_raw[:], func=AF.Silu), r=["s_raw"], w=["s"])
    for i in range(2):
        P.dma("sp", bt[:, i, :], b[i].partition_broadcast(2), w=["bt%d" % i])
    for i in range(2):
        wv = w[i].rearrange("(c p) n -> p c n", p=128)
        for g in range(4):
            P.dma("sp" if g % 2 == 0 else "pool", W[:, 4 * g:4 * g + 4, :], wv[:, 4 * g:4 * g + 4, :], w=["W%d" % g])
        for nb in range(3):
            for c in range(16):
                P.op("pe", lambda e, c=c, nb=nb: e.matmul(pss[nb][:], lhsT=s[:, c, :], rhs=W[:, c, nb * 512:(nb + 1) * 512],
                                                          start=(c == 0), stop=(c == 15)),
                     r=["s", "W%d" % (c // 4)], w=["ps%d" % nb])
            P.op("dve", lambda e, nb=nb, i=i: e.tensor_tensor(out=ot[:, i, nb * 512:(nb + 1) * 512], in0=pss[nb][:],
                                                              in1=bt[:, i, nb * 512:(nb + 1) * 512], op=ALU.add),
                 r=["ps%d" % nb, "bt%d" % i], w=["ot%d_%d" % (i, nb)])
    for i in range(2):
        P.dma("sp", o[i], ot[:, i, :], r=["ot%d_%d" % (i, nb) for nb in range(3)], w=["out:o%d" % i], sk="ot%d" % i)
    return P


def run_k0(c, c_ctx, mod_w, mod_b):
    P = build_k0()
    cv = np.stack([np.asarray(c, np.float32).reshape(-1), np.asarray(c_ctx, np.float32).reshape(-1)], -1)
    cvT = np.ascontiguousarray(cv.reshape(16, 128, 2).transpose(1, 0, 2))
    maps = []
    for j in range(NCORES):
        sl = slice(j * 1536, (j + 1) * 1536)
        maps.append({"cvT": cvT, "w": np.ascontiguousarray(mod_w[:, :, sl]), "b": np.ascontiguousarray(mod_b[:, sl])})
    res = run_prog(P, maps)
    return np.concatenate([r["o"] for r in res], axis=-1)


def _barrier(self):
    for e, eng in self.eng.items():
        for e2 in self.eng:
            if e2 == e or self.ecnt[e2] == 0:
                continue
            if self.seen[e].get("E" + e2, 0) < self.ecnt[e2]:
                eng.wait_ge(self.esem[e2], self.ecnt[e2])
                self.seen[e]["E" + e2] = self.ecnt[e2]
        for sk, (sem, cnt) in self.dsem.items():
            if cnt and self.seen[e].get("D" + str(sk), 0) < cnt:
                eng.wait_ge(sem, cnt)
                self.seen[e]["D" + str(sk)] = cnt


@contextlib.contextmanager
def _phase(self):
    old = self.es
    st = contextlib.ExitStack()
    self.es_sems = old
    self.es = st
    try:
        yield
    finally:
        self.barrier()
        self.es = old
        st.close()


Prog.barrier = _barrier
Prog.phase = _phase


def _dma2(self, q, out, in_, r=(), w=(), sk=None, **kw):
    cur = self.es
    self.es = self.root_es
    try:
        return Prog._dma_orig(self, q, out, in_, r=r, w=w, sk=sk, **kw)
    finally:
        self.es = cur


Prog._dma_orig = Prog.dma
Prog.dma = _dma2
_old_init = Prog.__init__


def _init2(self):
    _old_init(self)
    self.root_es = self.es


Prog.__init__ = _init2
_old_finish = Prog.finish


def _finish2(self):
    self.es = self.root_es
    return _old_finish(self)


Prog.finish = _finish2


def consts_np():
    s = np.arange(128)[:, None]
    c = np.arange(128)[None, :]
    ident = np.eye(128, dtype=np.float32)
    tri = np.stack([(s <= c), (s >= c)]).astype(np.float32)
    strict = np.stack([(s > c), (s < c)]).astype(np.float32)
    return ident, tri, strict


def pos_tables():
    quarter = D // 4
    omega = (1.0 / (10000.0 ** (np.arange(quarter, dtype=np.float32) / np.float32(quarter)))).astype(np.float32)
    ang_r = np.arange(128, dtype=np.float32)[:, None] * omega
    ang_c = np.arange(64, dtype=np.float32)[:, None] * omega
    emb_r = np.concatenate([np.sin(ang_r), np.cos(ang_r)], -1).astype(np.float32)
    emb_c = np.concatenate([np.sin(ang_c), np.cos(ang_c)], -1).astype(np.float32)
    return emb_r, np.concatenate([emb_c, emb_c], 0)


def emit_hT(P, PS, n_chunks, src_fn, pos_fn, ab_fn, embr, embc2_d, ident, dst_fn, dst_key_fn, pfx="h"):
    xt = [P.sb(pfx + "xt%d" % b, [128, D]) for b in range(2)]
    pr = [P.sb(pfx + "pr%d" % b, [128, 1024]) for b in range(2)]
    pc = P.sb(pfx + "pc", [128, 1024])
    junk = P.sb(pfx + "junk", [128, D], BF16)
    xn = P.sb(pfx + "xn", [128, D])
    st = P.sb(pfx + "st", [128, 4])
    hTt = [P.sb(pfx + "hTo%d" % b, [128, 16, 128], BF16) for b in range(2)]
    if embc2_d is not None:
        P.dma("sp", pc[:], embc2_d[:, :], w=[pfx + "pc"])

    def load(n):
        b = n % 2
        src = src_fn(n)
        P.dma("sp", xt[b][:, 0:1024], src[:, 0:1024], w=[pfx + "xt%da" % b])
        P.dma("pool", xt[b][:, 1024:2048], src[:, 1024:2048], w=[pfx + "xt%db" % b])
        r0 = pos_fn(n)
        if r0 is not None:
            P.dma("sp", pr[b][0:64, :], embr[r0].partition_broadcast(64), w=[pfx + "pr%d" % b], sk=pfx + "pr%dlo" % b)
            P.dma("sp", pr[b][64:128, :], embr[r0 + 1].partition_broadcast(64), w=[pfx + "pr%dh" % b], sk=pfx + "pr%dhi" % b)

    import os
    dbg = int(os.environ.get("HTDBG", "99"))
    if dbg < 99:
        n_chunks = 3
    load(0)
    for n in range(n_chunks):
        b = n % 2
        if n + 1 < n_chunks:
            load(n + 1)
        ka, kb = pfx + "xt%da" % b, pfx + "xt%db" % b
        if pos_fn(n) is not None:
            P.op("pool", lambda e: e.tensor_tensor(out=xt[b][:, 0:1024], in0=xt[b][:, 0:1024], in1=pr[b][:], op=ALU.add),
                 r=[ka, pfx + "pr%d" % b, pfx + "pr%dh" % b], w=[ka])
            P.op("dve", lambda e: e.tensor_tensor(out=xt[b][:, 1024:2048], in0=xt[b][:, 1024:2048], in1=pc[:], op=ALU.add),
                 r=[kb, pfx + "pc"], w=[kb])
        if dbg < 1:
            continue
        P.op("act", lambda e: e.activation(out=junk[:], in_=xt[b][:], func=AF.Square, accum_out=st[:, 0:1]),
             r=[ka, kb], w=[pfx + "junk", pfx + "st0"])
        P.op("dve", lambda e: e.tensor_scalar(out=st[:, 1:2], in0=st[:, 0:1], scalar1=1.0 / D, scalar2=EPS, op0=ALU.mult, op1=ALU.add),
             r=[pfx + "st0"], w=[pfx + "st1"])
        P.op("act", lambda e: e.activation(out=st[:, 2:3], in_=st[:, 1:2], func=AF.Sqrt), r=[pfx + "st1"], w=[pfx + "st2"])
        P.op("dve", lambda e: e.reciprocal(out=st[:, 3:4], in_=st[:, 2:3]), r=[pfx + "st2"], w=[pfx + "st3"])
        P.op("pool", lambda e: e.tensor_scalar(out=xn[:], in0=xt[b][:], scalar1=st[:, 3:4], scalar2=0.0, op0=ALU.mult, op1=ALU.add),
             r=[ka, kb, pfx + "st3"], w=[pfx + "xn"])
        if dbg < 2:
            continue
        A, Bv, keyA, keyB = ab_fn(n)
        for k4 in range(4):
            bank = PS[k4]
            bk = "B%d" % k4
            for q in range(4):
                c = 4 * k4 + q
                P.op("pe", lambda e, c=c, q=q: e.transpose(out=bank[:, q * 128:(q + 1) * 128], in_=xn[:, c * 128:(c + 1) * 128], identity=ident[:]),
                     r=[pfx + "xn", "ident"], w=[bk])
            for q in range(4):
                c = 4 * k4 + q
                if k4 % 2 == 0:
                    P.op("act", lambda e, c=c, q=q: e.activation(out=hTt[b][:, c, :], in_=bank[:, q * 128:(q + 1) * 128], func=AF.Identity,
                                                                   scale=A[:, c:c + 1], bias=Bv[:, c:c + 1]),
                         r=[keyA, keyB], w=[bk, pfx + "hTo%d_%d" % (b, c)])
                else:
                    P.op("dve", lambda e, c=c, q=q: e.tensor_scalar(out=hTt[b][:, c, :], in0=bank[:, q * 128:(q + 1) * 128],
                                                                      scalar1=A[:, c:c + 1], scalar2=Bv[:, c:c + 1], op0=ALU.mult, op1=ALU.add),
                         r=[keyA, keyB], w=[bk, pfx + "hTo%d_%d" % (b, c)])
        if dbg < 3:
            continue
        P.dma("sp", dst_fn(n), hTt[b][:], r=[pfx + "hTo%d_%d" % (b, c) for c in range(16)], w=[dst_key_fn(n)], sk=pfx + "hTo%d" % b)


def emit_AB(P, gn_d, sc_d, sh_d, name):
    g = P.sb(name + "g", [128, 16])
    sc = P.sb(name + "sc", [128, 16])
    A = P.sb(name + "A", [128, 16])
    Bv = P.sb(name + "B", [128, 16])
    P.dma("sp", g[:], gn_d[:, :], w=[name + "g"])
    P.dma("sp", sc[:], sc_d[:, :], w=[name + "sc"])
    P.dma("sp", Bv[:], sh_d[:, :], w=[name + "B"])
    P.op("dve", lambda e: e.scalar_tensor_tensor(out=A[:], in0=sc[:], scalar=1.0, in1=g[:], op0=ALU.add, op1=ALU.mult),
         r=[name + "g", name + "sc"], w=[name + "A"])
    return A, Bv, name + "A", name + "B"


K1_COLS = 736
LSEGS = [(0, 256)] + [(256 + 512 * k, 512) for k in range(16)]


def xa_col(t):
    return 2 + t if t < 256 else 261 + (t - 256)


def build_k1(stop=9):
    P = Prog()
    xl = P.din("xl", [SEQ, D])
    xc = P.din("xc", [CTX, D])
    embr = P.din("embr", [128, 1024])
    embc2 = P.din("embc2", [128, 1024])
    gn = P.din("gn", [128, 16])
    scl = P.din("scl", [128, 16]); shl = P.din("shl", [128, 16])
    scc = P.din("scc", [128, 16]); shc = P.din("shc", [128, 16])
    win = P.din("win", [D, K1_COLS])
    convw = P.din("convw", [128, 4]); convb = P.din("convb", [128, 1])
    bd = P.din("bd", [4, 128, 128])
    gb = P.din("gb", [128, 4])
    lam = P.din("lam", [128, 2])
    wg = P.din("wg", [2, 16, 64]); bg = P.din("bg", [2, 64])
    gng = P.din("gng", [128])
    identd = P.din("ident", [128, 128]); trid = P.din("tri", [2, 128, 128]); strd = P.din("strict", [2, 128, 128])
    yaT = P.dout("yaT", [128, T_ALL], BF16)
    yb = P.dout("yb", [T_ALL, 128], BF16)
    hTd = P.dscr("hTd", [NCH, 128, 16, 128], BF16)

    PS = [P.ps("PS%d" % i, [128, 512]) for i in range(8)]
    ident = P.sb("ident", [128, 128])
    P.dma("sp", ident[:], identd[:, :], w=["ident"])
    Wb = P.sb("Wb", [128, 16, K1_COLS], BF16)
    winv = win.rearrange("(c p) n -> p c n", p=128)
    for g4 in range(4):
        P.dma("pool", Wb[:, 4 * g4:4 * g4 + 4, :], winv[:, 4 * g4:4 * g4 + 4, :], w=["Wb%d" % g4])
    WK = ["Wb%d" % g4 for g4 in range(4)]

    with P.phase():
        Al = emit_AB(P, gn, scl, shl, "l")
        Ac = emit_AB(P, gn, scc, shc, "c")
        emit_hT(P, PS, NCH,
                src_fn=lambda n: (xc[n * 128:(n + 1) * 128, :] if n < 2 else xl[(n - 2) * 128:(n - 1) * 128, :]),
                pos_fn=lambda n: (None if n < 2 else 2 * (n - 2)),
                ab_fn=lambda n: (Ac if n < 2 else Al),
                embr=embr, embc2_d=embc2, ident=ident,
                dst_fn=lambda n: hTd[n], dst_key_fn=lambda n: "d:hT%d" % n)

    if stop < 1:
        return P
    with P.phase():
        XW = 8454
        XA = P.sb("XA", [128, XW])
        GG = P.sb("GG", [128, T_ALL], BF16)
        HF = P.sb("HF", [128, T_ALL])
        hTt = [P.sb("hTi%d" % b, [128, 16, 128], BF16) for b in range(2)]
        cw = P.sb("cw", [128, 4]); cb = P.sb("cb", [128, 1]); gbt = P.sb("gbt", [128, 4]); lamt = P.sb("lamt", [128, 2])
        cd = P.sb("cd", [128, 4])
        BD = P.sb("BD", [128, 4, 128], BF16)
        P.dma("sp", cw[:], convw[:, :], w=["cw"]); P.dma("sp", cb[:], convb[:, :], w=["cb"])
        P.dma("sp", gbt[:], gb[:, :], w=["gbt"]); P.dma("sp", lamt[:], lam[:, :], w=["lamt"])
        for i in range(4):
            P.dma("pool", BD[:, i, :], bd[i], w=["BD%d" % i])
        tmp2 = P.sb("tmp2", [128, 2])
        P.op("act", lambda e: e.activation(out=tmp2[:], in_=lamt[:], func=AF.Exp, scale=-1.0), r=["lamt"], w=["tmp2"])
        P.op("act", lambda e: e.activation(out=tmp2[:], in_=tmp2[:], func=AF.Ln, bias=1.0), r=["tmp2"], w=["tmp2"])
        P.op("dve", lambda e: e.tensor_scalar(out=cd[:, 0:2], in0=tmp2[:], scalar1=-8.0, scalar2=None, op0=ALU.mult), r=["tmp2"], w=["cd"])
        P.op("dve", lambda e: e.tensor_scalar(out=cd[:, 2:4], in0=tmp2[:], scalar1=-16.0, scalar2=None, op0=ALU.mult), r=["tmp2", "cd"], w=["cd"])
        P.op("pool", lambda e: e.memset(XA[:], 0.0), w=["XA"])
        gx = P.sb("gx", [128, 128]); gx2 = P.sb("gx2", [128, 128]); gs = P.sb("gs", [128, 128])

        def loadh(n):
            P.dma("sp", hTt[n % 2][:], hTd[n], r=["d:hT%d" % n], w=["hTi%d" % (n % 2)])
        loadh(0)
        for n in range(NCH):
            b = n % 2
            if n + 1 < NCH:
                loadh(n + 1)
            pa, pg = PS[2 * b], PS[2 * b + 1]
            for c in range(16):
                P.op("pe", lambda e, c=c: e.matmul(pa[:, 0:128], lhsT=Wb[:, c, 0:128], rhs=hTt[b][:, c, :], start=(c == 0), stop=(c == 15)),
                     r=["hTi%d" % b, WK[c // 4]], w=["PS%d" % (2 * b)])
            for c in range(16):
                P.op("pe", lambda e, c=c: e.matmul(pg[:, 0:128], lhsT=Wb[:, c, 128:256], rhs=hTt[b][:, c, :], start=(c == 0), stop=(c == 15)),
                     r=["hTi%d" % b, WK[c // 4]], w=["PS%d" % (2 * b + 1)])
            col = xa_col(n * 128)
            P.op("act", lambda e: e.activation(out=XA[:, col:col + 128], in_=pa[:, 0:128], func=AF.Copy), r=["PS%d" % (2 * b)], w=["XA"])
            P.op("act", lambda e: e.activation(out=gx[:], in_=pg[:, 0:128], func=AF.Copy), r=["PS%d" % (2 * b + 1)], w=["gx"])
            P.op("dve", lambda e: e.tensor_tensor(out=gx2[:], in0=gx[:], in1=gx[:], op=ALU.mult), r=["gx"], w=["gx2"])
            P.op("dve", lambda e: e.tensor_scalar(out=gx2[:], in0=gx2[:], scalar1=0.044715, scalar2=1.0, op0=ALU.mult, op1=ALU.add), r=["gx2"], w=["gx2"])
            P.op("dve", lambda e: e.tensor_tensor(out=gx2[:], in0=gx2[:], in1=gx[:], op=ALU.mult), r=["gx2", "gx"], w=["gx2"])
            P.op("act", lambda e: e.activation(out=gs[:], in_=gx2[:], func=AF.Sigmoid, scale=1.5957691216057308), r=["gx2"], w=["gs"])
            P.op("dve", lambda e: e.tensor_tensor(out=GG[:, n * 128:(n + 1) * 128], in0=gs[:], in1=gx[:], op=ALU.mult), r=["gs", "gx"], w=["GG"])
        L = 512
        xcv = P.sb("xcv", [128, L]); xcb = P.sb("xcb", [128, L], BF16)
        rr = P.sb("rr", [128, L]); ii = P.sb("ii", [128, L]); aa = P.sb("aa", [128, L]); tt = P.sb("tt", [128, L]); bb = P.sb("bb", [128, L])
        hb = P.sb("hb", [128, L]); yo = [P.sb("yo%d" % b, [128, L], BF16) for b in range(2)]
        carry = P.sb("carry", [128, 1])
        for d in range(2):
            order = LSEGS if d == 0 else [LSEGS[0]] + LSEGS[:0:-1]
            for si, (t0, Ls) in enumerate(order):
                c0 = xa_col(t0)
                P.op("dve", lambda e: e.tensor_scalar(out=xcv[:, :Ls], in0=XA[:, c0 - 2:c0 - 2 + Ls], scalar1=cw[:, 0:1], scalar2=cb[:, 0:1], op0=ALU.mult, op1=ALU.add),
                     r=["XA", "cw", "cb"], w=["xcv"])
                for j in range(1, 4):
                    P.op("dve", lambda e, j=j: e.scalar_tensor_tensor(out=xcv[:, :Ls], in0=XA[:, c0 - 2 + j:c0 - 2 + j + Ls], scalar=cw[:, j:j + 1], in1=xcv[:, :Ls], op0=ALU.mult, op1=ALU.add),
                         r=["XA", "cw", "xcv"], w=["xcv"])
                P.op("pool", lambda e: e.tensor_copy(out=xcb[:, :Ls], in_=xcv[:, :Ls]), r=["xcv"], w=["xcb"])
                P.op("pe", lambda e: e.matmul(PS[0][:, :Ls], lhsT=BD[:, d, :], rhs=xcb[:, :Ls], start=True, stop=True), r=["xcb", "BD%d" % d], w=["PS0"])
                P.op("pe", lambda e: e.matmul(PS[1][:, :Ls], lhsT=BD[:, 2 + d, :], rhs=xcb[:, :Ls], start=True, stop=True), r=["xcb", "BD%d" % (2 + d)], w=["PS1"])
                P.op("act", lambda e: e.activation(out=rr[:, :Ls], in_=PS[0][:, :Ls], func=AF.Sigmoid, bias=gbt[:, d:d + 1]), r=["PS0", "gbt"], w=["rr"])
                P.op("act", lambda e: e.activation(out=ii[:, :Ls], in_=PS[1][:, :Ls], func=AF.Sigmoid, bias=gbt[:, 2 + d:3 + d]), r=["PS1", "gbt"], w=["ii"])
                P.op("act", lambda e: e.activation(out=aa[:, :Ls], in_=rr[:, :Ls], func=AF.Exp, scale=cd[:, d:d + 1]), r=["rr", "cd"], w=["aa"])
                P.op("act", lambda e: e.activation(out=tt[:, :Ls], in_=rr[:, :Ls], func=AF.Exp, scale=cd[:, 2 + d:3 + d]), r=["rr", "cd"], w=["tt"])
                P.op("act", lambda e: e.activation(out=tt[:, :Ls], in_=tt[:, :Ls], func=AF.Sqrt, scale=-1.0, bias=1.0), r=["tt"], w=["tt"])
                P.op("dve", lambda e: e.tensor_tensor(out=bb[:, :Ls], in0=tt[:, :Ls], in1=ii[:, :Ls], op=ALU.mult), r=["tt", "ii"], w=["bb"])
                P.op("dve", lambda e: e.tensor_tensor(out=bb[:, :Ls], in0=bb[:, :Ls], in1=xcv[:, :Ls], op=ALU.mult), r=["bb", "xcv"], w=["bb"])
                if d == 0:
                    init = 0.0 if si == 0 else HF[:, t0 - 1:t0]
                    P.op("dve", lambda e: e.tensor_tensor_scan(out=HF[:, t0:t0 + Ls], data0=aa[:, :Ls], data1=bb[:, :Ls], initial=init, op0=ALU.mult, op1=ALU.add),
                         r=["aa", "bb", "HF"], w=["HF"])
                else:
                    init = 0.0 if si == 0 else carry[:, 0:1]
                    P.op("dve", lambda e: e.tensor_tensor_scan(out=hb[:, slice(Ls - 1, None, -1)], data0=aa[:, slice(Ls - 1, None, -1)], data1=bb[:, slice(Ls - 1, None, -1)],
                                                               initial=init, op0=ALU.mult, op1=ALU.add),
                         r=["aa", "bb", "carry"], w=["hb"])
                    P.op("dve", lambda e: e.tensor_copy(out=carry[:], in_=hb[:, 0:1]), r=["hb"], w=["carry"])
                    P.op("dve", lambda e: e.tensor_tensor(out=hb[:, :Ls], in0=hb[:, :Ls], in1=HF[:, t0:t0 + Ls], op=ALU.add), r=["hb", "HF"], w=["hb"])
                    yb_ = yo[si % 2]
                    P.op("dve", lambda e: e.tensor_tensor(out=yb_[:, :Ls], in0=hb[:, :Ls], in1=GG[:, t0:t0 + Ls], op=ALU.mult), r=["hb", "GG"], w=["yo%d" % (si % 2)])
                    P.dma("sp", yaT[:, t0:t0 + Ls], yb_[:, :Ls], r=["yo%d" % (si % 2)], w=["out:yaT%d" % t0])
    if stop < 2:
        return P
    emit_gla(P, PS, Wb, WK, hTd, wg, bg, gng, trid, strd, yb)
    return P


def emit_gla(P, PS, Wb, WK, hTd, wg, bg, gng, trid, strd, yb):
    with P.phase():
        OF = P.sb("OF", [128, NCH, 128])
        tri = P.sb("tri", [128, 2, 128]); stri = P.sb("stri", [128, 2, 128])
        for d in range(2):
            P.dma("sp", tri[:, d, :], trid[d], w=["tri%d" % d]); P.dma("sp", stri[:, d, :], strd[d], w=["stri%d" % d])
        wgt = P.sb("wgt", [16, 2, 64]); bgb = P.sb("bgb", [128, 2, 64]); gngb = P.sb("gngb", [128, 128])
        for d in range(2):
            P.dma("sp", wgt[:, d, :], wg[d], w=["wgt%d" % d])
            P.dma("sp", bgb[:, d, :], bg[d].partition_broadcast(128), w=["bgb%d" % d])
        P.dma("sp", gngb[:], gng.partition_broadcast(128), w=["gngb"])

        class R:
            pass
        RS = []
        for d in range(2):
            r = R(); s_ = "_%d" % d
            r.h = [P.sb("hTg%d" % b + s_, [128, 16, 128], BF16) for b in range(2)]
            r.S = P.sb("S" + s_, [64, 128]); r.Sb = P.sb("Sb" + s_, [64, 128], BF16)
            r.adT = P.sb("adT" + s_, [16, 128]); r.xg = P.sb("xg" + s_, [128, 64]); r.lg = P.sb("lg" + s_, [128, 64])
            r.E1 = P.sb("E1" + s_, [64, 128]); r.E2 = P.sb("E2" + s_, [64, 128]); r.E3 = P.sb("E3" + s_, [128, 64])
            r.qd = P.sb("qd" + s_, [64, 128], BF16); r.ki = P.sb("ki" + s_, [64, 128], BF16); r.ke = P.sb("ke" + s_, [128, 64], BF16)
            r.vb = P.sb("vb" + s_, [128, 128], BF16); r.sog = P.sb("sog" + s_, [128, 128]); r.scm = P.sb("scm" + s_, [128, 128], BF16)
            r.ot = P.sb("ot" + s_, [128, 128]); r.oj = P.sb("oj" + s_, [128, 128]); r.gst = P.sb("gst" + s_, [128, 4])
            r.ybt = [P.sb("ybt%d" % b + s_, [128, 128], BF16) for b in range(2)]
            r.order = list(range(NCH)) if d == 0 else [1, 0] + list(range(NCH - 1, 1, -1))
            r.arr = {n: i for i, n in enumerate(r.order)}
            RS.append(r)
            P.op("dve", lambda e: e.memset(r.S[:], 0.0), w=["S" + s_])
            P.op("dve", lambda e: e.memset(r.Sb[:], 0.0), w=["Sb" + s_])

        def loadh(d, i):
            r = RS[d]; n = r.order[i]
            P.dma("sp", r.h[i % 2][:], hTd[n], r=["d:hT%d" % n], w=["hTg%d_%d" % (i % 2, d)])

        def step(d, i):
            r = RS[d]; s_ = "_%d" % d; n = r.order[i]; b = i % 2
            K_ = lambda name: name + s_
            QK, TKC, LS, OU = PS[4 * d], PS[4 * d + 1], PS[4 * d + 2], PS[4 * d + 3]
            bQK, bTK, bLS, bOU = ["B%d" % (4 * d + k) for k in range(4)]
            cl = 127 if d == 0 else 0
            final = r.arr[n] > RS[1 - d].arr[n]
            if i + 1 < NCH:
                loadh(d, i + 1)
            hk = "hTg%d_%d" % (b, d); h = r.h[b]
            for (c0, c1, o0, M) in ((256, 320, 0, 64), (320, 384, 128, 64), (384 + 16 * d, 400 + 16 * d, 256, 16)):
                for c in range(16):
                    P.op("pe", lambda e, c=c: e.matmul(QK[0:M, o0:o0 + 128], lhsT=Wb[:, c, c0:c1], rhs=h[:, c, :], start=(c == 0), stop=(c == 15)),
                         r=[hk, WK[c // 4]], w=[bQK])
            for c in range(16):
                P.op("pe", lambda e, c=c: e.matmul(TKC[:, 0:320], lhsT=h[:, c, :], rhs=Wb[:, c, 416:736], start=(c == 0), stop=(c == 15)),
                     r=[hk, WK[c // 4]], w=[bTK])
            yield
            P.op("act", lambda e: e.activation(out=r.adT[:], in_=QK[0:16, 256:384], func=AF.Copy), r=[], w=[bQK, K_("adT")])
            P.op("pe", lambda e: e.matmul(LS[:, 0:64], lhsT=r.adT[:], rhs=wgt[:, d, :], start=True, stop=True), r=[K_("adT"), "wgt%d" % d], w=[bLS])
            P.op("dve", lambda e: e.tensor_tensor(out=r.xg[:], in0=LS[:, 0:64], in1=bgb[:, d, :], op=ALU.add), r=["bgb%d" % d], w=[bLS, K_("xg")])
            P.op("act", lambda e: e.activation(out=r.xg[:], in_=r.xg[:], func=AF.Exp, scale=-1.0), r=[], w=[K_("xg")])
            P.op("act", lambda e: e.activation(out=r.xg[:], in_=r.xg[:], func=AF.Ln, bias=1.0), r=[], w=[K_("xg")])
            P.op("dve", lambda e: e.tensor_scalar(out=r.lg[:], in0=r.xg[:], scalar1=-1.0 / 16.0, scalar2=None, op0=ALU.mult), r=[K_("xg")], w=[K_("lg")])
            yield
            P.op("pe", lambda e: e.matmul(LS[:, 64:128], lhsT=stri[:, d, :], rhs=r.lg[:], start=True, stop=True), r=[K_("lg"), "stri%d" % d], w=[bLS])
            P.op("pe", lambda e: e.matmul(TKC[0:64, 320:448], lhsT=r.lg[:], rhs=tri[:, d, :], start=True, stop=True), r=[K_("lg"), "tri%d" % d], w=[bTK])
            P.op("act", lambda e: e.activation(out=r.E1[:], in_=TKC[0:64, 320:448], func=AF.Exp), r=[], w=[bTK, K_("E1")])
            P.op("act", lambda e: e.activation(out=r.E2[:], in_=TKC[0:64, 320:448], func=AF.Exp, scale=-1.0), r=[], w=[bTK, K_("E2")])
            P.op("act", lambda e: e.activation(out=r.E3[:], in_=LS[:, 64:128], func=AF.Exp), r=[], w=[bLS, K_("E3")])
            P.op("dve", lambda e: e.scalar_tensor_tensor(out=r.qd[:], in0=r.E1[:], scalar=0.125, in1=QK[0:64, 0:128], op0=ALU.mult, op1=ALU.mult), r=[K_("E1")], w=[bQK, K_("qd")])
            P.op("dve", lambda e: e.tensor_tensor(out=r.ki[:], in0=r.E2[:], in1=QK[0:64, 128:256], op=ALU.mult), r=[K_("E2")], w=[bQK, K_("ki")])
            P.op("dve", lambda e: e.tensor_tensor(out=r.ke[:], in0=r.E3[:], in1=TKC[:, 0:64], op=ALU.mult), r=[K_("E3")], w=[bTK, K_("ke")])
            P.op("act", lambda e: e.activation(out=r.vb[:], in_=TKC[:, 64:192], func=AF.Copy), r=[], w=[bTK, K_("vb")])
            if final:
                P.op("act", lambda e: e.activation(out=r.sog[:], in_=TKC[:, 192:320], func=AF.Silu), r=[], w=[bTK, K_("sog")])
            yield
            P.op("pe", lambda e: e.matmul(LS[:, 128:256], lhsT=r.ki[:], rhs=r.qd[:], start=True, stop=True), r=[K_("ki"), K_("qd")], w=[bLS])
            P.op("dve", lambda e: e.tensor_tensor(out=r.scm[:], in0=LS[:, 128:256], in1=tri[:, d, :], op=ALU.mult), r=["tri%d" % d], w=[bLS, K_("scm")])
            P.op("pe", lambda e: e.matmul(OU[:, 0:128], lhsT=r.scm[:], rhs=r.vb[:], start=True, stop=False), r=[K_("scm"), K_("vb")], w=[bOU])
            P.op("pe", lambda e: e.matmul(OU[:, 0:128], lhsT=r.qd[:], rhs=r.Sb[:], start=False, stop=True), r=[K_("qd"), K_("Sb")], w=[bOU])
            P.op("pe", lambda e: e.matmul(OU[0:64, 128:256], lhsT=r.ke[:], rhs=r.vb[:], start=True, stop=True), r=[K_("ke"), K_("vb")], w=[bOU])
            yield
            P.op("dve", lambda e: e.scalar_tensor_tensor(out=r.S[:], in0=r.S[:], scalar=r.E1[:, cl:cl + 1], in1=OU[0:64, 128:256], op0=ALU.mult, op1=ALU.add),
                 r=[K_("E1")], w=[bOU, K_("S")])
            P.op("pool", lambda e: e.tensor_copy(out=r.Sb[:], in_=r.S[:]), r=[K_("S")], w=[K_("Sb")])
            if not final:
                P.op("act", lambda e: e.activation(out=OF[:, n, :], in_=OU[:, 0:128], func=AF.Copy), r=[], w=[bOU, "OF%d" % n])
            else:
                assert ("OF%d" % n) in P.lastw
                P.op("dve", lambda e: e.tensor_tensor(out=r.ot[:], in0=OU[:, 0:128], in1=OF[:, n, :], op=ALU.add), r=["OF%d" % n], w=[bOU, K_("ot")])
                P.op("act", lambda e: e.activation(out=r.oj[:], in_=r.ot[:], func=AF.Square, accum_out=r.gst[:, 0:1]), r=[K_("ot")], w=[K_("oj"), K_("gst0")])
                P.op("dve", lambda e: e.tensor_scalar(out=r.gst[:, 1:2], in0=r.gst[:, 0:1], scalar1=1.0 / 128, scalar2=EPS, op0=ALU.mult, op1=ALU.add), r=[K_("gst0")], w=[K_("gst1")])
                P.op("act", lambda e: e.activation(out=r.gst[:, 2:3], in_=r.gst[:, 1:2], func=AF.Sqrt), r=[K_("gst1")], w=[K_("gst2")])
                P.op("dve", lambda e: e.reciprocal(out=r.gst[:, 3:4], in_=r.gst[:, 2:3]), r=[K_("gst2")], w=[K_("gst3")])
                P.op("dve", lambda e: e.scalar_tensor_tensor(out=r.ot[:], in0=r.ot[:], scalar=r.gst[:, 3:4], in1=gngb[:], op0=ALU.mult, op1=ALU.mult), r=[K_("gst3"), "gngb"], w=[K_("ot")])
                yt = r.ybt[b]
                P.op("dve", lambda e: e.tensor_tensor(out=yt[:], in0=r.ot[:], in1=r.sog[:], op=ALU.mult), r=[K_("ot"), K_("sog")], w=[K_("ybt%d" % b)])
                P.dma("sp", yb[n * 128:(n + 1) * 128, :], yt[:], r=[K_("ybt%d" % b)], w=["out:yb%d" % n])
            yield

        loadh(0, 0); loadh(1, 0)

        def chain(d):
            for i in range(NCH):
                yield from step(d, i)
        ga, gb = chain(0), chain(1)
        for _ in range(2):
            next(ga)
        alive = [ga, gb]
        while alive:
            for g_ in list(alive):
                try:
                    next(g_)
                except StopIteration:
                    alive.remove(g_)


def k1_inputs(x, ctx, modv0, norm1_g0, ev_w_in, ev_conv_w, ev_conv_b, lru_wa, lru_ba, lru_wx, lru_bx, lru_lambda, gla_wg_up, gla_bg, gla_norm_g):
    ident, tri, strict = consts_np()
    embr, embc2 = pos_tables()
    xl = np.ascontiguousarray(x[0]); xc = np.ascontiguousarray(ctx[0])
    base = {"xl": xl, "xc": xc, "embr": embr, "embc2": embc2, "gn": feat_pc(norm1_g0),
            "shl": feat_pc(modv0[0, 0:2048]), "scl": feat_pc(modv0[0, 2048:4096]),
            "shc": feat_pc(modv0[1, 0:2048]), "scc": feat_pc(modv0[1, 2048:4096]),
            "ident": ident, "tri": tri, "strict": strict}
    maps = []
    W = ev_w_in[0]
    for j in range(NCORES):
        s128 = slice(128 * j, 128 * j + 128)
        cols = np.concatenate([np.arange(128 * j, 128 * j + 128), 1024 + np.arange(128 * j, 128 * j + 128),
                               2048 + np.arange(64 * j, 64 * j + 64), 2560 + np.arange(64 * j, 64 * j + 64),
                               5120 + np.arange(32),
                               2560 + np.arange(64 * j, 64 * j + 64), 3072 + np.arange(128 * j, 128 * j + 128),
                               4096 + np.arange(128 * j, 128 * j + 128)])
        bdm = np.zeros((4, 128, 128), np.float32)
        for i, (arr, d) in enumerate([(lru_wa[0], 0), (lru_wa[0], 1), (lru_wx[0], 0), (lru_wx[0], 1)]):
            for m in range(2):
                bdm[i, 64 * m:64 * m + 64, 64 * m:64 * m + 64] = arr[d, 2 * j + m]
        m = dict(base)
        m.update({"win": np.ascontiguousarray(W[:, cols]),
                  "convw": np.ascontiguousarray(ev_conv_w[0][:, s128].T), "convb": np.ascontiguousarray(ev_conv_b[0][s128, None]),
                  "bd": bdm,
                  "gb": np.ascontiguousarray(np.stack([lru_ba[0][0, s128], lru_ba[0][1, s128], lru_bx[0][0, s128], lru_bx[0][1, s128]], -1)),
                  "lam": np.ascontiguousarray(lru_lambda[0][:, s128].T),
                  "wg": np.ascontiguousarray(gla_wg_up[0][:, :, 64 * j:64 * j + 64]), "bg": np.ascontiguousarray(gla_bg[0][:, 64 * j:64 * j + 64]),
                  "gng": np.ascontiguousarray(gla_norm_g[0][s128])})
        maps.append(m)
    return maps


def run_k1(maps):
    P = build_k1()
    res = run_prog(P, maps)
    preT = np.concatenate([r["yaT"] for r in res] + [np.ascontiguousarray(r["yb"].T) for r in res], axis=0)
    return preT


def build_post(layer):
    P = Prog()
    NT = 9 if layer == 0 else 8
    KC = 16 if layer == 0 else 32
    xin = P.din("xin", [NT, 128, D])
    yT = P.din("yT", [KC, 128, NT * 128], BF16)
    wout = P.din("wout", [KC * 128, D])
    g1 = P.din("g1", [2, D])
    gn = P.din("gn", [128, 16])
    sc2l = P.din("sc2l", [128, 16]); sh2l = P.din("sh2l", [128, 16])
    sc2c = P.din("sc2c", [128, 16]); sh2c = P.din("sh2c", [128, 16])
    rw = P.din("rw", [128, 16, 32])
    rb = P.din("rb", [32])
    identd = P.din("ident", [128, 128])
    if layer == 0:
        embr = P.din("embr", [16, 1024]); embc2 = P.din("embc2", [128, 1024])
    else:
        ssqp = P.din("ssqp", [NT, 128, 8])
    x1o = P.dout("x1o", [NT, 128, D])
    h2To = P.dout("h2To", [16, 128, NT * 128], BF16)
    gto = P.dout("gto", [NT, 128, 32])

    PS = [P.ps("PS%d" % i, [128, 512]) for i in range(8)]
    ident = P.sb("ident", [128, 128]); P.dma("sp", ident[:], identd[:, :], w=["ident"])
    Wb = P.sb("Wb", [128, KC, D], BF16)
    wv = wout.rearrange("(c p) n -> p c n", p=128)
    for c in range(KC):
        P.dma("pool", Wb[:, c, :], wv[:, c, :], w=["Wb%d" % c])
    Al = emit_AB(P, gn, sc2l, sh2l, "l")
    Ac = emit_AB(P, gn, sc2c, sh2c, "c") if layer == 0 else None
    G1l = P.sb("G1l", [128, D]); P.dma("sp", G1l[:], g1[0].partition_broadcast(128), w=["G1l"])
    if layer == 0:
        G1c = P.sb("G1c", [128, D]); P.dma("sp", G1c[:], g1[1].partition_broadcast(128), w=["G1c"])
        pc = P.sb("pc", [128, 1024]); P.dma("sp", pc[:], embc2[:, :], w=["pc"])
        pr = P.sb("pr", [128, 1024])
    rwt = P.sb("rwt", [128, 16, 32]); P.dma("sp", rwt[:], rw[:, :, :], w=["rwt"])
    rbb = P.sb("rbb", [128, 32]); P.dma("sp", rbb[:], rb.partition_broadcast(128), w=["rbb"])
    xt = P.sb("xt", [128, D]); tmp = P.sb("tmp", [128, D]); junk = P.sb("junk", [128, D], BF16)
    yt = [P.sb("yt%d" % b, [128, KC, 128], BF16) for b in range(2)]
    st = P.sb("st", [128, 8])
    hq = [P.sb("hq%d" % i, [128, 128]) for i in range(4)]
    h2b = P.sb("h2b", [128, 16, 128], BF16)
    sc = P.sb("sc", [128, 32]); sel = P.sb("sel", [128, 32]); sel2 = P.sb("sel2", [128, 32]); eq = P.sb("eq", [128, 32])
    m1 = P.sb("m1", [128, 4]); m2 = P.sb("m2", [128, 4]); gsx = P.sb("gsx", [128, 4]); gmask = P.sb("gmask", [128, 4]); gmax = P.sb("gmax", [128, 4])
    gout = P.sb("gout", [128, 32])

    def loady(i):
        P.dma("sp", yt[i % 2][:], yT[:, :, i * 128:(i + 1) * 128].rearrange("c p t -> p c t"), w=["yt%d" % (i % 2)])
    loady(0)
    for i in range(NT):
        b = i % 2
        if i + 1 < NT:
            loady(i + 1)
        isctx = (layer == 0 and i == 0)
        P.dma("pool", xt[:], xin[i], w=["xt"])
        if layer == 0 and not isctx:
            r0 = 2 * (i - 1)
            P.dma("sp", pr[0:64, :], embr[r0].partition_broadcast(64), w=["pr"], sk="prlo")
            P.dma("sp", pr[64:128, :], embr[r0 + 1].partition_broadcast(64), w=["prh"], sk="prhi")
            P.op("pool", lambda e: e.tensor_tensor(out=xt[:, 0:1024], in0=xt[:, 0:1024], in1=pr[:], op=ALU.add), r=["pr", "prh"], w=["xt"])
            P.op("dve", lambda e: e.tensor_tensor(out=xt[:, 1024:2048], in0=xt[:, 1024:2048], in1=pc[:], op=ALU.add), r=["pc"], w=["xt"])
        if layer == 1:
            ssq = P.sb("ssq%d" % i, [128, 8])
            P.dma("sp", ssq[:], ssqp[i], w=["ssq%d" % i])
            P.op("dve", lambda e: e.tensor_reduce(out=st[:, 4:5], in_=ssq[:], axis=AX.X, op=ALU.add), r=["ssq%d" % i], w=["st4"])
            P.op("dve", lambda e: e.tensor_scalar(out=st[:, 5:6], in0=st[:, 4:5], scalar1=1.0 / 4096, scalar2=EPS, op0=ALU.mult, op1=ALU.add), r=["st4"], w=["st5"])
            P.op("act", lambda e: e.activation(out=st[:, 6:7], in_=st[:, 5:6], func=AF.Sqrt), r=["st5"], w=["st6"])
            P.op("dve", lambda e: e.reciprocal(out=st[:, 7:8], in_=st[:, 6:7]), r=["st6"], w=["st7"])
        for nb in range(4):
            for c in range(KC):
                P.op("pe", lambda e, c=c, nb=nb: e.matmul(PS[nb][:], lhsT=yt[b][:, c, :], rhs=Wb[:, c, nb * 512:(nb + 1) * 512], start=(c == 0), stop=(c == KC - 1)),
                     r=["yt%d" % b, "Wb%d" % c], w=["B%d" % nb])
        G1 = G1c if isctx else G1l
        for nb in range(4):
            sl = slice(nb * 512, (nb + 1) * 512)
            P.op("dve", lambda e, nb=nb, sl=sl: e.tensor_tensor(out=tmp[:, sl], in0=PS[nb][:], in1=G1[:, sl], op=ALU.mult), r=["G1l", "G1c"], w=["B%d" % nb, "tmp"])
            if layer == 1:
                P.op("dve", lambda e, sl=sl: e.scalar_tensor_tensor(out=xt[:, sl], in0=tmp[:, sl], scalar=st[:, 7:8], in1=xt[:, sl], op0=ALU.mult, op1=ALU.add), r=["tmp", "st7"], w=["xt"])
            else:
                P.op("pool", lambda e, sl=sl: e.tensor_tensor(out=xt[:, sl], in0=tmp[:, sl], in1=xt[:, sl], op=ALU.add), r=["tmp"], w=["xt"])
        P.dma("sp", x1o[i], xt[:], r=["xt"], w=["out:x1o%d" % i])
        P.op("act", lambda e: e.activation(out=junk[:], in_=xt[:], func=AF.Square, accum_out=st[:, 0:1]), r=["xt"], w=["junk", "st0"])
        P.op("dve", lambda e: e.tensor_scalar(out=st[:, 1:2], in0=st[:, 0:1], scalar1=1.0 / D, scalar2=EPS, op0=ALU.mult, op1=ALU.add), r=["st0"], w=["st1"])
        P.op("act", lambda e: e.activation(out=st[:, 2:3], in_=st[:, 1:2], func=AF.Sqrt), r=["st1"], w=["st2"])
        P.op("dve", lambda e: e.reciprocal(out=st[:, 3:4], in_=st[:, 2:3]), r=["st2"], w=["st3"])
        P.op("pool", lambda e: e.tensor_scalar(out=tmp[:], in0=xt[:], scalar1=st[:, 3:4], scalar2=0.0, op0=ALU.mult, op1=ALU.add), r=["xt", "st3"], w=["tmp"])
        A, Bv, keyA, keyB = (Ac if isctx else Al)
        for k4 in range(4):
            bank = PS[4 + k4]; bk = "B%d" % (4 + k4)
            for q in range(4):
                c = 4 * k4 + q
                P.op("pe", lambda e, c=c, q=q: e.transpose(out=bank[:, q * 128:(q + 1) * 128], in_=tmp[:, c * 128:(c + 1) * 128], identity=ident[:]), r=["tmp", "ident"], w=[bk])
            for q in range(4):
                c = 4 * k4 + q
                P.op("act", lambda e, c=c, q=q: e.activation(out=hq[q][:], in_=bank[:, q * 128:(q + 1) * 128], func=AF.Identity, scale=A[:, c:c + 1], bias=Bv[:, c:c + 1]),
                     r=[keyA, keyB], w=[bk, "hq%d" % q])
                P.op("pe", lambda e, c=c, q=q: e.matmul(PS[0][:, 0:32], lhsT=hq[q][:], rhs=rwt[:, c, :], start=(c == 0), stop=(c == 15)), r=["hq%d" % q, "rwt"], w=["B0"])
                P.op("dve", lambda e, c=c, q=q: e.tensor_copy(out=h2b[:, c, :], in_=hq[q][:]), r=["hq%d" % q], w=["h2b"])
        P.dma("sp", h2To[:, :, i * 128:(i + 1) * 128].rearrange("c p t -> p c t"), h2b[:], r=["h2b"], w=["out:h2T%d" % i])
        P.op("act", lambda e: e.activation(out=sc[:], in_=PS[0][:, 0:32], func=AF.Sigmoid), r=[], w=["B0", "sc"])
        P.op("dve", lambda e: e.tensor_tensor(out=sel[:], in0=sc[:], in1=rbb[:], op=ALU.add), r=["sc", "rbb"], w=["sel"])
        sel3 = sel[:].rearrange("p (g k) -> p g k", g=4); sel23 = sel2[:].rearrange("p (g k) -> p g k", g=4); eq3 = eq[:].rearrange("p (g k) -> p g k", g=4)
        P.op("dve", lambda e: e.tensor_reduce(out=m1[:], in_=sel3, axis=AX.X, op=ALU.max), r=["sel"], w=["m1"])
        P.op("dve", lambda e: e.tensor_tensor(out=eq3, in0=sel3, in1=m1[:].unsqueeze(2).to_broadcast([128, 4, 8]), op=ALU.is_equal), r=["sel", "m1"], w=["eq"])
        P.op("dve", lambda e: e.scalar_tensor_tensor(out=sel2[:], in0=eq[:], scalar=-1.0e9, in1=sel[:], op0=ALU.mult, op1=ALU.add), r=["eq", "sel"], w=["sel2"])
        P.op("dve", lambda e: e.tensor_reduce(out=m2[:], in_=sel23, axis=AX.X, op=ALU.max), r=["sel2"], w=["m2"])
        P.op("dve", lambda e: e.tensor_tensor(out=gsx[:], in0=m1[:], in1=m2[:], op=ALU.add), r=["m1", "m2"], w=["gsx"])
        P.op("dve", lambda e: e.tensor_reduce(out=gmax[:, 0:1], in_=gsx[:], axis=AX.X, op=ALU.max), r=["gsx"], w=["gmax"])
        P.op("dve", lambda e: e.tensor_scalar(out=gmask[:], in0=gsx[:], scalar1=gmax[:, 0:1], scalar2=None, op0=ALU.is_equal), r=["gsx", "gmax"], w=["gmask"])
        P.op("dve", lambda e: e.tensor_tensor(out=eq3, in0=sel3, in1=m2[:].unsqueeze(2).to_broadcast([128, 4, 8]), op=ALU.is_ge), r=["sel", "m2"], w=["eq"])
        P.op("dve", lambda e: e.tensor_tensor(out=eq3, in0=eq3, in1=gmask[:].unsqueeze(2).to_broadcast([128, 4, 8]), op=ALU.mult), r=["eq", "gmask"], w=["eq"])
        P.op("dve", lambda e: e.tensor_tensor(out=sel2[:], in0=eq[:], in1=sc[:], op=ALU.mult), r=["eq", "sc"], w=["sel2"])
        P.op("dve", lambda e: e.tensor_reduce(out=gmax[:, 1:2], in_=sel2[:], axis=AX.X, op=ALU.add), r=["sel2"], w=["gmax1"])
        P.op("dve", lambda e: e.reciprocal(out=gmax[:, 2:3], in_=gmax[:, 1:2]), r=["gmax1"], w=["gmax2"])
        P.op("dve", lambda e: e.tensor_scalar(out=gout[:], in0=sel2[:], scalar1=gmax[:, 2:3], scalar2=None, op0=ALU.mult), r=["sel2", "gmax2"], w=["gout"])
        P.dma("sp", gto[i], gout[:], r=["gout"], w=["out:gt%d" % i])
    return P


def tok_tiles(arr_c, arr_l, j, layer):
    F = arr_l.shape[-1]
    lat = arr_l[1024 * j:1024 * (j + 1)].reshape(8, 128, F)
    if layer == 0:
        t0 = np.zeros((1, 128, F), arr_l.dtype)
        t0[0, :32] = arr_c[32 * j:32 * (j + 1)]
        return np.ascontiguousarray(np.concatenate([t0, lat], 0))
    return np.ascontiguousarray(lat)


def featT_tiles(aT, j, layer):
    lat = aT[:, :, 256 + 1024 * j:256 + 1024 * (j + 1)]
    if layer == 0:
        t0 = np.zeros(aT.shape[:2] + (128,), aT.dtype)
        t0[:, :, :32] = aT[:, :, 32 * j:32 * (j + 1)]
        return np.ascontiguousarray(np.concatenate([t0, lat], -1))
    return np.ascontiguousarray(lat)


def untile(tiles_per_core, layer):
    if layer == 0:
        c = np.concatenate([t[0, :32] for t in tiles_per_core], 0)
        l = np.concatenate([t[1:].reshape(1024, -1) for t in tiles_per_core], 0)
        return c, l
    return None, np.concatenate([t.reshape(1024, -1) for t in tiles_per_core], 0)


def run_post(layer, x_c, x_l, preT, wout, modv_l, norm2_g, router_w, router_b, ssq_parts=None):
    P = build_post(layer)
    ident, _, _ = consts_np()
    embr, embc2 = pos_tables()
    KC = preT.shape[0] // 128
    aT = preT.reshape(KC, 128, T_ALL)
    base = {"wout": np.ascontiguousarray(wout, np.float32),
            "g1": np.ascontiguousarray(np.stack([modv_l[0, 4096:6144], modv_l[1, 4096:6144]])),
            "gn": feat_pc(norm2_g), "sh2l": feat_pc(modv_l[0, 6144:8192]), "sc2l": feat_pc(modv_l[0, 8192:10240]),
            "sh2c": feat_pc(modv_l[1, 6144:8192]), "sc2c": feat_pc(modv_l[1, 8192:10240]),
            "rw": np.ascontiguousarray(np.asarray(router_w, np.float32).reshape(16, 128, 32).transpose(1, 0, 2)),
            "rb": np.asarray(router_b, np.float32), "ident": ident}
    maps = []
    for j in range(NCORES):
        m = dict(base)
        m["xin"] = tok_tiles(x_c, x_l, j, layer)
        m["yT"] = featT_tiles(aT, j, layer)
        if layer == 0:
            m["embr"] = np.ascontiguousarray(embr[16 * j:16 * j + 16]); m["embc2"] = embc2
        else:
            m["ssqp"] = np.ascontiguousarray(ssq_parts[1024 * j:1024 * (j + 1)].reshape(8, 128, 8))
        maps.append(m)
    res = run_prog(P, maps)
    x1c, x1l = untile([r["x1o"] for r in res], layer)
    gc, gl = untile([r["gto"] for r in res], layer)
    if layer == 0:
        h2T = np.concatenate([r["h2To"][:, :, :32] for r in res] + [r["h2To"][:, :, 128:] for r in res], -1)
        gates = np.concatenate([gc, gl], 0)
    else:
        h2T = np.concatenate([r["h2To"] for r in res], -1)
        gates = gl
    return x1c, x1l, np.ascontiguousarray(h2T), gates


def build_moe(T):
    P = Prog()
    NT = T // 128
    FE = 1024
    h2T = P.din("h2T", [16, 128, T], BF16)
    gt = P.din("gt", [128, NT, 4])
    wg = P.din("wg", [4, D, FE]); wu = P.din("wu", [4, D, FE]); wd = P.din("wd", [4, FE, D])
    part = P.dout("part", [T, D])
    acc = P.dscr("acc", [T, D])
    PS = [P.ps("PS%d" % i, [128, 512]) for i in range(8)]
    gtt = P.sb("gtt", [128, NT, 4]); P.dma("sp", gtt[:], gt[:, :, :], w=["gtt"])
    groups = [(t0, min(512, T - t0)) for t0 in range(0, T, 512)]
    Wg = P.sb("Wg", [128, 16, FE], BF16); Wu = P.sb("Wu", [128, 16, FE], BF16); Wd = P.sb("Wd", [128, 8, D], BF16)
    hg = [P.sb("hg%d" % b, [128, 16, 512], BF16) for b in range(2)]
    AT = P.sb("AT", [128, 8, 512], BF16)
    sgt = [P.sb("sgt%d" % b, [128, 512], BF16) for b in range(2)]
    Yt = [P.sb("Yt%d" % b, [128, D]) for b in range(2)]
    Pv = [P.sb("Pv%d" % b, [128, D]) for b in range(2)]
    h2v = h2T.rearrange("c p t -> p c t")
    ycount = 0
    for e_ in range(4):
        wgv = wg[e_].rearrange("(c p) n -> p c n", p=128); wuv = wu[e_].rearrange("(c p) n -> p c n", p=128)
        wdv = wd[e_].rearrange("(c p) n -> p c n", p=128)
        for c in range(16):
            P.dma("pool", Wg[:, c, :], wgv[:, c, :], w=["Wg%d" % c])
            P.dma("pool", Wu[:, c, :], wuv[:, c, :], w=["Wu%d" % c])
        for c in range(8):
            P.dma("pool", Wd[:, c, :], wdv[:, c, :], w=["Wd%d" % c])

        def loadh(gi):
            t0, Lg = groups[gi]
            P.dma("sp", hg[gi % 2][:, :, :Lg], h2v[:, :, t0:t0 + Lg], w=["hg%d" % (gi % 2)])
        loadh(0)
        for gi, (t0, Lg) in enumerate(groups):
            b = gi % 2
            if gi + 1 < len(groups):
                loadh(gi + 1)
            for fc in range(8):
                pb = fc % 2
                Gb, Ub = PS[2 * pb], PS[2 * pb + 1]
                for c in range(16):
                    P.op("pe", lambda e, c=c: e.matmul(Gb[:, :Lg], lhsT=Wg[:, c, fc * 128:(fc + 1) * 128], rhs=hg[b][:, c, :Lg], start=(c == 0), stop=(c == 15)),
                         r=["hg%d" % b, "Wg%d" % c], w=["B%d" % (2 * pb)])
                for c in range(16):
                    P.op("pe", lambda e, c=c: e.matmul(Ub[:, :Lg], lhsT=Wu[:, c, fc * 128:(fc + 1) * 128], rhs=hg[b][:, c, :Lg], start=(c == 0), stop=(c == 15)),
                         r=["hg%d" % b, "Wu%d" % c], w=["B%d" % (2 * pb + 1)])
                P.op("act", lambda e: e.activation(out=sgt[pb][:, :Lg], in_=Gb[:, :Lg], func=AF.Silu), r=[], w=["B%d" % (2 * pb), "sgt%d" % pb])
                P.op("dve", lambda e: e.tensor_tensor(out=AT[:, fc, :Lg], in0=Ub[:, :Lg], in1=sgt[pb][:, :Lg], op=ALU.mult), r=["sgt%d" % pb], w=["B%d" % (2 * pb + 1), "AT%d" % fc])
            for tt in range(Lg // 128):
                tile = (t0 // 128) + tt
                yi = ycount % 2; ycount += 1
                yb_ = Yt[yi]; yk = "Yt%d" % yi
                rows = slice(tile * 128, (tile + 1) * 128)
                if e_ > 0:
                    P.dma("pool", Pv[yi][:], acc[rows, :], r=["d:acc%d" % tile], w=["Pv%d" % yi])
                for dmb in range(4):
                    bank = PS[4 + dmb]; bk = "B%d" % (4 + dmb)
                    for fc in range(8):
                        P.op("pe", lambda e, fc=fc: e.matmul(bank[:], lhsT=AT[:, fc, tt * 128:(tt + 1) * 128], rhs=Wd[:, fc, dmb * 512:(dmb + 1) * 512], start=(fc == 0), stop=(fc == 7)),
                             r=["AT%d" % fc, "Wd%d" % fc], w=[bk])
                    sl = slice(dmb * 512, (dmb + 1) * 512)
                    if e_ == 0:
                        if dmb % 2 == 0:
                            P.op("act", lambda e: e.activation(out=yb_[:, sl], in_=bank[:], func=AF.Copy, scale=gtt[:, tile, e_:e_ + 1]), r=["gtt"], w=[bk, yk + "_%d" % dmb])
                        else:
                            P.op("dve", lambda e: e.tensor_scalar(out=yb_[:, sl], in0=bank[:], scalar1=gtt[:, tile, e_:e_ + 1], scalar2=None, op0=ALU.mult), r=["gtt"], w=[bk, yk + "_%d" % dmb])
                    else:
                        P.op("dve", lambda e: e.scalar_tensor_tensor(out=yb_[:, sl], in0=bank[:], scalar=gtt[:, tile, e_:e_ + 1], in1=Pv[yi][:, sl], op0=ALU.mult, op1=ALU.add),
                             r=["gtt", "Pv%d" % yi], w=[bk, yk + "_%d" % dmb])
                if e_ < 3:
                    P.dma("sp", acc[rows, :], yb_[:], r=[yk + "_%d" % q for q in range(4)], w=["d:acc%d" % tile], sk=yk)
                else:
                    P.dma("sp", part[rows, :], yb_[:], r=[yk + "_%d" % q for q in range(4)], w=["out:part%d" % tile], sk=yk)
    return P


def run_moe(h2T, gates, wgate, wup, wdown):
    T = h2T.shape[-1]
    P = build_moe(T)
    maps = []
    for j in range(NCORES):
        g = np.ascontiguousarray(gates[:, 4 * j:4 * j + 4].reshape(T // 128, 128, 4).transpose(1, 0, 2))
        maps.append({"h2T": h2T, "gt": g, "wg": np.ascontiguousarray(wgate[4 * j:4 * j + 4]), "wu": np.ascontiguousarray(wup[4 * j:4 * j + 4]),
                     "wd": np.ascontiguousarray(wdown[4 * j:4 * j + 4])})
    res = run_prog(P, maps)
    return [r["part"] for r in res]


def build_combine(layer):
    P = Prog()
    NT = 9 if layer == 0 else 8
    x1 = P.din("x1", [NT, 128, D])
    parts = P.din("parts", [8, NT, 128, D])
    g2 = P.din("g2", [2, D])
    xo = P.dout("xo", [NT, 128, D])
    G2l = P.sb("G2l", [128, D]); P.dma("sp", G2l[:], g2[0].partition_broadcast(128), w=["G2l"])
    if layer == 0:
        G2c = P.sb("G2c", [128, D]); P.dma("sp", G2c[:], g2[1].partition_broadcast(128), w=["G2c"])
    else:
        fg = P.din("fg", [D])
        FG = P.sb("FG", [128, D]); P.dma("sp", FG[:], fg.partition_broadcast(128), w=["FG"])
        junk = P.sb("junk", [128, D], BF16); st = P.sb("st", [128, 4])
    pb = [P.sb("pb%d" % q, [128, D]) for q in range(8)]
    xt = [P.sb("xt%d" % b, [128, D]) for b in range(2)]
    for i in range(NT):
        b = i % 2
        P.dma("sp", xt[b][:], x1[i], w=["xt%d" % b])
        for q in range(8):
            P.dma("sp" if q % 2 == 0 else "pool", pb[q][:], parts[q, i], w=["pb%d" % q])
        for (a, c, eng) in ((0, 1, "dve"), (2, 3, "pool"), (4, 5, "dve"), (6, 7, "pool"), (0, 2, "dve"), (4, 6, "pool"), (0, 4, "dve")):
            P.op(eng, lambda e, a=a, c=c: e.tensor_tensor(out=pb[a][:], in0=pb[a][:], in1=pb[c][:], op=ALU.add), r=["pb%d" % c], w=["pb%d" % a])
        G2 = G2c if (layer == 0 and i == 0) else G2l
        P.op("dve", lambda e: e.tensor_tensor(out=pb[0][:], in0=pb[0][:], in1=G2[:], op=ALU.mult), r=["G2l", "G2c"], w=["pb0"])
        P.op("pool", lambda e: e.tensor_tensor(out=xt[b][:], in0=xt[b][:], in1=pb[0][:], op=ALU.add), r=["pb0"], w=["xt%d" % b])
        if layer == 1:
            P.op("act", lambda e: e.activation(out=junk[:], in_=xt[b][:], func=AF.Square, accum_out=st[:, 0:1]), r=["xt%d" % b], w=["junk", "st0"])
            P.op("dve", lambda e: e.tensor_scalar(out=st[:, 1:2], in0=st[:, 0:1], scalar1=1.0 / D, scalar2=EPS, op0=ALU.mult, op1=ALU.add), r=["st0"], w=["st1"])
            P.op("act", lambda e: e.activation(out=st[:, 2:3], in_=st[:, 1:2], func=AF.Sqrt), r=["st1"], w=["st2"])
            P.op("dve", lambda e: e.reciprocal(out=st[:, 3:4], in_=st[:, 2:3]), r=["st2"], w=["st3"])
            P.op("dve", lambda e: e.scalar_tensor_tensor(out=xt[b][:], in0=xt[b][:], scalar=st[:, 3:4], in1=FG[:], op0=ALU.mult, op1=ALU.mult), r=["st3", "FG"], w=["xt%d" % b])
        P.dma("sp", xo[i], xt[b][:], r=["xt%d" % b], w=["out:xo%d" % i])
    return P


def run_combine(layer, x1c, x1l, parts, modv_l, final_g=None):
    P = build_combine(layer)
    maps = []
    g2 = np.ascontiguousarray(np.stack([modv_l[0, 10240:12288], modv_l[1, 10240:12288]]))
    for j in range(NCORES):
        if layer == 0:
            pj = np.stack([tok_tiles(p[:256], p[256:], j, 0) for p in parts])
        else:
            pj = np.stack([tok_tiles(None, p, j, 1) for p in parts])
        m = {"x1": tok_tiles(x1c, x1l, j, layer), "parts": np.ascontiguousarray(pj), "g2": g2}
        if layer == 1:
            m["fg"] = np.asarray(final_g, np.float32)
        maps.append(m)
    res = run_prog(P, maps)
    return untile([r["xo"] for r in res], layer)


K5_COLS = 1296
XW = 8454


def build_k5():
    P = Prog()
    xl = P.din("xl", [SEQ, D]); xc = P.din("xc", [CTX, D])
    gn = P.din("gn", [128, 16])
    scl = P.din("scl", [128, 16]); shl = P.din("shl", [128, 16]); scc = P.din("scc", [128, 16]); shc = P.din("shc", [128, 16])
    win = P.din("win", [D, K5_COLS])
    convw = P.din("convw", [128, 6, 4]); convb = P.din("convb", [128, 6])
    alog = P.din("alog", [16]); dtb = P.din("dtb", [16]); dsk = P.din("dsk", [8]); ng = P.din("ng", [512])
    identd = P.din("ident", [128, 128]); trid = P.din("tri", [2, 128, 128]); strd = P.din("strict", [2, 128, 128])
    seld = P.din("sel", [8, 1024])
    yzo = P.dout("yzo", [SEQ, 512], BF16)
    ssqo = P.dout("ssqo", [SEQ, 1])
    hTd = P.dscr("hTd", [128, 16, XW], BF16)
    YF = P.dscr("YF", [SEQ, 512])

    PS = [P.ps("PS%d" % i, [128, 512]) for i in range(8)]
    ident = P.sb("ident", [128, 128]); P.dma("sp", ident[:], identd[:, :], w=["ident"])
    Wb = P.sb("Wb", [128, 16, K5_COLS], BF16)
    winv = win.rearrange("(c p) n -> p c n", p=128)
    for c in range(16):
        P.dma("pool", Wb[:, c, :], winv[:, c, :], w=["Wb%d" % c])
    with P.phase():
        zt = P.sb("zt", [128, 16, 4], BF16)
        P.op("dve", lambda e: e.memset(zt[:], 0.0), w=["zt"])
        for (a, b_) in ((0, 2), (258, 261), (8453, 8454)):
            P.dma("sp", hTd[:, :, a:b_], zt[:, :, 0:b_ - a], r=["zt"], w=["d:pad%d" % a], sk="zt", allow_slow_non_contiguous=True)
        Al = emit_AB(P, gn, scl, shl, "l")
        Ac = emit_AB(P, gn, scc, shc, "c")
        emit_hT(P, PS, NCH,
                src_fn=lambda n: (xc[n * 128:(n + 1) * 128, :] if n < 2 else xl[(n - 2) * 128:(n - 1) * 128, :]),
                pos_fn=lambda n: None, ab_fn=lambda n: (Ac if n < 2 else Al), embr=None, embc2_d=None, ident=ident,
                dst_fn=lambda n: hTd[:, :, xa_col(128 * n):xa_col(128 * n) + 128], dst_key_fn=lambda n: "d:hT%d" % n)
    YB = P.dscr("YB", [SEQ, 512])
    emit_ssd(P, PS, Wb, hTd, ident, trid, strd, seld, convw, convb, alog, dtb, dsk, ng, [YF, YB], yzo, ssqo)
    return P


def emit_ssd(P, PS, Wb, hTd, ident, trid, strd, seld, convw, convb, alog, dtb, dsk, ng, YS, yzo, ssqo):
    with P.phase():
        tri = P.sb("tri", [128, 2, 128]); stri = P.sb("stri", [128, 2, 128]); ones = P.sb("ones", [128, 128])
        for d in range(2):
            P.dma("sp", tri[:, d, :], trid[d], w=["tri%d" % d]); P.dma("sp", stri[:, d, :], strd[d], w=["stri%d" % d])
        P.op("dve", lambda e: e.memset(ones[:], 1.0), w=["ones"])
        SEL = P.sb("SEL", [8, 1024]); P.dma("sp", SEL[:], seld[:, :], w=["SEL"])
        cw = P.sb("cw", [128, 6, 4]); cbv = P.sb("cbv", [128, 6])
        P.dma("sp", cw[:], convw[:, :, :], w=["cw"]); P.dma("sp", cbv[:], convb[:, :], w=["cbv"])
        aneg = P.sb("aneg", [128, 16]); dtbb = P.sb("dtbb", [128, 16]); dskb = P.sb("dskb", [128, 8]); ngb = P.sb("ngb", [128, 512])
        P.dma("sp", aneg[:], alog.partition_broadcast(128), w=["aneg"]); P.dma("sp", dtbb[:], dtb.partition_broadcast(128), w=["dtbb"])
        P.dma("sp", dskb[:], dsk.partition_broadcast(128), w=["dskb"]); P.dma("sp", ngb[:], ng.partition_broadcast(128), w=["ngb"])
        P.op("act", lambda e: e.activation(out=aneg[:], in_=aneg[:], func=AF.Exp), r=[], w=["aneg"])
        P.op("dve", lambda e: e.tensor_scalar(out=aneg[:], in0=aneg[:], scalar1=-1.0, scalar2=None, op0=ALU.mult), r=[], w=["aneg"])

        def v3(t, h=8):
            return t[:].rearrange("p (h q) -> p h q", h=h)

        class R:
            pass
        RS = []
        for d in range(2):
            r = R(); s_ = "_%d" % d
            r.hw = [P.sb("hw%d" % b + s_, [128, 16, 131], BF16) for b in range(2)]
            r.X = P.sb("X" + s_, [128, 786]); r.U = P.sb("U" + s_, [128, 6, 128]); r.BCb = P.sb("BCb" + s_, [128, 2, 128], BF16)
            r.Btk = P.sb("Btk" + s_, [128, 128], BF16); r.xst = P.sb("xst" + s_, [128, 512]); r.sz = P.sb("sz" + s_, [128, 512])
            r.dtp = P.sb("dtp" + s_, [128, 8]); r.la = P.sb("la" + s_, [128, 8]); r.ecum = P.sb("ecum" + s_, [128, 8]); r.erest = P.sb("erest" + s_, [128, 8]); r.dec = P.sb("dec" + s_, [128, 8])
            r.ncT = P.sb("ncT" + s_, [8, 128]); r.cT = P.sb("cT" + s_, [8, 128]); r.BDc = P.sb("BDc" + s_, [8, 1024]); r.cbm = P.sb("cbm" + s_, [128, 128])
            r.dmin = P.sb("dmin" + s_, [128, 1024]); r.M = P.sb("M" + s_, [128, 8, 128], BF16)
            r.xdt32 = P.sb("xdt32" + s_, [128, 512]); r.xdtb = P.sb("xdtb" + s_, [128, 512], BF16); r.xdte = P.sb("xdte" + s_, [128, 512], BF16)
            r.yt = P.sb("yt" + s_, [128, 512]); r.yf = [P.sb("yf%d" % b + s_, [128, 512]) for b in range(2)]; r.tmpd = P.sb("tmpd" + s_, [128, 512])
            r.ST = P.sb("ST" + s_, [128, 512]); r.STb = P.sb("STb" + s_, [128, 512], BF16)
            r.yo = [P.sb("yo%d" % b + s_, [128, 512], BF16) for b in range(2)]; r.sq = [P.sb("sq%d" % b + s_, [128, 2]) for b in range(2)]
            r.junk = P.sb("junk" + s_, [128, 512], BF16)
            r.ctmp = P.sb("ctmp" + s_, [128, 6, 128])
            r.order = list(range(NCH)) if d == 0 else [1, 0] + list(range(NCH - 1, 1, -1))
            RS.append(r)
            P.op("dve", lambda e: e.memset(r.ST[:], 0.0), w=["ST" + s_])
            P.op("dve", lambda e: e.memset(r.STb[:], 0.0), w=["STb" + s_])

        def loadw(d, i):
            r = RS[d]; n = r.order[i]
            c0 = xa_col(128 * n) - 2
            P.dma("sp", r.hw[i % 2][:], hTd[:, :, c0:c0 + 131], r=["d:hT%d" % n], w=["hw%d_%d" % (i % 2, d)])

        def step(d, i):
            r = RS[d]; s_ = "_%d" % d; n = r.order[i]; b = i % 2
            K_ = lambda name: name + s_
            B_ = lambda k: "B%d" % (4 * d + k)
            PB = lambda k: PS[4 * d + k]
            if i + 1 < NCH:
                loadw(d, i + 1)
            h = r.hw[b]; hk = "hw%d_%d" % (b, d)
            lat = n >= 2
            row0 = (n - 2) * 128
            final = lat and ((n >= 34) if d == 0 else (n <= 33))
            for k in range(6):
                bank = PB(k // 3); o0 = (k % 3) * 131
                for c in range(16):
                    P.op("pe", lambda e, c=c: e.matmul(bank[:, o0:o0 + 131], lhsT=Wb[:, c, k * 128:(k + 1) * 128], rhs=h[:, c, :], start=(c == 0), stop=(c == 15)),
                         r=[hk, "Wb%d" % c], w=[B_(k // 3)])
            for c in range(16):
                P.op("pe", lambda e, c=c: e.matmul(PB(3)[:, 0:16], lhsT=h[:, c, 2:130], rhs=Wb[:, c, 1280:1296], start=(c == 0), stop=(c == 15)), r=[hk, "Wb%d" % c], w=[B_(3)])
            if final:
                for c in range(16):
                    P.op("pe", lambda e, c=c: e.matmul(PB(2)[:, 0:512], lhsT=h[:, c, 2:130], rhs=Wb[:, c, 768:1280], start=(c == 0), stop=(c == 15)), r=[hk, "Wb%d" % c], w=[B_(2)])
                P.op("act", lambda e: e.activation(out=r.sz[:], in_=PB(2)[:, 0:512], func=AF.Silu), r=[], w=[B_(2), K_("sz")])
            yield
            P.op("act", lambda e: e.activation(out=r.X[:, 0:393], in_=PB(0)[:, 0:393], func=AF.Copy), r=[], w=[B_(0), K_("X0")])
            P.op("act", lambda e: e.activation(out=r.X[:, 393:786], in_=PB(1)[:, 0:393], func=AF.Copy), r=[], w=[B_(1), K_("X1")])
            X3 = r.X[:].rearrange("p (k t) -> p k t", k=6)
            xks = [K_("X0"), K_("X1")]
            P.op("dve", lambda e: e.tensor_tensor(out=r.U[:], in0=X3[:, :, 0:128], in1=cw[:, :, 0:1].to_broadcast([128, 6, 128]), op=ALU.mult), r=xks + ["cw"], w=[K_("U")])
            for t_ in range(1, 4):
                P.op("pool", lambda e, t_=t_: e.tensor_tensor(out=r.ctmp[:], in0=X3[:, :, t_:t_ + 128], in1=cw[:, :, t_:t_ + 1].to_broadcast([128, 6, 128]), op=ALU.mult), r=xks + ["cw"], w=[K_("ctmp")])
                P.op("dve", lambda e: e.tensor_tensor(out=r.U[:], in0=r.U[:], in1=r.ctmp[:], op=ALU.add), r=[K_("ctmp")], w=[K_("U")])
            P.op("dve", lambda e: e.tensor_tensor(out=r.U[:], in0=r.U[:], in1=cbv[:].unsqueeze(2).to_broadcast([128, 6, 128]), op=ALU.add), r=["cbv"], w=[K_("U")])
            yield
            P.op("act", lambda e: e.activation(out=r.U[:], in_=r.U[:], func=AF.Silu), r=[], w=[K_("U")])
            yield
            for k in range(4):
                P.op("pe", lambda e, k=k: e.transpose(out=PB(0)[:, k * 128:(k + 1) * 128], in_=r.U[:, k, :], identity=ident[:]), r=[K_("U"), "ident"], w=[B_(0)])
            P.op("pe", lambda e: e.transpose(out=PB(3)[:, 320:448], in_=r.U[:, 4, :], identity=ident[:]), r=[K_("U"), "ident"], w=[B_(3)])
            P.op("pool", lambda e: e.tensor_copy(out=r.BCb[:], in_=r.U[:, 4:6, :]), r=[K_("U")], w=[K_("BCb")])
            P.op("act", lambda e: e.activation(out=r.xst[:], in_=PB(0)[:, 0:512], func=AF.Copy), r=[], w=[B_(0), K_("xst")])
            P.op("act", lambda e: e.activation(out=r.Btk[:], in_=PB(3)[:, 320:448], func=AF.Copy), r=[], w=[B_(3), K_("Btk")])
            yield
            P.op("dve", lambda e: e.tensor_tensor(out=r.dtp[:], in0=PB(3)[:, 8 * d:8 * d + 8], in1=dtbb[:, 8 * d:8 * d + 8], op=ALU.add), r=["dtbb"], w=[B_(3), K_("dtp")])
            P.op("act", lambda e: e.activation(out=r.dtp[:], in_=r.dtp[:], func=AF.Exp), r=[], w=[K_("dtp")])
            P.op("act", lambda e: e.activation(out=r.dtp[:], in_=r.dtp[:], func=AF.Ln, bias=1.0), r=[], w=[K_("dtp")])
            P.op("dve", lambda e: e.tensor_tensor(out=r.la[:], in0=r.dtp[:], in1=aneg[:, 8 * d:8 * d + 8], op=ALU.mult), r=[K_("dtp"), "aneg"], w=[K_("la")])
            P.op("pe", lambda e: e.matmul(PB(3)[:, 16:24], lhsT=tri[:, d, :], rhs=r.la[:], start=True, stop=True), r=[K_("la"), "tri%d" % d], w=[B_(3)])
            P.op("pe", lambda e: e.matmul(PB(3)[:, 24:32], lhsT=stri[:, d, :], rhs=r.la[:], start=True, stop=True), r=[K_("la"), "stri%d" % d], w=[B_(3)])
            P.op("pe", lambda e: e.matmul(PB(3)[:, 32:40], lhsT=ones[:], rhs=r.la[:], start=True, stop=True), r=[K_("la"), "ones"], w=[B_(3)])
            P.op("pe", lambda e: e.matmul(PB(3)[0:8, 64:192], lhsT=r.la[:], rhs=tri[:, d, :], start=True, stop=True), r=[K_("la"), "tri%d" % d], w=[B_(3)])
            P.op("pe", lambda e: e.matmul(PB(3)[:, 192:320], lhsT=r.BCb[:, 0, :], rhs=r.BCb[:, 1, :], start=True, stop=True), r=[K_("BCb")], w=[B_(3)])
            P.op("act", lambda e: e.activation(out=r.ecum[:], in_=PB(3)[:, 16:24], func=AF.Exp), r=[], w=[B_(3), K_("ecum")])
            P.op("act", lambda e: e.activation(out=r.erest[:], in_=PB(3)[:, 24:32], func=AF.Exp), r=[], w=[B_(3), K_("erest")])
            P.op("act", lambda e: e.activation(out=r.dec[:], in_=PB(3)[:, 32:40], func=AF.Exp), r=[], w=[B_(3), K_("dec")])
            P.op("act", lambda e: e.activation(out=r.cT[:], in_=PB(3)[0:8, 64:192], func=AF.Copy), r=[], w=[B_(3), K_("cT")])
            P.op("dve", lambda e: e.tensor_scalar(out=r.ncT[:], in0=r.cT[:], scalar1=-1.0, scalar2=None, op0=ALU.mult), r=[K_("cT")], w=[K_("ncT")])
            P.op("dve", lambda e: e.tensor_tensor(out=r.BDc[:].rearrange("p (h c) -> p h c", h=8), in0=SEL[:].rearrange("p (h c) -> p h c", h=8),
                                                  in1=r.cT[:].unsqueeze(1).to_broadcast([8, 8, 128]), op=ALU.mult), r=[K_("cT"), "SEL"], w=[K_("BDc")])
            P.op("dve", lambda e: e.tensor_tensor(out=r.cbm[:], in0=PB(3)[:, 192:320], in1=tri[:, d, :], op=ALU.mult), r=["tri%d" % d], w=[B_(3), K_("cbm")])
            yield
            for hf in range(2):
                P.op("pe", lambda e: e.matmul(PB(1 + hf)[:, 0:512], lhsT=r.ncT[:], rhs=SEL[:, hf * 512:(hf + 1) * 512], start=True, stop=False), r=[K_("ncT"), "SEL"], w=[B_(1 + hf)])
                P.op("pe", lambda e: e.matmul(PB(1 + hf)[:, 0:512], lhsT=ones[0:8, :], rhs=r.BDc[:, hf * 512:(hf + 1) * 512], start=False, stop=True), r=["ones", K_("BDc")], w=[B_(1 + hf)])
                P.op("dve", lambda e: e.tensor_scalar(out=r.dmin[:, hf * 512:(hf + 1) * 512], in0=PB(1 + hf)[:, 0:512], scalar1=0.0, scalar2=None, op0=ALU.min), r=[], w=[B_(1 + hf), K_("dmin")])
            P.op("act", lambda e: e.activation(out=r.dmin[:], in_=r.dmin[:], func=AF.Exp), r=[], w=[K_("dmin")])
            P.op("dve", lambda e: e.tensor_tensor(out=r.M[:], in0=r.dmin[:].rearrange("p (h c) -> p h c", h=8), in1=r.cbm[:].unsqueeze(1).to_broadcast([128, 8, 128]), op=ALU.mult),
                 r=[K_("dmin"), K_("cbm")], w=[K_("M")])
            P.op("dve", lambda e: e.tensor_tensor(out=v3(r.xdt32), in0=v3(r.xst), in1=r.dtp[:].unsqueeze(2).to_broadcast([128, 8, 64]), op=ALU.mult), r=[K_("xst"), K_("dtp")], w=[K_("xdt32")])
            P.op("pool", lambda e: e.tensor_copy(out=r.xdtb[:], in_=r.xdt32[:]), r=[K_("xdt32")], w=[K_("xdtb")])
            P.op("dve", lambda e: e.tensor_tensor(out=v3(r.xdte), in0=v3(r.xdt32), in1=r.erest[:].unsqueeze(2).to_broadcast([128, 8, 64]), op=ALU.mult), r=[K_("xdt32"), K_("erest")], w=[K_("xdte")])
            yield
            for hh in range(8):
                P.op("pe", lambda e, hh=hh: e.matmul(PB(0)[:, hh * 64:(hh + 1) * 64], lhsT=r.M[:, hh, :], rhs=r.xdtb[:, hh * 64:(hh + 1) * 64], start=True, stop=True), r=[K_("M"), K_("xdtb")], w=[B_(0)])
            P.op("pe", lambda e: e.matmul(PB(1)[:, 0:512], lhsT=r.BCb[:, 1, :], rhs=r.STb[:], start=True, stop=True), r=[K_("BCb"), K_("STb")], w=[B_(1)])
            P.op("pe", lambda e: e.matmul(PB(2)[:, 0:512], lhsT=r.Btk[:], rhs=r.xdte[:], start=True, stop=True), r=[K_("Btk"), K_("xdte")], w=[B_(2)])
            yield
            if final:
                assert ("d:Y%d_%d" % (1 - d, n)) in P.lastw
                P.dma("pool", r.yf[b][:], YS[1 - d][row0:row0 + 128, :], r=["d:Y%d_%d" % (1 - d, n)], w=[K_("yf%d" % b)])
            P.op("dve", lambda e: e.tensor_tensor(out=v3(r.yt), in0=PB(1)[:, 0:512].rearrange("p (h q) -> p h q", h=8), in1=r.ecum[:].unsqueeze(2).to_broadcast([128, 8, 64]), op=ALU.mult),
                 r=[K_("ecum")], w=[B_(1), K_("yt")])
            P.op("dve", lambda e: e.tensor_tensor(out=r.yt[:], in0=PB(0)[:, 0:512], in1=r.yt[:], op=ALU.add), r=[], w=[B_(0), K_("yt")])
            P.op("dve", lambda e: e.tensor_tensor(out=v3(r.ST), in0=v3(r.ST), in1=r.dec[:].unsqueeze(2).to_broadcast([128, 8, 64]), op=ALU.mult), r=[K_("dec")], w=[K_("ST")])
            P.op("dve", lambda e: e.tensor_tensor(out=r.ST[:], in0=PB(2)[:, 0:512], in1=r.ST[:], op=ALU.add), r=[], w=[B_(2), K_("ST")])
            P.op("pool", lambda e: e.tensor_copy(out=r.STb[:], in_=r.ST[:]), r=[K_("ST")], w=[K_("STb")])
            if not lat:
                return
            if not final:
                P.dma("sp", YS[d][row0:row0 + 128, :], r.yt[:], r=[K_("yt")], w=["d:Y%d_%d" % (d, n)])
            else:
                P.op("pool", lambda e: e.tensor_tensor(out=r.yt[:], in0=r.yt[:], in1=r.yf[b][:], op=ALU.add), r=[K_("yf%d" % b)], w=[K_("yt")])
                P.op("dve", lambda e: e.tensor_tensor(out=v3(r.tmpd), in0=v3(r.xst), in1=dskb[:].unsqueeze(2).to_broadcast([128, 8, 64]), op=ALU.mult), r=[K_("xst"), "dskb"], w=[K_("tmpd")])
                P.op("pool", lambda e: e.tensor_tensor(out=r.yt[:], in0=r.yt[:], in1=r.tmpd[:], op=ALU.add), r=[K_("tmpd")], w=[K_("yt")])
                P.op("dve", lambda e: e.tensor_tensor(out=r.yt[:], in0=r.yt[:], in1=r.sz[:], op=ALU.mult), r=[K_("sz")], w=[K_("yt")])
                P.op("act", lambda e: e.activation(out=r.junk[:], in_=r.yt[:], func=AF.Square, accum_out=r.sq[b][:, 0:1]), r=[K_("yt")], w=[K_("junk"), K_("sq%d" % b)])
                P.op("dve", lambda e: e.tensor_tensor(out=r.yo[b][:], in0=r.yt[:], in1=ngb[:], op=ALU.mult), r=[K_("yt"), "ngb"], w=[K_("yo%d" % b)])
                P.dma("sp", yzo[row0:row0 + 128, :], r.yo[b][:], r=[K_("yo%d" % b)], w=["out:yz%d" % n])
                P.dma("sp", ssqo[row0:row0 + 128, :], r.sq[b][:, 0:1], r=[K_("sq%d" % b)], w=["out:sq%d" % n])

        loadw(0, 0); loadw(1, 0)

        def chain(d):
            for i in range(NCH):
                yield from step(d, i)
        ga, gb = chain(0), chain(1)
        for _ in range(3):
            next(ga)
        alive = [ga, gb]
        while alive:
            for g_ in list(alive):
                try:
                    next(g_)
                except StopIteration:
                    alive.remove(g_)


def k5_inputs(x2c, x2l, modv1, norm1_g1, od_w_in, od_conv_w, od_conv_b, ssd_a_log, ssd_dt_bias, ssd_d, ssd_norm_g):
    ident, tri, strict = consts_np()
    sel = np.zeros((8, 8, 128), np.float32)
    for hh in range(8):
        sel[hh, hh, :] = 1.0
    base = {"xl": np.ascontiguousarray(x2l), "xc": np.ascontiguousarray(x2c), "gn": feat_pc(norm1_g1),
            "shl": feat_pc(modv1[0, 0:2048]), "scl": feat_pc(modv1[0, 2048:4096]),
            "shc": feat_pc(modv1[1, 0:2048]), "scc": feat_pc(modv1[1, 2048:4096]),
            "ident": ident, "tri": tri, "strict": strict, "sel": sel.reshape(8, 1024)}
    W = od_w_in[0]; cwf = od_conv_w[0]; cbf = od_conv_b[0]
    maps = []
    for j in range(NCORES):
        cols = np.concatenate([4096 + 512 * j + np.arange(512), 8192 + 128 * j + np.arange(128), 9216 + 128 * j + np.arange(128),
                               512 * j + np.arange(512), 10240 + 8 * j + np.arange(8), 10304 + 8 * j + np.arange(8)])
        ch = np.stack([512 * j + 128 * k + np.arange(128) for k in range(4)] + [4096 + 128 * j + np.arange(128), 5120 + 128 * j + np.arange(128)], 1)
        m = dict(base)
        m.update({"win": np.ascontiguousarray(W[:, cols]),
                  "convw": np.ascontiguousarray(cwf[:, ch].transpose(1, 2, 0)), "convb": np.ascontiguousarray(cbf[ch]),
                  "alog": np.concatenate([ssd_a_log[0][0, 8 * j:8 * j + 8], ssd_a_log[0][1, 8 * j:8 * j + 8]]).astype(np.float32),
                  "dtb": np.concatenate([ssd_dt_bias[0][0, 8 * j:8 * j + 8], ssd_dt_bias[0][1, 8 * j:8 * j + 8]]).astype(np.float32),
                  "dsk": np.ascontiguousarray(ssd_d[0][8 * j:8 * j + 8]), "ng": np.ascontiguousarray(ssd_norm_g[0][512 * j:512 * j + 512])})
        maps.append(m)
    return maps


def run_k5(maps):
    P = build_k5()
    res = run_prog(P, maps)
    pre1T = np.zeros((4096, T_ALL), NPBF)
    for j, r in enumerate(res):
        pre1T[512 * j:512 * (j + 1), 256:] = r["yzo"].T
    ssq = np.concatenate([r["ssqo"] for r in res], 1)
    return pre1T, np.ascontiguousarray(ssq)


def kernel(x, c, ctx, c_ctx, mod_w, mod_b, norm1_g, norm2_g, ev_w_in, ev_conv_w, ev_conv_b, lru_wa, lru_ba, lru_wx, lru_bx,
           lru_lambda, gla_wg_up, gla_bg, gla_norm_g, ev_w_out, od_w_in, od_conv_w, od_conv_b, ssd_a_log, ssd_dt_bias, ssd_d,
           ssd_norm_g, od_w_out, router_w, router_b, exp_w_gate, exp_w_up, exp_w_down, final_norm_g):
    f = lambda a: np.asarray(a, np.float32)
    x = f(x); ctx = f(ctx)
    modv = run_k0(f(c), f(c_ctx), f(mod_w), f(mod_b))
    maps = k1_inputs(x, ctx, modv[0], f(norm1_g)[0], f(ev_w_in), f(ev_conv_w), f(ev_conv_b), f(lru_wa), f(lru_ba), f(lru_wx), f(lru_bx),
                     f(lru_lambda), f(gla_wg_up), f(gla_bg), f(gla_norm_g))
    preT = run_k1(maps)
    del maps
    x1c, x1l, h2T, gates = run_post(0, ctx[0], x[0], preT, f(ev_w_out)[0], modv[0], f(norm2_g)[0], f(router_w), f(router_b))
    parts = run_moe(h2T, gates, f(exp_w_gate)[0], f(exp_w_up)[0], f(exp_w_down)[0])
    x2c, x2l = run_combine(0, x1c, x1l, parts, modv[0])
    del parts, preT, h2T
    maps = k5_inputs(x2c, x2l, modv[1], f(norm1_g)[1], f(od_w_in), f(od_conv_w), f(od_conv_b), f(ssd_a_log), f(ssd_dt_bias), f(ssd_d), f(ssd_norm_g))
    pre1T, ssq = run_k5(maps)
    del maps
    _, x3l, h2T, gates = run_post(1, None, x2l, pre1T, f(od_w_out)[0], modv[1], f(norm2_g)[1], f(router_w), f(router_b), ssq_parts=ssq)
    parts = run_moe(h2T, gates, f(exp_w_gate)[1], f(exp_w_up)[1], f(exp_w_down)[1])
    _, out = run_combine(1, None, x3l, parts, modv[1], final_g=f(final_norm_g))
    return out.reshape(1, SEQ, D).astype(np.float32)
```
